# Optimizing a Trainium2 kernel written in Bass

```python
import math
import jax, jax.numpy as jnp
from jax import lax
import numpy as np

D_MODEL = 1024
BATCH = 8
SEQ = 2048
DEPTH = 4

HEAD_DIM = 64
ROPE_THETA = 10000.0
LN_EPS = 1e-5
A_HEADS = 8
IDX_HEADS = 8
IDX_DIM = 32
DSA_TOPK = 256
DSA_Q_BLOCK = 128
B_HEADS = 8
MOBA_BLOCK = 256
MOBA_TOPK = 3
MOBA_Q_CHUNK = 32
C_HEADS = 16
DILATED_CFG = ((128, 1), (512, 4), (2048, 16))
BAND_BLOCK = 128
D_FF = 2816
N_EXPERTS = 8
TOP_K = 2
D_FF_EXPERT = 3584
MOE_BLOCK = 256
DEEPNORM_ALPHA = (2 * DEPTH) ** 0.25
DEEPNORM_BETA = (8 * DEPTH) ** -0.25
N_EVEN = (DEPTH + 1) // 2
N_ODD = DEPTH // 2
EVEN_SPLITS = (A_HEADS * HEAD_DIM, HEAD_DIM, HEAD_DIM, IDX_HEADS * IDX_DIM, IDX_DIM, IDX_HEADS,
               B_HEADS * HEAD_DIM, B_HEADS * HEAD_DIM, B_HEADS * HEAD_DIM)
EVEN_IN = sum(EVEN_SPLITS)
EVEN_MIX = (A_HEADS + B_HEADS) * HEAD_DIM
ODD_MIX = C_HEADS * HEAD_DIM
ODD_IN = 3 * ODD_MIX

kernel_name = 'hybrid_dsa_moba_dilated_deepnorm_moe'

F32 = jnp.float32


def split_cols(h, sizes):
    idx = [int(v) for v in np.cumsum(sizes)[:-1]]
    return jnp.split(h, idx, axis=-1)


def layer_norm(x, g, b):
    xf = x.astype(F32)
    mu = xf.mean(-1, keepdims=True)
    var = jnp.mean(jnp.square(xf - mu), -1, keepdims=True)
    return ((xf - mu) * lax.rsqrt(var + LN_EPS) * g.astype(F32) + b.astype(F32)).astype(x.dtype)


def rope_tables(seq, dim):
    inv = ROPE_THETA ** (-jnp.arange(0, dim, 2, dtype=F32) / dim)
    ang = jnp.arange(seq, dtype=F32)[:, None] * inv[None, :]
    return jnp.cos(ang), jnp.sin(ang)


def apply_rope(x, cos, sin):
    half = x.shape[-1] // 2
    xf = x.astype(F32)
    x1, x2 = xf[..., :half], xf[..., half:]
    c, s = cos[None, :, None, :], sin[None, :, None, :]
    return jnp.concatenate([x1 * c - x2 * s, x1 * s + x2 * c], -1).astype(x.dtype)


def dsa_attention(q, k, v, iq, ik, iw):
    B, S, H, Dh = q.shape
    topk = min(DSA_TOPK, S // 4)
    qb = min(DSA_Q_BLOCK, S)
    nq = S // qb

    def chunk(t):
        return jnp.moveaxis(t.reshape((B, nq, qb) + t.shape[2:]), 1, 0)

    ikf = ik.astype(F32)
    key_pos = jnp.arange(S)
    take = jax.vmap(lambda rows, idx: rows[idx])

    def block(args):
        qc, iqc, iwc, start = args
        qpos = start + jnp.arange(qb)
        rel = jax.nn.relu(jnp.einsum('bqhi,bsi->bqhs', iqc.astype(F32), ikf) * IDX_DIM ** -0.5)
        score = jnp.einsum('bqhs,bqh->bqs', rel, iwc.astype(F32) * IDX_HEADS ** -0.5)
        causal = key_pos[None, :] <= qpos[:, None]
        score = jnp.where(causal[None], score, -jnp.inf)
        _, sel = lax.top_k(score, topk)
        valid = sel <= qpos[None, :, None]
        kg = take(k, sel)
        vg = take(v, sel)
        s = jnp.einsum('bqhd,bqkd->bqhk', qc, kg, preferred_element_type=F32) * Dh ** -0.5
        s = jnp.where(valid[:, :, None, :], s, -jnp.inf)
        p = jax.nn.softmax(s, axis=-1).astype(v.dtype)
        return jnp.einsum('bqhk,bqkd->bqhd', p, vg)

    starts = jnp.arange(nq, dtype=jnp.int32) * qb
    o = lax.map(block, (chunk(q), chunk(iq), chunk(iw), starts))
    return jnp.moveaxis(o, 0, 1).reshape(B, S, H, Dh)


def moba_attention(q, k, v):
    B, S, H, Dh = q.shape
    bs = MOBA_BLOCK
    nb = -(-S // bs)
    Sp = nb * bs
    scale = Dh ** -0.5
    pad = ((0, 0), (0, Sp - S), (0, 0), (0, 0))
    qb = jnp.pad(q, pad).reshape(B, nb, bs, H, Dh)
    kb = jnp.pad(k, pad).reshape(B, nb, bs, H, Dh)
    vb = jnp.pad(v, pad).reshape(B, nb, bs, H, Dh)
    s_own = jnp.einsum('bnqhd,bnkhd->bnhqk', qb, kb, preferred_element_type=F32) * scale
    tri = jnp.tril(jnp.ones((bs, bs), dtype=bool))
    s_own = jnp.where(tri, s_own, -jnp.inf)
    lse_own = jax.nn.logsumexp(s_own, axis=-1)
    p_own = jnp.exp(s_own - lse_own[..., None]).astype(v.dtype)
    o_own = jnp.einsum('bnhqk,bnkhd->bnqhd', p_own, vb).reshape(B, Sp, H, Dh)[:, :S]
    lse_own = jnp.moveaxis(lse_own, 2, 3).reshape(B, Sp, H)[:, :S]
    kt = min(MOBA_TOPK, nb - 1)
    if kt == 0:
        return o_own
    kmean = kb.astype(F32).mean(axis=2)
    gate = jnp.einsum('bshd,bnhd->bshn', q.astype(F32), kmean)
    qblk = jnp.arange(S) // bs
    past = jnp.arange(nb)[None, :] < qblk[:, None]
    gate = jnp.where(past[None, :, None, :], gate, -jnp.inf)
    _, sel = lax.top_k(gate, kt)
    valid = sel < qblk[None, :, None, None]
    kbh = jnp.transpose(kb, (0, 3, 1, 2, 4))
    vbh = jnp.transpose(vb, (0, 3, 1, 2, 4))
    take = jax.vmap(jax.vmap(lambda blocks, idx: blocks[idx]))
    qc_size = min(MOBA_Q_CHUNK, S)
    nc = S // qc_size

    def chunk(t):
        return jnp.moveaxis(t.reshape((B, nc, qc_size) + t.shape[2:]), 1, 0)

    def block(args):
        qc, selc, validc, oc, lc = args
        qh = jnp.swapaxes(qc, 1, 2)
        selh = jnp.swapaxes(selc, 1, 2)
        validh = jnp.swapaxes(validc, 1, 2)
        kg = take(kbh, selh)
        vg = take(vbh, selh)
        s = jnp.einsum('bhcd,bhcjkd->bhcjk', qh, kg, preferred_element_type=F32) * scale
        s = jnp.where(validh[..., None], s, -jnp.inf)
        m = jnp.where(jnp.any(validh, -1), jnp.max(s, axis=(-2, -1)), 0.0)
        e = jnp.exp(s - m[..., None, None])
        den = e.sum(axis=(-2, -1))
        num = jnp.einsum('bhcjk,bhcjkd->bhcd', e.astype(v.dtype), vg, preferred_element_type=F32)
        lo = jnp.swapaxes(lc, 1, 2)
        oo = jnp.swapaxes(oc, 1, 2).astype(F32)
        mx = jnp.maximum(lo, m)
        a = jnp.exp(lo - mx)
        c = jnp.exp(m - mx)
        out = (a[..., None] * oo + c[..., None] * num) / (a + c * den)[..., None]
        return jnp.swapaxes(out, 1, 2).astype(v.dtype)

    o = lax.map(block, (chunk(q), chunk(sel), chunk(valid), chunk(o_own), chunk(lse_own)))
    return jnp.moveaxis(o, 0, 1).reshape(B, S, H, Dh)


def dilated_branch(q, k, v, window, dil):
    B, S, H, Dh = q.shape
    L = S // dil
    wsub = window // dil
    blk = BAND_BLOCK
    nblk = -(-L // blk)
    Lp = nblk * blk

    def strided(t):
        t = t.reshape(B, L, dil, H, Dh).transpose(0, 2, 1, 3, 4)
        t = jnp.pad(t, ((0, 0), (0, 0), (0, Lp - L), (0, 0), (0, 0)))
        return t.reshape(B, dil, nblk, blk, H, Dh)

    def with_prev(t):
        prev = jnp.pad(t, ((0, 0), (0, 0), (1, 0), (0, 0), (0, 0), (0, 0)))[:, :, :-1]
        return jnp.concatenate([prev, t], axis=3)

    qs = strided(q)
    kk = with_prev(strided(k))
    vv = with_prev(strided(v))
    i = jnp.arange(blk)[:, None]
    j = jnp.arange(2 * blk)[None, :]
    dist = blk + i - j
    key_u = jnp.arange(nblk)[:, None, None] * blk - blk + j[None]
    mask = (dist >= 0)[None] & (dist <= wsub)[None] & (key_u >= 0)
    s = jnp.einsum('bgnqhd,bgnkhd->bgnhqk', qs, kk, preferred_element_type=F32) * Dh ** -0.5
    s = jnp.where(mask[None, None, :, None], s, -jnp.inf)
    lse = jax.nn.logsumexp(s, axis=-1)
    p = jnp.exp(s - lse[..., None]).astype(v.dtype)
    o = jnp.einsum('bgnhqk,bgnkhd->bgnqhd', p, vv)
    o = o.reshape(B, dil, Lp, H, Dh)[:, :, :L].transpose(0, 2, 1, 3, 4).reshape(B, S, H, Dh)
    lse = jnp.moveaxis(lse, 3, 4).reshape(B, dil, Lp, H)[:, :, :L].transpose(0, 2, 1, 3).reshape(B, S, H)
    return o, lse


def dilated_mixture(q, k, v):
    outs, lses = [], []
    for window, dil in DILATED_CFG:
        o, l = dilated_branch(q, k, v, window, dil)
        outs.append(o)
        lses.append(l)
    wts = jax.nn.softmax(jnp.stack(lses, 0), axis=0)
    o = jnp.sum(wts[..., None] * jnp.stack(outs, 0).astype(F32), axis=0)
    return o.astype(v.dtype)


def even_mixer(x, w_in, w_out, cos64, sin64, cos32, sin32):
    B, S, _ = x.shape
    qa, ka, va, iq, ik, iw, qb, kb, vb = split_cols(x @ w_in, EVEN_SPLITS)
    qa = apply_rope(qa.reshape(B, S, A_HEADS, HEAD_DIM), cos64, sin64)
    ka = apply_rope(ka.reshape(B, S, 1, HEAD_DIM), cos64, sin64)[:, :, 0]
    iq = apply_rope(iq.reshape(B, S, IDX_HEADS, IDX_DIM), cos32, sin32)
    ik = apply_rope(ik.reshape(B, S, 1, IDX_DIM), cos32, sin32)[:, :, 0]
    shp = (B, S, B_HEADS, HEAD_DIM)
    qb = apply_rope(qb.reshape(shp), cos64, sin64)
    kb = apply_rope(kb.reshape(shp), cos64, sin64)
    o_a = dsa_attention(qa, ka, va, iq, ik, iw)
    o_b = moba_attention(qb, kb, vb.reshape(shp))
    o = jnp.concatenate([o_a.reshape(B, S, -1), o_b.reshape(B, S, -1)], axis=-1)
    return o @ w_out


def odd_mixer(x, w_in, w_out, cos64, sin64):
    B, S, _ = x.shape
    q, k, v = split_cols(x @ w_in, (ODD_MIX, ODD_MIX, ODD_MIX))
    shp = (B, S, C_HEADS, HEAD_DIM)
    q = apply_rope(q.reshape(shp), cos64, sin64)
    k = apply_rope(k.reshape(shp), cos64, sin64)
    o = dilated_mixture(q, k, v.reshape(shp))
    return o.reshape(B, S, ODD_MIX) @ w_out


def swiglu(x, w1, w3, w2):
    return (jax.nn.silu(x @ w1) * (x @ w3)) @ w2


def moe_swiglu(x2d, router, w1, w3, w2):
    n_tok, dm = x2d.shape
    logits = (x2d @ router).astype(F32)
    top_val, top_exp = lax.top_k(logits, TOP_K)
    gates = jax.nn.softmax(top_val, axis=-1)
    n_asg = n_tok * TOP_K
    eid = top_exp.reshape(n_asg)
    tok = jnp.repeat(jnp.arange(n_tok, dtype=jnp.int32), TOP_K)
    gate = gates.reshape(n_asg)
    order = jnp.argsort(eid)
    es, ts, gs = eid[order], tok[order], gate[order]
    counts = jnp.bincount(eid, length=N_EXPERTS)
    starts = jnp.cumsum(counts) - counts
    padded = (counts + MOE_BLOCK - 1) // MOE_BLOCK * MOE_BLOCK
    ends = jnp.cumsum(padded)
    pstarts = ends - padded
    dest = pstarts[es] + jnp.arange(n_asg) - starts[es]
    n_blocks = -(-n_asg // MOE_BLOCK) + N_EXPERTS
    xbuf = jnp.zeros((n_blocks * MOE_BLOCK, dm), x2d.dtype).at[dest].set(x2d[ts])
    block_exp = jnp.clip(jnp.searchsorted(ends, jnp.arange(n_blocks) * MOE_BLOCK, side='right'), 0, N_EXPERTS - 1)

    def expert_block(args):
        xb, e = args
        return (jax.nn.silu(xb @ w1[e]) * (xb @ w3[e])) @ w2[e]

    ybuf = lax.map(expert_block, (xbuf.reshape(n_blocks, MOE_BLOCK, dm), block_exp)).reshape(-1, dm)
    contrib = ybuf[dest] * gs[:, None].astype(x2d.dtype)
    return jnp.zeros_like(x2d).at[ts].add(contrib)


def setup_inputs(seed: int = 0) -> dict:
    key = jax.random.key(seed)
    ks = jax.random.split(key, 20)

    def nrm(k, shape, scale):
        return jax.random.normal(k, shape, F32) * scale

    beta = DEEPNORM_BETA
    hd = HEAD_DIM
    even_col = jnp.concatenate([
        jnp.ones((A_HEADS * hd + hd,), F32), jnp.full((hd,), beta, F32),
        jnp.ones((IDX_HEADS * IDX_DIM + IDX_DIM + IDX_HEADS + 2 * B_HEADS * hd,), F32),
        jnp.full((B_HEADS * hd,), beta, F32)])
    odd_col = jnp.concatenate([jnp.ones((2 * ODD_MIX,), F32), jnp.full((ODD_MIX,), beta, F32)])
    d = D_MODEL
    return {
        'x': nrm(ks[0], (BATCH, SEQ, d), 1.0),
        'even_w_in': nrm(ks[1], (N_EVEN, d, EVEN_IN), d ** -0.5) * even_col,
        'even_w_out': nrm(ks[2], (N_EVEN, EVEN_MIX, d), EVEN_MIX ** -0.5 * beta),
        'even_ln1_g': 1.0 + nrm(ks[3], (N_EVEN, d), 0.02),
        'even_ln1_b': nrm(ks[4], (N_EVEN, d), 0.02),
        'even_w1': nrm(ks[5], (N_EVEN, d, D_FF), d ** -0.5),
        'even_w3': nrm(ks[6], (N_EVEN, d, D_FF), d ** -0.5),
        'even_w2': nrm(ks[7], (N_EVEN, D_FF, d), D_FF ** -0.5 * beta),
        'even_ln2_g': 1.0 + nrm(ks[8], (N_EVEN, d), 0.02),
        'even_ln2_b': nrm(ks[9], (N_EVEN, d), 0.02),
        'odd_w_in': nrm(ks[10], (N_ODD, d, ODD_IN), d ** -0.5) * odd_col,
        'odd_w_out': nrm(ks[11], (N_ODD, ODD_MIX, d), ODD_MIX ** -0.5 * beta),
        'odd_ln1_g': 1.0 + nrm(ks[12], (N_ODD, d), 0.02),
        'odd_ln1_b': nrm(ks[13], (N_ODD, d), 0.02),
        'odd_router': nrm(ks[14], (N_ODD, d, N_EXPERTS), d ** -0.5),
        'odd_w1': nrm(ks[15], (N_ODD, N_EXPERTS, d, D_FF_EXPERT), d ** -0.5),
        'odd_w3': nrm(ks[16], (N_ODD, N_EXPERTS, d, D_FF_EXPERT), d ** -0.5),
        'odd_w2': nrm(ks[17], (N_ODD, N_EXPERTS, D_FF_EXPERT, d), D_FF_EXPERT ** -0.5 * beta),
        'odd_ln2_g': 1.0 + nrm(ks[18], (N_ODD, d), 0.02),
        'odd_ln2_b': nrm(ks[19], (N_ODD, d), 0.02),
    }


def reference(x, even_w_in, even_w_out, even_ln1_g, even_ln1_b, even_w1, even_w3, even_w2,
              even_ln2_g, even_ln2_b, odd_w_in, odd_w_out, odd_ln1_g, odd_ln1_b, odd_router,
              odd_w1, odd_w3, odd_w2, odd_ln2_g, odd_ln2_b):
    B, S, D = x.shape
    cos64, sin64 = rope_tables(S, HEAD_DIM)
    cos32, sin32 = rope_tables(S, IDX_DIM)
    a = DEEPNORM_ALPHA
    for layer in range(DEPTH):
        i = layer // 2
        if layer % 2 == 0:
            mix = even_mixer(x, even_w_in[i], even_w_out[i], cos64, sin64, cos32, sin32)
            x = layer_norm(a * x + mix, even_ln1_g[i], even_ln1_b[i])
            ffn = swiglu(x, even_w1[i], even_w3[i], even_w2[i])
            x = layer_norm(a * x + ffn, even_ln2_g[i], even_ln2_b[i])
        else:
            mix = odd_mixer(x, odd_w_in[i], odd_w_out[i], cos64, sin64)
            x = layer_norm(a * x + mix, odd_ln1_g[i], odd_ln1_b[i])
            ffn = moe_swiglu(x.reshape(B * S, D), odd_router[i], odd_w1[i], odd_w3[i], odd_w2[i]).reshape(B, S, D)
            x = layer_norm(a * x + ffn, odd_ln2_g[i], odd_ln2_b[i])
    return x
```

```python
import numpy as np
import concourse.bass as bass
import concourse.mybir as mybir
from concourse.bass_utils import run_bass_kernel_spmd
from contextlib import ExitStack

F32 = mybir.dt.float32
BF16 = mybir.dt.bfloat16
U8 = mybir.dt.uint8
ALU = mybir.AluOpType
AF = mybir.ActivationFunctionType
AX = mybir.AxisListType

SEQ = 2048
DM = 1024
NT = 16
DEPTH = 4
ALPHA = float((2 * DEPTH) ** 0.25)
EPS = 1e-5
NEG = -30000.0
DFF = 2816
DFFE = 3584
NEXP = 8
KB = 1024
MOE_C = 640
SPARSE_MOE = True

ENGS = ['tensor', 'vector', 'scalar', 'gpsimd', 'sync']


class _Op(object):
    __slots__ = ('eng', 'fn', 'waits', 'dma_sem', 'idx', 'signal', 'count', 'dma_count')


class Sched(object):
    def __init__(self):
        self.ops = dict((e, []) for e in ENGS)
        self.last_write = {}
        self.readers = {}
        self.waited = dict((e, {}) for e in ENGS)
        self.dma_counts = {}
        self.dma_last = {}
        self.last_compute = {}

    def _dep(self, o, d, kind):
        if d is None or d is o:
            return
        if d.dma_sem is None:
            if d.eng == o.eng and o.dma_sem is None:
                if o.eng == 'tensor':
                    return
            key = d.eng
            val = d.idx
        else:
            key = ('dma', d.dma_sem)
            val = d.dma_count
        w = self.waited[o.eng]
        if w.get(key, -1) >= val:
            return
        w[key] = val
        d.signal = True
        o.waits.append(d)

    def op(self, eng, fn, reads=(), writes=(), dma_sem=None):
        o = _Op()
        o.eng = eng
        o.fn = fn
        o.waits = []
        o.dma_sem = dma_sem
        o.signal = False
        o.idx = len(self.ops[eng])
        if dma_sem is not None:
            c = self.dma_counts.get(dma_sem, 0) + 1
            self.dma_counts[dma_sem] = c
            o.dma_count = c
            self.dma_last[dma_sem] = o
        elif fn is not None:
            self.last_compute[eng] = o
        writes = list(writes) + [r for r in reads if r.startswith('ps') and r not in writes]
        reads = [r for r in reads if not r.startswith('ps')]
        for r in reads:
            self._dep(o, self.last_write.get(r), 'raw')
        for w_ in writes:
            self._dep(o, self.last_write.get(w_), 'waw')
            for rd in self.readers.get(w_, ()):
                self._dep(o, rd, 'war')
        for r in reads:
            self.readers.setdefault(r, []).append(o)
        for w_ in writes:
            self.last_write[w_] = o
            self.readers[w_] = []
        self.ops[eng].append(o)
        return o

    def barrier(self):
        lasts = list(self.last_compute.values()) + list(self.dma_last.values())
        for e in ENGS:
            o = self.op(e, None)
            for d in lasts:
                if d.dma_sem is None and d.eng == e and e in ('tensor', 'sync'):
                    continue
                self._dep(o, d, 'raw')
        self.last_write = {}
        self.readers = {}

    def emit(self, nc):
        dma_names = sorted(self.dma_counts.keys(), key=str)
        with ExitStack() as st:
            esem = {}
            for e in ENGS:
                esem[e] = st.enter_context(nc.semaphore("s_" + e))
            dsem = {}
            for i, n in enumerate(dma_names):
                dsem[n] = st.enter_context(nc.semaphore("d%d" % i))
            for e in ENGS:
                c = 0
                for o in self.ops[e]:
                    if o.dma_sem is None and o.signal:
                        assert o.fn is not None
                        c += 1
                        o.count = c
            block = st.enter_context(nc.Block())

            def mk(e):
                def body(eng):
                    for o in self.ops[e]:
                        for d in o.waits:
                            if d.dma_sem is None:
                                eng.wait_ge(esem[d.eng], d.count)
                            else:
                                eng.wait_ge(dsem[d.dma_sem], 16 * d.dma_count)
                        if o.fn is None:
                            continue
                        inst = o.fn(eng)
                        if o.dma_sem is not None:
                            inst.then_inc(dsem[o.dma_sem], 16)
                        elif o.signal:
                            inst.then_inc(esem[e], 1)
                return body

            for e in ENGS:
                if self.ops[e]:
                    getattr(block, e)(mk(e))


def _rope_tab(dim):
    inv = (np.float32(10000.0) ** (-np.arange(0, dim, 2, dtype=np.float32) / np.float32(dim))).astype(np.float32)
    ang = (np.arange(SEQ, dtype=np.float32)[:, None] * inv[None, :]).astype(np.float32)
    return np.cos(ang).astype(np.float32), np.sin(ang).astype(np.float32)


def make_consts():
    c = {}
    c['ident'] = np.eye(128, dtype=np.float32)
    cos64, sin64 = _rope_tab(64)
    cos32, sin32 = _rope_tab(32)
    p = np.arange(128)
    c['c64'] = np.ascontiguousarray(cos64.T[p % 32, :])
    sg = np.where((p % 64) < 32, -1.0, 1.0).astype(np.float32)[:, None]
    c['s64'] = np.ascontiguousarray(sin64.T[p % 32, :] * sg)
    c['c32'] = np.ascontiguousarray(cos32.T[p % 16, :])
    sg = np.where((p % 32) < 16, -1.0, 1.0).astype(np.float32)[:, None]
    c['s32'] = np.ascontiguousarray(sin32.T[p % 16, :] * sg)
    d = np.arange(SEQ)[None, :] - np.arange(128)[:, None]
    c['tcaus'] = np.where(d >= 0, 0.0, NEG).astype(np.float32)
    mult = ((d >= 0) & (d <= 128)).astype(np.int32) + ((d >= 0) & (d <= 512) & (d % 4 == 0)).astype(np.int32) \
        + ((d >= 0) & (d % 16 == 0)).astype(np.int32)
    lut = np.array([NEG, 0.0, 8.0 * np.log(2.0), 8.0 * np.log(3.0)], dtype=np.float32)
    c['tdil'] = lut[mult].astype(np.float32)
    e = np.zeros((8, 8, 128), np.float32)
    for n in range(8):
        e[n, n, :] = 1.0
    c['eoh'] = e.reshape(8, 1024)
    gm = np.zeros((128, 16, 8), np.float32)
    for qt in range(16):
        gm[:, qt, (qt // 2):] = -1e30
    c['gmask'] = gm.reshape(128, 128)
    tp = np.arange(128)
    c['tris'] = (tp[:, None] < tp[None, :]).astype(np.float32)
    c['iotar'] = np.tile(np.arange(MOE_C, dtype=np.float32)[None, :], (128, 1))
    c['iotap'] = (tp[:, None] + 128.0 * np.arange(8)[None, :]).astype(np.float32)
    return c


def swap_halves(w, lo, nheads, hd):
    out = w
    for h in range(nheads):
        a = lo + h * hd
        first = w[..., a:a + hd // 2].copy()
        out[..., a:a + hd // 2] = w[..., a + hd // 2:a + hd]
        out[..., a + hd // 2:a + hd] = first
    return out


def build(layer_ids, stop=99):
    nc = bass.Bass("TRN2", target_bir_lowering=False)
    S = Sched()
    op = S.op
    dr = {}

    def din(name, shape):
        dr[name] = nc.dram_tensor(name, list(shape), F32, kind="ExternalInput").ap()
        return dr[name]

    x_in = din("x", [SEQ, DM])
    for n, shp in (('ident', [128, 128]), ('c64', [128, SEQ]), ('s64', [128, SEQ]), ('c32', [128, SEQ]),
                   ('s32', [128, SEQ]), ('tcaus', [128, SEQ]), ('tdil', [128, SEQ]), ('eoh', [8, 1024]),
                   ('gmask', [128, 128]), ('tris', [128, 128]), ('iotar', [128, MOE_C]), ('iotap', [128, 8])):
        din(n, shp)
    for L in layer_ids:
        if L % 2 == 0:
            din("win%d" % L, [DM, 2472]); din("wsw%d" % L, [DM, 2472]); din("wout%d" % L, [DM, DM])
            din("w1_%d" % L, [DM, DFF]); din("w3_%d" % L, [DM, DFF]); din("w2_%d" % L, [DFF, DM])
        else:
            din("win%d" % L, [DM, 3072]); din("wsw%d" % L, [DM, 3072]); din("wout%d" % L, [DM, DM])
            din("rt%d" % L, [DM, NEXP])
            din("w1_%d" % L, [NEXP, DM, DFFE]); din("w3_%d" % L, [NEXP, DM, DFFE]); din("w2_%d" % L, [NEXP, DFFE, DM])
        for n in ('g1', 'b1', 'g2', 'b2'):
            din("%s_%d" % (n, L), [1, DM])
    y_out = nc.dram_tensor("y", [SEQ, DM], F32, kind="ExternalOutput").ap()
    xsp = nc.dram_tensor("xsp", [SEQ, DM], F32, kind="Internal").ap()

    st = ExitStack()
    big = st.enter_context(nc.sbuf_tensor("big", [128, 204 * KB], U8))
    P = st.enter_context(nc.psum_tensor("P", [128, 8, 512], F32))

    def V(off, shape, dt):
        esz = 4 if dt == F32 else 2
        n = 1
        for s_ in shape[1:]:
            n *= s_
        v = big[0:shape[0], off:off + n * esz].bitcast(dt)
        if len(shape) == 3:
            v = v.rearrange("p (a b) -> p a b", a=shape[1])
        elif len(shape) == 4:
            v = v.rearrange("p (a b c) -> p a b c", a=shape[1], b=shape[2])
        return v

    def PS(i):
        return P[:, i, :]

    def PSB(i):
        return P[:, i, :].bitcast(BF16)

    xT = V(0, [128, 8, SEQ], BF16)
    lnG = V(32 * KB, [128, DM], F32)
    lnB = V(36 * KB, [128, DM], F32)
    identb = V(40 * KB, [128, 128], BF16)
    onesf = V(40 * KB + 256, [128, 128], F32)
    stats = V(41 * KB, [128, 2, 2, 6], F32)
    mv = V(41 * KB + 128, [128, 2, 2], F32)
    rstd = V(41 * KB + 192, [128, 2, 1], F32)
    m8 = V(41 * KB + 256, [128, 8], F32)
    Z0 = 42 * KB

    def fm(f, *a):
        return lambda e: f(e, *a)

    op('gpsimd', lambda e: e.dma_start(out=identb, in_=dr['ident']), writes=['identb'], dma_sem='c0')
    op('vector', lambda e: e.memset(onesf, 1.0), writes=['onesf'])

    def emit_xT(tt, xb, xb_res, bank):
        def tr(e):
            pv = PSB(bank)
            for c in range(8):
                i = e.transpose(out=pv[:, c * 128:(c + 1) * 128], in_=xb[:, c * 128:(c + 1) * 128], identity=identb)
            return i
        op('tensor', tr, reads=[xb_res, 'identb'], writes=['ps%d' % bank])
        op('vector', lambda e: e.tensor_copy(out=xT[:, :, tt * 128:(tt + 1) * 128],
                                             in_=PSB(bank).rearrange("p (a b) -> p a b", a=8)),
           reads=['ps%d' % bank], writes=['xT'])

    def emit_ln(z, z_res, xn, xn_res, xb, xb_res, r, z_res2=None):
        z_res2 = z_res2 or z_res
        st_ = stats[:, r]
        op('vector', lambda e: e.bn_stats(out=st_[:, 0, :], in_=z[:, 0:512]), reads=[z_res], writes=['stats%d' % r])
        op('vector', lambda e: e.bn_stats(out=st_[:, 1, :], in_=z[:, 512:1024]), reads=[z_res2], writes=['stats%db' % r])
        op('vector', lambda e: e.bn_aggr(out=mv[:, r, :], in_=st_.rearrange("p a b -> p (a b)")),
           reads=['stats%d' % r, 'stats%db' % r], writes=['mv%d' % r])
        op('vector', lambda e: e.tensor_scalar(out=rstd[:, r, :], in0=mv[:, r, 1:2], scalar1=EPS, scalar2=None,
                                               op0=ALU.add), reads=['mv%d' % r], writes=['rstd%d' % r])
        op('scalar', lambda e: e.activation(out=rstd[:, r, :], in_=rstd[:, r, :], func=AF.Sqrt),
           reads=['rstd%d' % r], writes=['rstd%d' % r])
        op('vector', lambda e: e.reciprocal(out=rstd[:, r, :], in_=rstd[:, r, :]), reads=['rstd%d' % r], writes=['rstd%d' % r])
        op('vector', lambda e: e.tensor_scalar(out=xn, in0=z, scalar1=mv[:, r, 0:1], scalar2=rstd[:, r, :],
                                               op0=ALU.subtract, op1=ALU.mult),
           reads=[z_res, z_res2, 'mv%d' % r, 'rstd%d' % r], writes=[xn_res])
        op('gpsimd', lambda e: e.tensor_tensor(out=xn, in0=xn, in1=lnG, op=ALU.mult), reads=[xn_res, 'lnG'], writes=[xn_res])
        op('gpsimd', lambda e: e.tensor_tensor(out=xn, in0=xn, in1=lnB, op=ALU.add), reads=[xn_res, 'lnB'], writes=[xn_res])
        if xb is not None:
            op('scalar', lambda e: e.copy(out=xb, in_=xn), reads=[xn_res], writes=[xb_res])

    def load_ln_params(L, which):
        g = dr["g%d_%d" % (which, L)]
        b = dr["b%d_%d" % (which, L)]
        op('sync', lambda e: e.dma_start(out=lnG, in_=g.to_broadcast([128, DM])), writes=['lnG'], dma_sem='lng')
        op('sync', lambda e: e.dma_start(out=lnB, in_=b.to_broadcast([128, DM])), writes=['lnB'], dma_sem='lnb')

    wcount = [0]

    def proj_fm(wsrc, wswsrc, col_specs, ncols, rope, dst_fn, dst_res, WB, Ct=None, St=None):
        s_ = wcount[0] % 2
        wcount[0] += 1
        wt = V(WB + s_ * 4 * KB, [128, 8, 128], BF16)
        ws = V(WB + s_ * 4 * KB + 2 * KB, [128, 8, 128], BF16)
        for (doff, slo, n) in col_specs:
            op('gpsimd', fm(lambda e, doff, slo, n: e.dma_start(
                out=wt[:, :, doff:doff + n], in_=wsrc[:, slo:slo + n].rearrange("(c p) n -> p c n", p=128)), doff, slo, n),
               writes=['wt%d' % s_], dma_sem='wt%d' % s_)
            if rope:
                op('gpsimd', fm(lambda e, doff, slo, n: e.dma_start(
                    out=ws[:, :, doff:doff + n], in_=wswsrc[:, slo:slo + n].rearrange("(c p) n -> p c n", p=128)), doff, slo, n),
                   writes=['ws%d' % s_], dma_sem='ws%d' % s_)
        for tc in range(4):
            pa = tc % 2
            tok = slice(tc * 512, (tc + 1) * 512)

            def mm(e, w_, bank, tok=tok):
                for k in range(8):
                    i = e.matmul(PS(bank)[0:ncols, :], lhsT=w_[:, k, 0:ncols], rhs=xT[:, k, tok],
                                 start=(k == 0), stop=(k == 7))
                return i
            op('tensor', fm(mm, wt, pa), reads=['wt%d' % s_, 'xT'], writes=['ps%d' % pa])
            dst = dst_fn(tc)
            if rope:
                op('tensor', fm(mm, ws, 2 + pa), reads=['ws%d' % s_, 'xT'], writes=['ps%d' % (2 + pa)])
                t1 = V(WB + 8 * KB + pa * 4 * KB, [128, 512], F32)
                t2 = V(WB + 8 * KB + pa * 4 * KB + 2 * KB, [128, 512], F32)
                op('vector', fm(lambda e, t1, pa, tok: e.tensor_tensor(out=t1[0:ncols, :], in0=PS(pa)[0:ncols, :],
                                                                       in1=Ct[0:ncols, tok], op=ALU.mult), t1, pa, tok),
                   reads=['ps%d' % pa, 'ctab', 'ctab32'], writes=['t1_%d' % pa])
                op('vector', fm(lambda e, t2, pa, tok: e.tensor_tensor(out=t2[0:ncols, :], in0=PS(2 + pa)[0:ncols, :],
                                                                       in1=St[0:ncols, tok], op=ALU.mult), t2, pa, tok),
                   reads=['ps%d' % (2 + pa), 'stab', 'stab32'], writes=['t2_%d' % pa])
                op('gpsimd', fm(lambda e, t1, t2, dst: e.tensor_tensor(out=dst, in0=t1[0:ncols, :], in1=t2[0:ncols, :],
                                                                       op=ALU.add), t1, t2, dst),
                   reads=['t1_%d' % pa, 't2_%d' % pa], writes=[dst_res])
            else:
                op('scalar', fm(lambda e, dst, pa: e.copy(out=dst, in_=PS(pa)[0:ncols, :]), dst, pa),
                   reads=['ps%d' % pa], writes=[dst_res])

    def proj_tm(wsrc, slo, ncols, evac, WB):
        s_ = wcount[0] % 2
        wcount[0] += 1
        wt = V(WB + s_ * 4 * KB, [128, 8, 128], BF16)
        op('gpsimd', lambda e: e.dma_start(out=wt[:, :, 0:ncols],
                                           in_=wsrc[:, slo:slo + ncols].rearrange("(c p) n -> p c n", p=128)),
           writes=['wt%d' % s_], dma_sem='wt%d' % s_)
        for tt in range(NT):
            bank = 4 + tt % 2

            def mm(e, tt=tt, bank=bank):
                for k in range(8):
                    i = e.matmul(PS(bank)[:, 0:ncols], lhsT=xT[:, k, tt * 128:(tt + 1) * 128], rhs=wt[:, k, 0:ncols],
                                 start=(k == 0), stop=(k == 7))
                return i
            op('tensor', mm, reads=['wt%d' % s_, 'xT'], writes=['ps%d' % bank])
            evac(tt, PS(bank)[:, 0:ncols], bank)

    ucount = [0]
    hcount = [0]
    deferred = []

    def defer(delay, fn):
        deferred.append([delay, fn])

    def tick():
        due = []
        for it in deferred:
            it[0] -= 1
        for it in list(deferred):
            if it[0] <= 0:
                due.append(it)
                deferred.remove(it)
        for it in due:
            it[1]()

    def flush_all():
        while deferred:
            it = deferred.pop(0)
            it[1]()

    def attn_head(chunks, kT_fn, qT_fn, bias_fn, V_fn, parity, OTdst_fn, reads, WKB):
        pT = [V(WKB + i * KB, [128, 512], BF16) for i in range(3)]
        rd = V(WKB + 3 * KB, [128, 512], F32)
        osb = [V(WKB + 5 * KB + i * 2 * KB, [128, 512], F32) for i in range(2)]
        M = 65 if parity == 0 else 128
        for (q0, N) in chunks:
            hc = hcount[0]
            hcount[0] += 1
            ob = 2 + hc % 2
            ktmax = (q0 + N - 1) // 128
            for kt in range(ktmax + 1):
                u = ucount[0]
                ucount[0] += 1
                sb = u % 2
                pb = u % 3
                qs = max(q0, kt * 128)
                n = q0 + N - qs
                bl, br = bias_fn(kt, qs, n, q0)

                def mms(e, kt=kt, qs=qs, n=n, sb=sb, bl=bl, br=br):
                    e.matmul(PS(sb)[:, 0:n], lhsT=kT_fn(kt), rhs=qT_fn(qs, n), start=True, stop=False)
                    return e.matmul(PS(sb)[:, 0:n], lhsT=bl, rhs=br, start=False, stop=True)
                op('tensor', mms, reads=list(reads), writes=['ps%d' % sb])
                op('scalar', fm(lambda e, pb, sb, n: e.activation(out=pT[pb][:, 0:n], in_=PS(sb)[:, 0:n], func=AF.Exp,
                                                                  scale=0.125), pb, sb, n),
                   reads=['ps%d' % sb], writes=['pT%d' % pb])

                def mmo(e, kt=kt, qs=qs, n=n, pb=pb, ob=ob, q0=q0, ktmax=ktmax):
                    return e.matmul(PS(ob)[0:M, qs - q0:qs - q0 + n], lhsT=V_fn(kt), rhs=pT[pb][:, 0:n],
                                    start=(kt == 0), stop=(kt == ktmax), skip_group_check=True)
                tick()
                defer(1, lambda mmo=mmo, pb=pb, ob=ob: op('tensor', mmo, reads=['pT%d' % pb] + list(reads),
                                                          writes=['ps%d' % ob]))
                if kt == ktmax:
                    dp = 64 if parity == 0 else 0
                    lo, hi = (0, 64) if parity == 0 else (64, 128)
                    mo = 64 if parity == 0 else 128
                    osl = osb[hc % 2]
                    dst = OTdst_fn(q0, N)

                    def norm1(ob=ob, N=N, dp=dp):
                        op('vector', lambda e: e.reciprocal(out=rd[dp:dp + 1, 0:N], in_=PS(ob)[dp:dp + 1, 0:N]),
                           reads=['ps%d' % ob], writes=['rd'])

                    def norm2(ob=ob, N=N, dp=dp, lo=lo, hi=hi, mo=mo, osl=osl, dst=dst, hc=hc):
                        op('tensor', lambda e: e.matmul(PS(4)[0:mo, 0:N], lhsT=onesf[dp:dp + 1, 0:mo], rhs=rd[dp:dp + 1, 0:N],
                                                        start=True, stop=True), reads=['rd', 'onesf'], writes=['ps4'])
                        op('scalar', lambda e: e.copy(out=osl[lo:hi, 0:N], in_=PS(ob)[lo:hi, 0:N]),
                           reads=['ps%d' % ob], writes=['osb%d' % (hc % 2)])
                        op('vector', lambda e: e.tensor_tensor(out=dst, in0=osl[lo:hi, 0:N], in1=PS(4)[lo:hi, 0:N], op=ALU.mult),
                           reads=['osb%d' % (hc % 2), 'ps4'], writes=['OT'])
                    defer(1, norm1)
                    defer(2, norm2)

    def init_vaug(VE, VO, nE, nO):
        op('gpsimd', lambda e: e.memset(VE, 1.0), writes=['VE'])
        op('gpsimd', lambda e: e.memset(VO, 0.0), writes=['VO'])
        op('gpsimd', lambda e: e.memset(VO[:, :, :, 0:1], 1.0), writes=['VO'])

    def initial_load():
        for tt in range(NT):
            r = tt % 2
            xs = V(Z0 + r * 4 * KB, [128, DM], F32)
            xb = V(Z0 + 8 * KB + r * 2 * KB, [128, DM], BF16)
            op('sync', fm(lambda e, xs, tt: e.dma_start(out=xs, in_=x_in[tt * 128:(tt + 1) * 128, :]), xs, tt),
               writes=['xs%d' % r], dma_sem='xs%d' % r)
            op('scalar', fm(lambda e, xs, xb: e.copy(out=xb, in_=xs), xs, xb), reads=['xs%d' % r], writes=['xb%d' % r])
            emit_xT(tt, xb, 'xb%d' % r, 7)

    OT = V(Z0, [128, 8, SEQ], BF16)
    c64 = V(Z0 + 32 * KB, [128, SEQ], F32)
    s64 = V(Z0 + 40 * KB, [128, SEQ], F32)
    tcaus = V(Z0 + 48 * KB, [128, SEQ], BF16)
    tdil = V(Z0 + 52 * KB, [128, SEQ], BF16)
    eoh = V(Z0 + 56 * KB, [8, 8, 128], BF16)
    gmask = V(Z0 + 58 * KB, [128, 16, 8], F32)
    M0 = Z0 + 59 * KB

    def load_mixer_consts():
        op('sync', lambda e: e.dma_start(out=c64, in_=dr['c64']), writes=['ctab'], dma_sem='c64')
        op('sync', lambda e: e.dma_start(out=s64, in_=dr['s64']), writes=['stab'], dma_sem='s64')
        op('gpsimd', lambda e: e.dma_start(out=tcaus, in_=dr['tcaus']), writes=['tcaus'], dma_sem='tcaus')
        op('gpsimd', lambda e: e.dma_start(out=tdil, in_=dr['tdil']), writes=['tdil'], dma_sem='tdil')
        op('gpsimd', lambda e: e.dma_start(out=eoh, in_=dr['eoh'].rearrange("p (a b) -> p a b", a=8)), writes=['eoh'], dma_sem='eoh')
        op('sync', lambda e: e.dma_start(out=gmask, in_=dr['gmask'].rearrange("p (a b) -> p a b", a=16)), writes=['gmask'], dma_sem='gmask')

    def dsa_phase(L):
        win = dr["win%d" % L]
        wsw = dr["wsw%d" % L]
        kaT = V(M0, [128, SEQ], BF16)
        ikT = V(M0 + 4 * KB, [128, SEQ], BF16)
        qaT = V(M0 + 8 * KB, [128, 4, SEQ], BF16)
        iqT = V(M0 + 24 * KB, [128, 3, SEQ], BF16)
        VaE = V(M0 + 36 * KB, [128, 16, 1, 65], BF16)
        VaO = V(M0 + 36 * KB + 2560, [128, 16, 1, 128], BF16)
        wtok = V(M0 + 36 * KB + 2560 + 4 * KB, [128, 16, 8], F32)
        W0 = M0 + 44 * KB
        c32 = V(W0, [128, SEQ], F32)
        s32 = V(W0 + 8 * KB, [128, SEQ], F32)
        WB = W0 + 16 * KB
        op('sync', lambda e: e.dma_start(out=c32, in_=dr['c32']), writes=['ctab32'], dma_sem='c32')
        op('sync', lambda e: e.dma_start(out=s32, in_=dr['s32']), writes=['stab32'], dma_sem='s32')
        proj_fm(win, wsw, [(0, 896, 32), (32, 896, 32), (64, 896, 32)], 96, True,
                lambda tc: ikT[0:96, tc * 512:(tc + 1) * 512], 'ikT', WB, c32, s32)
        for j in range(3):
            nc_ = 96 if j < 2 else 64
            proj_fm(win, wsw, [(0, 640 + 96 * j, nc_)], nc_, True,
                    fm(lambda tc, j, nc_: iqT[0:nc_, j, tc * 512:(tc + 1) * 512], j, nc_), 'iqT', WB, c32, s32)
        proj_tm(win, 928, 8, lambda tt, ps, bank: op(
            'scalar', lambda e: e.mul(out=wtok[:, tt, :], in_=ps, mul=1.0 / 16.0), reads=['ps%d' % bank], writes=['wtok']), WB)
        proj_fm(win, wsw, [(0, 512, 64), (64, 512, 64)], 128, True,
                lambda tc: kaT[:, tc * 512:(tc + 1) * 512], 'kaT', WB, c64, s64)
        for j in range(4):
            proj_fm(win, wsw, [(0, 128 * j, 128)], 128, True,
                    fm(lambda tc, j: qaT[:, j, tc * 512:(tc + 1) * 512], j), 'qaT', WB, c64, s64)
        if stop < 1:
            return
        init_vaug(VaE, VaO, 1, 1)

        def evac_va(tt, ps, bank):
            op('scalar', lambda e: e.copy(out=VaE[:, tt, 0, 0:64], in_=ps), reads=['ps%d' % bank], writes=['VE'])
            op('vector', lambda e: e.tensor_copy(out=VaO[:, tt, 0, 64:128], in_=ps), reads=['ps%d' % bank], writes=['VO'])
        proj_tm(win, 576, 64, evac_va, WB)
        S.barrier()
        if stop < 2:
            return
        sc = [V(W0 + i * 8 * KB, [128, SEQ], F32) for i in range(2)]
        rl = [V(W0 + 16 * KB + i * 2 * KB, [128, 512], F32) for i in range(2)]
        ng = V(W0 + 20 * KB, [128, SEQ], BF16)
        negT = V(W0 + 24 * KB, [128, 16, 512], BF16)
        WKB = W0 + 40 * KB
        rlc = [0]
        junk = [V(WKB + 9 * KB + i * 4 * KB, [128, SEQ], BF16) for i in range(2)]
        ntau = V(41 * KB + 320, [128, 2], F32)
        cnt = V(41 * KB + 336, [128, 2], F32)
        dlt = V(41 * KB + 352, [128, 2], F32)
        tsel = V(41 * KB + 368, [128, 2], F32)
        RNG = 4096.0
        NIT = 32

        def score_tile(qt):
            scb = sc[qt % 2]
            scr = 'sc%d' % (qt % 2)
            NK = 128 * (qt + 1)
            nkc = (NK + 511) // 512
            for kc in range(nkc):
                wdt = min(512, NK - kc * 512)
                for h in range(8):
                    bank = 5 + rlc[0] % 2
                    rb = rlc[0] % 2
                    rlc[0] += 1
                    pbase = 32 * (h % 3)

                    def mm(e, h=h, kc=kc, wdt=wdt, bank=bank, pbase=pbase, qt=qt):
                        return e.matmul(PS(bank)[:, 0:wdt], lhsT=iqT[pbase:pbase + 32, h // 3, qt * 128:(qt + 1) * 128],
                                        rhs=ikT[pbase:pbase + 32, kc * 512:kc * 512 + wdt], start=True, stop=True)
                    op('tensor', mm, reads=['iqT', 'ikT'], writes=['ps%d' % bank])
                    op('scalar', fm(lambda e, rb, bank, wdt: e.activation(out=rl[rb][:, 0:wdt], in_=PS(bank)[:, 0:wdt],
                                                                          func=AF.Relu), rb, bank, wdt),
                       reads=['ps%d' % bank], writes=['rl%d' % rb])
                    dstv = scb[:, kc * 512:kc * 512 + wdt]
                    if h == 0:
                        op('vector', fm(lambda e, dstv, rb, wdt, qt, h: e.tensor_scalar(
                            out=dstv, in0=rl[rb][:, 0:wdt], scalar1=wtok[:, qt, h:h + 1], scalar2=None, op0=ALU.mult),
                            dstv, rb, wdt, qt, h), reads=['rl%d' % rb, 'wtok'], writes=[scr])
                    else:
                        op('vector', fm(lambda e, dstv, rb, wdt, qt, h: e.scalar_tensor_tensor(
                            out=dstv, in0=rl[rb][:, 0:wdt], scalar=wtok[:, qt, h:h + 1], in1=dstv, op0=ALU.mult, op1=ALU.add),
                            dstv, rb, wdt, qt, h), reads=['rl%d' % rb, 'wtok', scr], writes=[scr])
            dg = scb[:, qt * 128:(qt + 1) * 128]
            op('gpsimd', fm(lambda e, dg: e.affine_select(out=dg, in_=dg, pattern=[[-1, 128]], compare_op=ALU.is_ge,
                                                          fill=-3e30, base=0, channel_multiplier=1), dg),
               reads=[scr], writes=[scr])

        def bisect_tiles(qts):
            qts = [q for q in qts if q >= 2]
            for qt in qts:
                j = qt % 2
                op('vector', fm(lambda e, j: e.memset(ntau[:, j:j + 1], 0.0), j), writes=['ntau%d' % j])
            for it in range(NIT):
                step = RNG / float(2 ** (it + 1))
                for qt in qts:
                    j = qt % 2
                    NK = 128 * (qt + 1)
                    scb = sc[j]
                    op('scalar', fm(lambda e, j, NK, scb: e.activation(out=junk[j][:, 0:NK], in_=scb[:, 0:NK], func=AF.Sign,
                                                                       bias=ntau[:, j:j + 1], scale=1.0,
                                                                       accum_out=cnt[:, j:j + 1]), j, NK, scb),
                       reads=['sc%d' % j, 'ntau%d' % j], writes=['junk%d' % j, 'cnt%d' % j])
                    op('vector', fm(lambda e, j, NK, step: e.tensor_scalar(out=dlt[:, j:j + 1], in0=cnt[:, j:j + 1],
                                                                           scalar1=float(512 - NK), scalar2=-2.0 * step,
                                                                           op0=ALU.is_ge, op1=ALU.mult), j, NK, step),
                       reads=['cnt%d' % j], writes=['dlt%d' % j])
                    op('vector', fm(lambda e, j, step: e.scalar_tensor_tensor(out=ntau[:, j:j + 1], in0=dlt[:, j:j + 1],
                                                                              scalar=step, in1=ntau[:, j:j + 1],
                                                                              op0=ALU.add, op1=ALU.add), j, step),
                       reads=['dlt%d' % j, 'ntau%d' % j], writes=['ntau%d' % j])
            last = RNG / float(2 ** NIT)
            for qt in qts:
                j = qt % 2
                op('vector', fm(lambda e, j: e.tensor_scalar(out=tsel[:, j:j + 1], in0=ntau[:, j:j + 1], scalar1=-1.0,
                                                             scalar2=-last, op0=ALU.mult, op1=ALU.add), j),
                   reads=['ntau%d' % j], writes=['tsel%d' % j])

        def mask_tile(qt):
            j = qt % 2
            scb = sc[j]
            scr = 'sc%d' % j
            NK = 128 * (qt + 1)
            if qt >= 2:
                op('vector', fm(lambda e, scb, NK, j: e.tensor_scalar(out=ng[:, 0:NK], in0=scb[:, 0:NK], scalar1=tsel[:, j:j + 1],
                                                                      scalar2=NEG, op0=ALU.is_lt, op1=ALU.mult), scb, NK, j),
                   reads=[scr, 'tsel%d' % j], writes=['ng'])
            else:
                op('vector', fm(lambda e, scb, NK: e.tensor_scalar(out=ng[:, 0:NK], in0=scb[:, 0:NK], scalar1=-1e29,
                                                                   scalar2=NEG, op0=ALU.is_lt, op1=ALU.mult), scb, NK),
                   reads=[scr], writes=['ng'])
            qcol = (qt % 4) * 128
            for k0 in range(0, qt + 1, 4):
                k1 = min(qt + 1, k0 + 4)

                def tr(e, k0=k0, k1=k1):
                    pv = PSB(7)
                    for kt in range(k0, k1):
                        i = e.transpose(out=pv[:, (kt - k0) * 128:(kt - k0 + 1) * 128], in_=ng[:, kt * 128:(kt + 1) * 128],
                                        identity=identb)
                    return i
                op('tensor', tr, reads=['ng', 'identb'], writes=['ps7'])
                op('vector', fm(lambda e, k0, k1, qcol: e.tensor_copy(
                    out=negT[:, k0:k1, qcol:qcol + 128],
                    in_=PSB(7)[:, 0:(k1 - k0) * 128].rearrange("p (a b) -> p a b", a=k1 - k0)), k0, k1, qcol),
                   reads=['ps7'], writes=['negT'])

        for qp in range(0, NT, 2):
            score_tile(qp)
            score_tile(qp + 1)
            bisect_tiles([qp, qp + 1])
            mask_tile(qp)
            mask_tile(qp + 1)
            if qp % 4 == 2:
                q0 = (qp // 4) * 512
                for h in range(8):
                    par = h % 2
                    pb_ = 64 * par
                    attn_head(
                        [(q0, 512)],
                        lambda kt, pb_=pb_: kaT[pb_:pb_ + 64, kt * 128:(kt + 1) * 128],
                        lambda qs, n, pb_=pb_, h=h: qaT[pb_:pb_ + 64, h // 2, qs:qs + n],
                        lambda kt, qs, n, q0_: (identb, negT[:, kt, qs - q0_:qs - q0_ + n]),
                        (lambda kt: VaE[:, kt, 0, :]) if par == 0 else (lambda kt: VaO[:, kt, 0, :]),
                        par,
                        lambda q0_, N, pb_=pb_, h=h: OT[pb_:pb_ + 64, h // 2, q0_:q0_ + N],
                        ['kaT', 'qaT', 'negT', 'VE', 'VO', 'identb'], WKB)
                flush_all()
        S.barrier()

    def pair_phase(L, kind, npairs, qoff, koff, voff, otc0):
        win = dr["win%d" % L]
        wsw = dr["wsw%d" % L]
        qT = V(M0, [128, SEQ], BF16)
        kT = V(M0 + 4 * KB, [128, SEQ], BF16)
        VE = V(M0 + 8 * KB, [128, 16, 1, 65], BF16)
        VO = V(M0 + 8 * KB + 2560, [128, 16, 1, 128], BF16)
        G0 = M0 + 15 * KB
        kmf = V(G0, [128, 8], F32)
        kmb = V(G0 + 64, [128, 8], BF16)
        gm = V(G0 + 256, [128, 16, 2, 8], F32)
        nsel = V(G0 + 256 + KB, [128, 16, 2, 8], BF16)
        nselT = V(G0 + 2 * KB, [8, 2, SEQ], BF16)
        m8p = V(G0 + 10 * KB, [128, 8], F32)
        WB = G0 + 11 * KB
        WKB = WB + 16 * KB
        init_vaug(VE, VO, 1, 1)
        for j in range(npairs):
            if stop < 3.015:
                continue
            proj_fm(win, wsw, [(0, qoff + 128 * j, 128)], 128, True, lambda tc: qT[:, tc * 512:(tc + 1) * 512], 'qT', WB, c64, s64)
            if stop < 3.025:
                continue
            proj_fm(win, wsw, [(0, koff + 128 * j, 128)], 128, True, lambda tc: kT[:, tc * 512:(tc + 1) * 512], 'kT', WB, c64, s64)
            if stop < 3.035:
                continue

            def evac_v(tt, ps, bank):
                op('scalar', lambda e: e.copy(out=VE[:, tt, 0, 0:64], in_=ps[:, 0:64]), reads=['ps%d' % bank], writes=['VE'])
                op('scalar', lambda e: e.copy(out=VO[:, tt, 0, 64:128], in_=ps[:, 64:128]), reads=['ps%d' % bank], writes=['VO'])
            proj_tm(win, voff + 128 * j, 128, evac_v, WB)
            if stop < 3.1:
                continue
            if kind == 'moba':
                op('vector', lambda e: e.tensor_reduce(out=kmf, in_=kT.rearrange("p (n k) -> p n k", n=8), axis=AX.X, op=ALU.add),
                   reads=['kT'], writes=['kmf'])
                op('vector', lambda e: e.tensor_scalar(out=kmb, in0=kmf, scalar1=1.0 / 256.0, scalar2=None, op0=ALU.mult),
                   reads=['kmf'], writes=['kmb'])

                def gmm(e):
                    for qt in range(NT):
                        for hh in range(2):
                            i = e.matmul(PS(5)[:, (qt * 2 + hh) * 8:(qt * 2 + hh) * 8 + 8],
                                         lhsT=qT[64 * hh:64 * hh + 64, qt * 128:(qt + 1) * 128],
                                         rhs=kmb[64 * hh:64 * hh + 64, :], start=True, stop=True)
                    return i
                op('tensor', gmm, reads=['qT', 'kmb'], writes=['ps5'])
                for hh in range(2):
                    op('vector', fm(lambda e, hh: e.tensor_tensor(
                        out=gm[:, :, hh, :], in0=PS(5)[:, 0:256].rearrange("p (a b c) -> p a b c", a=16, b=2)[:, :, hh, :],
                        in1=gmask, op=ALU.add), hh), reads=['ps5', 'gmask'], writes=['gm'])
                for qt in range(NT if stop >= 3.2 else 0):
                    for hh in range(2):
                        op('vector', fm(lambda e, qt, hh: e.max(out=m8p, in_=gm[:, qt, hh, :]), qt, hh), reads=['gm'], writes=['m8p'])
                        op('vector', fm(lambda e, qt, hh: e.tensor_scalar(out=nsel[:, qt, hh, :], in0=gm[:, qt, hh, :],
                                                                          scalar1=m8p[:, 2:3], scalar2=NEG, op0=ALU.is_lt,
                                                                          op1=ALU.mult), qt, hh),
                           reads=['gm', 'm8p'], writes=['nsel'])
                for q4 in range(4 if stop >= 3.3 else 0):
                    def trs(e, q4=q4):
                        pv = PSB(7)
                        for hh in range(2):
                            for t4 in range(4):
                                qt = q4 * 4 + t4
                                i = e.transpose(out=pv[0:8, hh * 512 + t4 * 128:hh * 512 + (t4 + 1) * 128],
                                                in_=nsel[:, qt, hh, :], identity=identb)
                        return i
                    op('tensor', trs, reads=['nsel', 'identb'], writes=['ps7'])
                    op('vector', fm(lambda e, q4: e.tensor_copy(
                        out=nselT[:, :, q4 * 512:(q4 + 1) * 512],
                        in_=PSB(7)[0:8, :].rearrange("p (a b) -> p a b", a=2)), q4), reads=['ps7'], writes=['nselT'])
            for hh in range(2 if stop >= 3.4 else 0):
                pb_ = 64 * hh
                if kind == 'moba':
                    chunks = [(256 * b, 256) for b in range(8)]

                    def bias_fn(kt, qs, n, q0_, hh=hh):
                        b = q0_ // 256
                        if kt // 2 < b:
                            return eoh[:, kt // 2, :], nselT[:, hh, qs:qs + n]
                        return identb, tcaus[:, qs - 128 * kt:qs - 128 * kt + n]
                    rds = ['kT', 'qT', 'nselT', 'VE', 'VO', 'identb', 'tcaus', 'eoh']
                else:
                    chunks = [(512 * c, 512) for c in range(4)]

                    def bias_fn(kt, qs, n, q0_):
                        return identb, tdil[:, qs - 128 * kt:qs - 128 * kt + n]
                    rds = ['kT', 'qT', 'VE', 'VO', 'identb', 'tdil']
                attn_head(chunks,
                          lambda kt, pb_=pb_: kT[pb_:pb_ + 64, kt * 128:(kt + 1) * 128],
                          lambda qs, n, pb_=pb_: qT[pb_:pb_ + 64, qs:qs + n],
                          bias_fn,
                          (lambda kt: VE[:, kt, 0, :]) if hh == 0 else (lambda kt: VO[:, kt, 0, :]),
                          hh,
                          lambda q0_, N, pb_=pb_, j=j: OT[pb_:pb_ + 64, otc0 + j, q0_:q0_ + N],
                          rds, WKB)
            flush_all()
        S.barrier()

    ACC = V(140 * KB, [128, NT, DM], F32)

    XTOK = V(76 * KB, [128, NT, DM], BF16)

    def outproj_phase(L, resid_src):
        sparse = SPARSE_MOE and (L % 2 == 1)
        wo = V(108 * KB, [128, 8, DM], BF16) if sparse else V(Z0 + 32 * KB, [128, 8, DM], BF16)
        wout = dr["wout%d" % L]
        for hf in range(2):
            op('gpsimd', fm(lambda e, hf: e.dma_start(out=wo[:, :, hf * 512:(hf + 1) * 512],
                                                      in_=wout[:, hf * 512:(hf + 1) * 512].rearrange("(c p) n -> p c n", p=128)), hf),
               writes=['wo'], dma_sem='wo')
        load_ln_params(L, 1)
        for tt in range(NT):
            r = tt % 2
            xr = V(Z0 + 48 * KB + r * 4 * KB, [128, DM], F32)
            z = V(Z0 + 56 * KB + r * 4 * KB, [128, DM], F32)
            xb = V(Z0 + 64 * KB + r * 2 * KB, [128, DM], BF16)
            if sparse:
                xr = V(124 * KB + r * 4 * KB, [128, DM], F32)
                z = V(132 * KB + r * 4 * KB, [128, DM], F32)
                xb = XTOK[:, tt, :]
            op('sync', fm(lambda e, xr, tt: e.dma_start(out=xr, in_=resid_src[tt * 128:(tt + 1) * 128, :]), xr, tt),
               writes=['xr%d' % r], dma_sem='xr%d' % r)
            for hf in range(2):
                bank = 2 * r + hf

                def mm(e, tt=tt, hf=hf, bank=bank):
                    for c in range(8):
                        i = e.matmul(PS(bank), lhsT=OT[:, c, tt * 128:(tt + 1) * 128], rhs=wo[:, c, hf * 512:(hf + 1) * 512],
                                     start=(c == 0), stop=(c == 7))
                    return i
                op('tensor', mm, reads=['OT', 'wo'], writes=['ps%d' % bank])
                op('vector', fm(lambda e, z, xr, hf, bank: e.scalar_tensor_tensor(
                    out=z[:, hf * 512:(hf + 1) * 512], in0=xr[:, hf * 512:(hf + 1) * 512], scalar=ALPHA, in1=PS(bank),
                    op0=ALU.mult, op1=ALU.add), z, xr, hf, bank), reads=['xr%d' % r, 'ps%d' % bank], writes=['z%d' % r])
            emit_ln(z, 'z%d' % r, z, 'z%d' % r, xb, 'xb%d' % r, r)
            op('scalar', fm(lambda e, tt, z: e.mul(out=ACC[:, tt, :], in_=z, mul=ALPHA), tt, z), reads=['z%d' % r], writes=['acc%d_0' % tt, 'acc%d_1' % tt])
            emit_xT(tt, xb, 'xb%d' % r, 7)
        S.barrier()

    def ffn_phase(L, is_last):
        moe = (L % 2 == 1)
        nexp = NEXP if moe else 1
        nchunks = (DFFE if moe else DFF) // 128
        groups = []
        c0 = 0
        while c0 < nchunks:
            g = min(4, nchunks - c0)
            groups.append((c0, g))
            c0 += g
        hT = [V(Z0 + 48 * KB + i * 4 * KB, [128, 4, 512], BF16) for i in range(3)]
        sil = [V(Z0 + 60 * KB + i * KB, [128, 512], BF16) for i in range(2)]
        G0 = Z0 + 62 * KB
        rw = V(G0, [128, 8, 8], BF16)
        lg = V(G0 + 256, [128, 16, 8], F32)
        m8a = V(G0 + 256 + 512, [128, 16, 8], F32)
        gates = V(G0 + 256 + 1024, [128, 16, 8], F32)
        e2 = V(G0 + 256 + 1536, [128, 16, 1], F32)
        p1 = V(G0 + 256 + 1536 + 64, [128, 16, 1], F32)
        p2 = V(G0 + 256 + 1536 + 128, [128, 16, 1], F32)
        tmpg = V(G0 + 256 + 1536 + 192, [128, 16, 8], F32)
        if moe:
            op('gpsimd', lambda e: e.dma_start(out=rw, in_=dr["rt%d" % L].rearrange("(c p) n -> p c n", p=128)),
               writes=['rw'], dma_sem='rw')

            def lmm(e):
                for tt in range(NT):
                    for c in range(8):
                        i = e.matmul(PS(6)[:, tt * 8:(tt + 1) * 8], lhsT=xT[:, c, tt * 128:(tt + 1) * 128], rhs=rw[:, c, :],
                                     start=(c == 0), stop=(c == 7))
                return i
            op('tensor', lmm, reads=['xT', 'rw'], writes=['ps6'])
            op('vector', lambda e: e.tensor_copy(out=lg, in_=PS(6)[:, 0:128].rearrange("p (a b) -> p a b", a=16)),
               reads=['ps6'], writes=['lg'])
            for tt in range(NT):
                op('vector', fm(lambda e, tt: e.max(out=m8a[:, tt, :], in_=lg[:, tt, :]), tt), reads=['lg'], writes=['m8a'])
            op('vector', lambda e: e.tensor_tensor(out=e2, in0=m8a[:, :, 1:2], in1=m8a[:, :, 0:1], op=ALU.subtract),
               reads=['m8a'], writes=['e2'])
            op('scalar', lambda e: e.activation(out=e2, in_=e2, func=AF.Exp), reads=['e2'], writes=['e2'])
            op('vector', lambda e: e.tensor_scalar(out=p1, in0=e2, scalar1=1.0, scalar2=None, op0=ALU.add), reads=['e2'], writes=['p1'])
            op('vector', lambda e: e.reciprocal(out=p1, in_=p1), reads=['p1'], writes=['p1'])
            op('vector', lambda e: e.tensor_tensor(out=p2, in0=e2, in1=p1, op=ALU.mult), reads=['e2', 'p1'], writes=['p2'])
            op('vector', lambda e: e.tensor_tensor(out=gates, in0=lg, in1=m8a[:, :, 0:1].to_broadcast([128, 16, 8]), op=ALU.is_equal),
               reads=['lg', 'm8a'], writes=['gates'])
            op('vector', lambda e: e.tensor_tensor(out=gates, in0=gates, in1=p1.to_broadcast([128, 16, 8]), op=ALU.mult),
               reads=['gates', 'p1'], writes=['gates'])
            op('vector', lambda e: e.tensor_tensor(out=tmpg, in0=lg, in1=m8a[:, :, 1:2].to_broadcast([128, 16, 8]), op=ALU.is_equal),
               reads=['lg', 'm8a'], writes=['tmpg'])
            op('vector', lambda e: e.tensor_tensor(out=tmpg, in0=tmpg, in1=p2.to_broadcast([128, 16, 8]), op=ALU.mult),
               reads=['tmpg', 'p2'], writes=['tmpg'])
            op('vector', lambda e: e.tensor_tensor(out=gates, in0=gates, in1=tmpg, op=ALU.add), reads=['gates', 'tmpg'], writes=['gates'])

        units = []
        for ex in range(nexp):
            for (c0, g) in groups:
                for tc in range(4):
                    units.append((ex, c0, g, tc))
        wsl = [0]
        state = {}

        def load_w(ex, c0, g):
            s_ = wsl[0] % 2
            wsl[0] += 1
            base = Z0 + s_ * 24 * KB
            w1s = V(base, [128, 8, 512], BF16)
            w3s = V(base + 8 * KB, [128, 8, 512], BF16)
            w2s = V(base + 16 * KB, [128, 4, DM], BF16)
            if moe:
                a1 = dr["w1_%d" % L][ex]; a3 = dr["w3_%d" % L][ex]; a2 = dr["w2_%d" % L][ex]
            else:
                a1 = dr["w1_%d" % L]; a3 = dr["w3_%d" % L]; a2 = dr["w2_%d" % L]
            cs = slice(c0 * 128, (c0 + g) * 128)
            op('gpsimd', lambda e: e.dma_start(out=w1s[:, :, 0:g * 128], in_=a1[:, cs].rearrange("(c p) n -> p c n", p=128)),
               writes=['fw%d' % s_], dma_sem='fw%d' % s_)
            op('gpsimd', lambda e: e.dma_start(out=w3s[:, :, 0:g * 128], in_=a3[:, cs].rearrange("(c p) n -> p c n", p=128)),
               writes=['fw%d' % s_], dma_sem='fw%d' % s_)
            op('gpsimd', lambda e: e.dma_start(out=w2s[:, 0:g, :], in_=a2[cs, :].rearrange("(j p) n -> p j n", p=128)),
               writes=['fw%d' % s_], dma_sem='fw%d' % s_)
            return (s_, w1s, w3s, w2s)

        def partA(ui):
            ex, c0, g, tc = units[ui]
            if (ex, c0) not in state:
                state[(ex, c0)] = load_w(ex, c0, g)
            s_, w1s, w3s, w2s = state[(ex, c0)]
            hb = ui % 3
            tok = slice(tc * 512, (tc + 1) * 512)
            for j in range(g):
                pa = j % 2

                def mm(e, w_, bank, j=j):
                    for k in range(8):
                        i = e.matmul(PS(bank), lhsT=w_[:, k, j * 128:(j + 1) * 128], rhs=xT[:, k, tok], start=(k == 0), stop=(k == 7))
                    return i
                op('tensor', fm(mm, w1s, pa), reads=['fw%d' % s_, 'xT'], writes=['ps%d' % pa])
                op('tensor', fm(mm, w3s, 2 + pa), reads=['fw%d' % s_, 'xT'], writes=['ps%d' % (2 + pa)])
                op('scalar', fm(lambda e, pa: e.activation(out=sil[pa], in_=PS(pa), func=AF.Silu), pa),
                   reads=['ps%d' % pa], writes=['sil%d' % pa])
                op('vector', fm(lambda e, pa, j, hb: e.tensor_tensor(out=hT[hb][:, j, :], in0=sil[pa], in1=PS(2 + pa), op=ALU.mult),
                                pa, j, hb), reads=['sil%d' % pa, 'ps%d' % (2 + pa)], writes=['hT%d' % hb])

        ycount = [0]

        def partB(ui):
            ex, c0, g, tc = units[ui]
            s_, w1s, w3s, w2s = state[(ex, c0)]
            hb = ui % 3
            for t4 in range(4):
                tt = tc * 4 + t4
                for hf in range(2):
                    bank = 4 + ycount[0] % 2
                    ycount[0] += 1

                    def mm(e, t4=t4, hf=hf, bank=bank):
                        for j in range(g):
                            i = e.matmul(PS(bank), lhsT=hT[hb][:, j, t4 * 128:(t4 + 1) * 128], rhs=w2s[:, j, hf * 512:(hf + 1) * 512],
                                         start=(j == 0), stop=(j == g - 1))
                        return i
                    op('tensor', mm, reads=['hT%d' % hb, 'fw%d' % s_], writes=['ps%d' % bank])
                    dst = ACC[:, tt, hf * 512:(hf + 1) * 512]
                    sc_ = gates[:, tt, ex:ex + 1] if moe else 1.0
                    op('vector', fm(lambda e, dst, bank, sc_: e.scalar_tensor_tensor(out=dst, in0=PS(bank), scalar=sc_, in1=dst,
                                                                                     op0=ALU.mult, op1=ALU.add), dst, bank, sc_),
                       reads=['ps%d' % bank, 'acc%d_%d' % (tt, hf), 'gates'], writes=['acc%d_%d' % (tt, hf)])

        for ui in range(len(units)):
            partA(ui)
            if ui >= 1:
                partB(ui - 1)
        partB(len(units) - 1)
        load_ln_params(L, 2)
        for tt in range(NT):
            r = tt % 2
            xn = V(Z0 + 66 * KB + r * 4 * KB, [128, DM], F32)
            xb = V(Z0 + 74 * KB + r * 2 * KB, [128, DM], BF16)
            z = ACC[:, tt, :]
            emit_ln(z, 'acc%d_0' % tt, xn, 'xn%d' % r, None if is_last else xb, 'xb%d' % r, r, z_res2='acc%d_1' % tt)
            dstd = y_out if is_last else xsp
            op('sync', fm(lambda e, xn, tt, dstd: e.dma_start(out=dstd[tt * 128:(tt + 1) * 128, :], in_=xn), xn, tt, dstd),
               reads=['xn%d' % r], writes=['xout'], dma_sem='xo%d' % r)
            if not is_last:
                emit_xT(tt, xb, 'xb%d' % r, 7)
        S.barrier()

    def moe_sparse_phase(L, is_last):
        C = MOE_C
        NST = C // 128
        CCH = [(0, 512), (512, C - 512)]
        SEL = V(0, [128, NT, C], BF16)
        SELT = V(0, [128, NST, SEQ], BF16)
        YBF = V(20 * KB, [128, NST, DM], BF16)
        XG = V(108 * KB, [128, 8, C], BF16)
        YACC = V(118 * KB, [128, NST, DM], F32)
        sil = [V(30 * KB, [128, C], BF16), V(138 * KB, [128, C], BF16)]
        hT = [V(66 * KB + i * 3 * KB, [128, 2, C], BF16) for i in range(3)]
        G0 = 32 * KB
        rw = V(G0, [128, 8, 8], BF16)
        lg = V(G0 + 256, [128, 16, 8], F32)
        m8a = V(G0 + 768, [128, 16, 8], F32)
        gates = V(G0 + 1280, [128, 16, 8], F32)
        tmpg = V(G0 + 1792, [128, 16, 8], F32)
        e2 = V(G0 + 2304, [128, 16, 1], F32)
        p1 = V(G0 + 2368, [128, 16, 1], F32)
        p2 = V(G0 + 2432, [128, 16, 1], F32)
        ind = V(G0 + 2560, [128, 16, 8], BF16)
        posm = V(G0 + 2816, [128, 16, 8], F32)
        iotap = V(G0 + 3328, [128, 8], F32)
        tris = V(G0 + 3392, [128, 128], BF16)
        onesb = V(G0 + 3648, [128, 128], BF16)
        iotar = V(G0 + 4096, [128, C], F32)
        op('gpsimd', lambda e: e.dma_start(out=rw, in_=dr["rt%d" % L].rearrange("(c p) n -> p c n", p=128)),
           writes=['rw'], dma_sem='rw')
        op('gpsimd', lambda e: e.dma_start(out=tris, in_=dr['tris']), writes=['tris'], dma_sem='tris')
        op('sync', lambda e: e.dma_start(out=iotap, in_=dr['iotap']), writes=['iotap'], dma_sem='iotap')
        op('sync', lambda e: e.dma_start(out=iotar, in_=dr['iotar']), writes=['iotar'], dma_sem='iotar')
        op('vector', lambda e: e.memset(onesb, 1.0), writes=['onesb'])

        def lmm(e):
            for tt in range(NT):
                for c in range(8):
                    i = e.matmul(PS(6)[:, tt * 8:(tt + 1) * 8], lhsT=xT[:, c, tt * 128:(tt + 1) * 128], rhs=rw[:, c, :],
                                 start=(c == 0), stop=(c == 7))
            return i
        op('tensor', lmm, reads=['xT', 'rw'], writes=['ps6'])
        op('vector', lambda e: e.tensor_copy(out=lg, in_=PS(6)[:, 0:128].rearrange("p (a b) -> p a b", a=16)),
           reads=['ps6'], writes=['lg'])
        for tt in range(NT):
            op('vector', fm(lambda e, tt: e.max(out=m8a[:, tt, :], in_=lg[:, tt, :]), tt), reads=['lg'], writes=['m8a'])
        op('vector', lambda e: e.tensor_tensor(out=e2, in0=m8a[:, :, 1:2], in1=m8a[:, :, 0:1], op=ALU.subtract),
           reads=['m8a'], writes=['e2'])
        op('scalar', lambda e: e.activation(out=e2, in_=e2, func=AF.Exp), reads=['e2'], writes=['e2'])
        op('vector', lambda e: e.tensor_scalar(out=p1, in0=e2, scalar1=1.0, scalar2=None, op0=ALU.add), reads=['e2'], writes=['p1'])
        op('vector', lambda e: e.reciprocal(out=p1, in_=p1), reads=['p1'], writes=['p1'])
        op('vector', lambda e: e.tensor_tensor(out=p2, in0=e2, in1=p1, op=ALU.mult), reads=['e2', 'p1'], writes=['p2'])
        op('vector', lambda e: e.tensor_tensor(out=gates, in0=lg, in1=m8a[:, :, 0:1].to_broadcast([128, 16, 8]), op=ALU.is_equal),
           reads=['lg', 'm8a'], writes=['gates'])
        op('vector', lambda e: e.tensor_tensor(out=gates, in0=gates, in1=p1.to_broadcast([128, 16, 8]), op=ALU.mult),
           reads=['gates', 'p1'], writes=['gates'])
        op('vector', lambda e: e.tensor_tensor(out=tmpg, in0=lg, in1=m8a[:, :, 1:2].to_broadcast([128, 16, 8]), op=ALU.is_equal),
           reads=['lg', 'm8a'], writes=['tmpg'])
        op('vector', lambda e: e.tensor_tensor(out=tmpg, in0=tmpg, in1=p2.to_broadcast([128, 16, 8]), op=ALU.mult),
           reads=['tmpg', 'p2'], writes=['tmpg'])
        op('vector', lambda e: e.tensor_tensor(out=gates, in0=gates, in1=tmpg, op=ALU.add), reads=['gates', 'tmpg'], writes=['gates'])
        op('vector', lambda e: e.tensor_scalar(out=ind, in0=gates, scalar1=0.0, scalar2=None, op0=ALU.is_gt),
           reads=['gates'], writes=['ind'])

        def pmm(e):
            for tt in range(NT):
                for tp in range(tt + 1):
                    i = e.matmul(PS(6)[:, tt * 8:(tt + 1) * 8], lhsT=(onesb if tp < tt else tris), rhs=ind[:, tp, :],
                                 start=(tp == 0), stop=(tp == tt))
            return i
        op('tensor', pmm, reads=['ind', 'onesb', 'tris'], writes=['ps6'])
        op('vector', lambda e: e.scalar_tensor_tensor(out=posm, in0=PS(6)[:, 0:128].rearrange("p (a b) -> p a b", a=16), scalar=1.0,
                                                      in1=ind, op0=ALU.add, op1=ALU.mult), reads=['ps6', 'ind'], writes=['posm'])
        op('vector', lambda e: e.tensor_scalar(out=posm, in0=posm, scalar1=-1.0, scalar2=None, op0=ALU.add),
           reads=['posm'], writes=['posm'])
        S.barrier()

        nchunks = DFFE // 128
        groups = [(c0, 2) for c0 in range(0, nchunks, 2)]
        a1_ = dr["w1_%d" % L]
        a3_ = dr["w3_%d" % L]
        a2_ = dr["w2_%d" % L]
        wsl = [0]
        bk = [0]
        ycount = [0]

        def load_w(ex, c0, g):
            s_ = wsl[0] % 2
            wsl[0] += 1
            base = Z0 + s_ * 12 * KB
            w1s = V(base, [128, 8, 256], BF16)
            w3s = V(base + 4 * KB, [128, 8, 256], BF16)
            w2s = V(base + 8 * KB, [128, 2, DM], BF16)
            cs = slice(c0 * 128, (c0 + g) * 128)
            op('gpsimd', lambda e: e.dma_start(out=w1s, in_=a1_[ex][:, cs].rearrange("(c p) n -> p c n", p=128)),
               writes=['fw%d' % s_], dma_sem='fw%d' % s_)
            op('gpsimd', lambda e: e.dma_start(out=w3s, in_=a3_[ex][:, cs].rearrange("(c p) n -> p c n", p=128)),
               writes=['fw%d' % s_], dma_sem='fw%d' % s_)
            op('gpsimd', lambda e: e.dma_start(out=w2s, in_=a2_[ex][cs, :].rearrange("(j p) n -> p j n", p=128)),
               writes=['fw%d' % s_], dma_sem='fw%d' % s_)
            return (s_, w1s, w3s, w2s)

        for ex in range(NEXP):
            for tt in range(NT):
                eng = 'vector' if tt % 2 == 0 else 'gpsimd'
                op(eng, fm(lambda e, tt, ex: e.tensor_scalar(out=SEL[:, tt, :], in0=iotar, scalar1=posm[:, tt, ex:ex + 1],
                                                            scalar2=None, op0=ALU.is_equal), tt, ex),
                   reads=['iotar', 'posm'], writes=['sel%d' % tt])
            for fc in range(8):
                ba = bk[0] % 2
                bk[0] += 1

                def gmm(e, fc=fc, ba=ba):
                    for tt in range(NT):
                        e.matmul(PS(ba), lhsT=XTOK[:, tt, fc * 128:(fc + 1) * 128], rhs=SEL[:, tt, 0:512],
                                 start=(tt == 0), stop=(tt == NT - 1))
                    for tt in range(NT):
                        i = e.matmul(PS(2 + ba)[:, 0:C - 512], lhsT=XTOK[:, tt, fc * 128:(fc + 1) * 128], rhs=SEL[:, tt, 512:C],
                                     start=(tt == 0), stop=(tt == NT - 1))
                    return i
                op('tensor', gmm, reads=['xtok'] + ['sel%d' % t for t in range(NT)], writes=['ps%d' % ba, 'ps%d' % (2 + ba)])
                op('scalar', fm(lambda e, fc, ba: e.copy(out=XG[:, fc, 0:512], in_=PS(ba)), fc, ba),
                   reads=['ps%d' % ba], writes=['xg'])
                op('scalar', fm(lambda e, fc, ba: e.copy(out=XG[:, fc, 512:C], in_=PS(2 + ba)[:, 0:C - 512]), fc, ba),
                   reads=['ps%d' % (2 + ba)], writes=['xg'])
            wstate = {}

            def partA(gi):
                c0, g = groups[gi]
                wstate[gi] = load_w(ex, c0, g)
                s_, w1s, w3s, w2s = wstate[gi]
                hb = gi % 3
                for (cs0, cn) in CCH:
                    for j in range(g):
                        pa = bk[0] % 2
                        bk[0] += 1

                        def mm(e, w_, bank, j=j, cs0=cs0, cn=cn):
                            for k in range(8):
                                i = e.matmul(PS(bank)[:, 0:cn], lhsT=w_[:, k, j * 128:(j + 1) * 128], rhs=XG[:, k, cs0:cs0 + cn],
                                             start=(k == 0), stop=(k == 7))
                            return i
                        op('tensor', fm(mm, w1s, pa), reads=['fw%d' % s_, 'xg'], writes=['ps%d' % pa])
                        op('tensor', fm(mm, w3s, 2 + pa), reads=['fw%d' % s_, 'xg'], writes=['ps%d' % (2 + pa)])
                        op('scalar', fm(lambda e, pa, cn: e.activation(out=sil[pa][:, 0:cn], in_=PS(pa)[:, 0:cn], func=AF.Silu), pa, cn),
                           reads=['ps%d' % pa], writes=['sil%d' % pa])
                        op('vector', fm(lambda e, pa, j, hb, cs0, cn: e.tensor_tensor(
                            out=hT[hb][:, j, cs0:cs0 + cn], in0=sil[pa][:, 0:cn], in1=PS(2 + pa)[:, 0:cn], op=ALU.mult),
                            pa, j, hb, cs0, cn), reads=['sil%d' % pa, 'ps%d' % (2 + pa)], writes=['hT%d' % hb])

            def partB(gi):
                c0, g = groups[gi]
                s_, w1s, w3s, w2s = wstate[gi]
                hb = gi % 3
                for st_ in range(NST):
                    for hf in range(2):
                        bank = 4 + ycount[0] % 2
                        ycount[0] += 1

                        def mm(e, st_=st_, hf=hf, bank=bank):
                            for j in range(g):
                                i = e.matmul(PS(bank), lhsT=hT[hb][:, j, st_ * 128:(st_ + 1) * 128],
                                             rhs=w2s[:, j, hf * 512:(hf + 1) * 512], start=(j == 0), stop=(j == g - 1))
                            return i
                        op('tensor', mm, reads=['hT%d' % hb, 'fw%d' % s_], writes=['ps%d' % bank])
                        dst = YACC[:, st_, hf * 512:(hf + 1) * 512]
                        rn = 'yacc%d_%d' % (st_, hf)
                        if gi == 0:
                            op('vector', fm(lambda e, dst, bank: e.tensor_copy(out=dst, in_=PS(bank)), dst, bank),
                               reads=['ps%d' % bank], writes=[rn])
                        else:
                            op('vector', fm(lambda e, dst, bank: e.tensor_tensor(out=dst, in0=dst, in1=PS(bank), op=ALU.add), dst, bank),
                               reads=['ps%d' % bank, rn], writes=[rn])

            for gi in range(len(groups)):
                partA(gi)
                if gi >= 1:
                    partB(gi - 1)
            partB(len(groups) - 1)
            for st_ in range(NST):
                eng = 'scalar' if st_ % 2 == 0 else 'gpsimd'
                if eng == 'scalar':
                    op(eng, fm(lambda e, st_: e.copy(out=YBF[:, st_, :], in_=YACC[:, st_, :]), st_),
                       reads=['yacc%d_0' % st_, 'yacc%d_1' % st_], writes=['ybf'])
                else:
                    op(eng, fm(lambda e, st_: e.tensor_copy(out=YBF[:, st_, :], in_=YACC[:, st_, :]), st_),
                       reads=['yacc%d_0' % st_, 'yacc%d_1' % st_], writes=['ybf'])
            for tc in range(4):
                qb = 6 + tc % 2

                def bmm(e, tc=tc, qb=qb, ex=ex):
                    for t4 in range(4):
                        tt = tc * 4 + t4
                        for tp in range(tt + 1):
                            i = e.matmul(PS(qb)[:, t4 * 128:(t4 + 1) * 128],
                                         lhsT=ind[:, tp, ex:ex + 1].to_broadcast([128, 128]),
                                         rhs=(onesb if tp < tt else tris), start=(tp == 0), stop=(tp == tt))
                    return i
                op('tensor', bmm, reads=['ind', 'onesb', 'tris'] + ['sel%d' % t for t in range(NT)], writes=['ps%d' % qb])
                for st_ in range(NST):
                    op('vector', fm(lambda e, tc, qb, st_: e.tensor_scalar(out=SELT[:, st_, tc * 512:(tc + 1) * 512], in0=PS(qb),
                                                                           scalar1=iotap[:, st_:st_ + 1], scalar2=None,
                                                                           op0=ALU.is_equal), tc, qb, st_),
                       reads=['ps%d' % qb, 'iotap'], writes=['selt'])
            for tt in range(NT):
                for hf in range(2):
                    bank = 4 + ycount[0] % 2
                    ycount[0] += 1

                    def smm(e, tt=tt, hf=hf, bank=bank):
                        for st_ in range(NST):
                            i = e.matmul(PS(bank), lhsT=SELT[:, st_, tt * 128:(tt + 1) * 128], rhs=YBF[:, st_, hf * 512:(hf + 1) * 512],
                                         start=(st_ == 0), stop=(st_ == NST - 1))
                        return i
                    op('tensor', smm, reads=['selt', 'ybf'] + ['sel%d' % t for t in range(NT)], writes=['ps%d' % bank])
                    dst = ACC[:, tt, hf * 512:(hf + 1) * 512]
                    op('vector', fm(lambda e, dst, bank, tt, ex: e.scalar_tensor_tensor(
                        out=dst, in0=PS(bank), scalar=gates[:, tt, ex:ex + 1], in1=dst, op0=ALU.mult, op1=ALU.add), dst, bank, tt, ex),
                       reads=['ps%d' % bank, 'acc%d_%d' % (tt, hf), 'gates'], writes=['acc%d_%d' % (tt, hf)])
        S.barrier()
        load_ln_params(L, 2)
        for tt in range(NT):
            r = tt % 2
            xn = V(Z0 + 66 * KB + r * 4 * KB, [128, DM], F32)
            xb = V(Z0 + 74 * KB + r * 2 * KB, [128, DM], BF16)
            z = ACC[:, tt, :]
            emit_ln(z, 'acc%d_0' % tt, xn, 'xn%d' % r, None if is_last else xb, 'xb%d' % r, r, z_res2='acc%d_1' % tt)
            dstd = y_out if is_last else xsp
            op('sync', fm(lambda e, xn, tt, dstd: e.dma_start(out=dstd[tt * 128:(tt + 1) * 128, :], in_=xn), xn, tt, dstd),
               reads=['xn%d' % r], writes=['xout'], dma_sem='xo%d' % r)
            if not is_last:
                emit_xT(tt, xb, 'xb%d' % r, 7)
        S.barrier()

    initial_load()
    S.barrier()
    resid = x_in
    for li, L in enumerate(layer_ids):
        load_mixer_consts()
        if L % 2 == 0:
            if not (3.0 < stop <= 3.05):
                dsa_phase(L)
            if stop < 3:
                break
            pair_phase(L, 'moba', 4, 936, 1448, 1960, 4)
        else:
            pair_phase(L, 'dil', 8, 0, 1024, 2048, 0)
        if stop < 4:
            break
        outproj_phase(L, resid)
        if stop < 5:
            break
        if SPARSE_MOE and L % 2 == 1:
            moe_sparse_phase(L, li == len(layer_ids) - 1)
        else:
            ffn_phase(L, li == len(layer_ids) - 1)
        resid = xsp
    S.barrier()
    S.emit(nc)
    st.close()
    return nc


_CACHE = {}


def _layer_inputs(L, inp):
    i = L // 2
    d = {}
    if L % 2 == 0:
        w = np.ascontiguousarray(inp['even_w_in'][i])
        d["win%d" % L] = w
        sw = w.copy()
        sw = swap_halves(sw, 0, 8, 64)
        sw = swap_halves(sw, 512, 1, 64)
        sw = swap_halves(sw, 640, 8, 32)
        sw = swap_halves(sw, 896, 1, 32)
        sw = swap_halves(sw, 936, 8, 64)
        sw = swap_halves(sw, 1448, 8, 64)
        d["wsw%d" % L] = sw
        d["wout%d" % L] = np.ascontiguousarray(inp['even_w_out'][i])
        d["w1_%d" % L] = np.ascontiguousarray(inp['even_w1'][i])
        d["w3_%d" % L] = np.ascontiguousarray(inp['even_w3'][i])
        d["w2_%d" % L] = np.ascontiguousarray(inp['even_w2'][i])
        pre = 'even'
    else:
        w = np.ascontiguousarray(inp['odd_w_in'][i])
        d["win%d" % L] = w
        sw = w.copy()
        sw = swap_halves(sw, 0, 16, 64)
        sw = swap_halves(sw, 1024, 16, 64)
        d["wsw%d" % L] = sw
        d["wout%d" % L] = np.ascontiguousarray(inp['odd_w_out'][i])
        d["rt%d" % L] = np.ascontiguousarray(inp['odd_router'][i])
        d["w1_%d" % L] = np.ascontiguousarray(inp['odd_w1'][i])
        d["w3_%d" % L] = np.ascontiguousarray(inp['odd_w3'][i])
        d["w2_%d" % L] = np.ascontiguousarray(inp['odd_w2'][i])
        pre = 'odd'
    d["g1_%d" % L] = np.ascontiguousarray(inp[pre + '_ln1_g'][i]).reshape(1, DM)
    d["b1_%d" % L] = np.ascontiguousarray(inp[pre + '_ln1_b'][i]).reshape(1, DM)
    d["g2_%d" % L] = np.ascontiguousarray(inp[pre + '_ln2_g'][i]).reshape(1, DM)
    d["b2_%d" % L] = np.ascontiguousarray(inp[pre + '_ln2_b'][i]).reshape(1, DM)
    return d


def swap_fix(w):
    return w


def run_layers(x, layer_ids, inp):
    key = tuple(layer_ids)
    if key not in _CACHE:
        _CACHE[key] = build(list(layer_ids))
    nc = _CACHE[key]
    shared = dict(make_consts())
    for L in layer_ids:
        shared.update(_layer_inputs(L, inp))
    in_maps = []
    for b in range(8):
        m = dict(shared)
        m["x"] = np.ascontiguousarray(x[b])
        in_maps.append(m)
    res = run_bass_kernel_spmd(nc, in_maps, core_ids=list(range(8)))
    return np.stack([np.asarray(r["y"]) for r in res.results], axis=0).astype(np.float32)


LAUNCH_GROUPS = [[0, 1, 2, 3]]


def kernel(**inputs):
    inp = {k: np.asarray(v) for k, v in inputs.items()}
    x = np.asarray(inp['x'], dtype=np.float32)
    for grp in LAUNCH_GROUPS:
        x = run_layers(x, grp, inp)
    return x
```

```python
import numpy as np
import concourse.bass as bass
import concourse.mybir as mybir
from concourse.bass_utils import run_bass_kernel_spmd
from contextlib import ExitStack

F32 = mybir.dt.float32
BF16 = mybir.dt.bfloat16
U8 = mybir.dt.uint8
ALU = mybir.AluOpType
AF = mybir.ActivationFunctionType
AX = mybir.AxisListType

SEQ = 2048
DM = 1024
NT = 16
DEPTH = 4
ALPHA = float((2 * DEPTH) ** 0.25)
EPS = 1e-5
NEG = -30000.0
DFF = 2816
DFFE = 3584
NEXP = 8
KB = 1024
MOE_C = 640
SPARSE_MOE = True

ENGS = ['tensor', 'vector', 'scalar', 'gpsimd', 'sync']


class _Op(object):
    __slots__ = ('eng', 'fn', 'waits', 'dma_sem', 'idx', 'signal', 'count', 'dma_count')


class Sched(object):
    def __init__(self):
        self.ops = dict((e, []) for e in ENGS)
        self.last_write = {}
        self.readers = {}
        self.waited = dict((e, {}) for e in ENGS)
        self.dma_counts = {}
        self.dma_last = {}
        self.last_compute = {}

    def _dep(self, o, d, kind):
        if d is None or d is o:
            return
        if d.dma_sem is None:
            if d.eng == o.eng and o.dma_sem is None:
                if o.eng == 'tensor':
                    return
            key = d.eng
            val = d.idx
        else:
            key = ('dma', d.dma_sem)
            val = d.dma_count
        w = self.waited[o.eng]
        if w.get(key, -1) >= val:
            return
        w[key] = val
        d.signal = True
        o.waits.append(d)

    def op(self, eng, fn, reads=(), writes=(), dma_sem=None):
        o = _Op()
        o.eng = eng
        o.fn = fn
        o.waits = []
        o.dma_sem = dma_sem
        o.signal = False
        o.idx = len(self.ops[eng])
        if dma_sem is not None:
            c = self.dma_counts.get(dma_sem, 0) + 1
            self.dma_counts[dma_sem] = c
            o.dma_count = c
            self.dma_last[dma_sem] = o
        elif fn is not None:
            self.last_compute[eng] = o
        writes = list(writes) + [r for r in reads if r.startswith('ps') and r not in writes]
        reads = [r for r in reads if not r.startswith('ps')]
        for r in reads:
            self._dep(o, self.last_write.get(r), 'raw')
        for w_ in writes:
            self._dep(o, self.last_write.get(w_), 'waw')
            for rd in self.readers.get(w_, ()):
                self._dep(o, rd, 'war')
        for r in reads:
            self.readers.setdefault(r, []).append(o)
        for w_ in writes:
            self.last_write[w_] = o
            self.readers[w_] = []
        self.ops[eng].append(o)
        return o

    def barrier(self):
        lasts = list(self.last_compute.values()) + list(self.dma_last.values())
        for e in ENGS:
            o = self.op(e, None)
            for d in lasts:
                if d.dma_sem is None and d.eng == e and e in ('tensor', 'sync'):
                    continue
                self._dep(o, d, 'raw')
        self.last_write = {}
        self.readers = {}

    def emit(self, nc):
        dma_names = sorted(self.dma_counts.keys(), key=str)
        with ExitStack() as st:
            esem = {}
            for e in ENGS:
                esem[e] = st.enter_context(nc.semaphore("s_" + e))
            dsem = {}
            for i, n in enumerate(dma_names):
                dsem[n] = st.enter_context(nc.semaphore("d%d" % i))
            for e in ENGS:
                c = 0
                for o in self.ops[e]:
                    if o.dma_sem is None and o.signal:
                        assert o.fn is not None
                        c += 1
                        o.count = c
            block = st.enter_context(nc.Block())

            def mk(e):
                def body(eng):
                    for o in self.ops[e]:
                        for d in o.waits:
                            if d.dma_sem is None:
                                eng.wait_ge(esem[d.eng], d.count)
                            else:
                                eng.wait_ge(dsem[d.dma_sem], 16 * d.dma_count)
                        if o.fn is None:
                            continue
                        inst = o.fn(eng)
                        if o.dma_sem is not None:
                            inst.then_inc(dsem[o.dma_sem], 16)
                        elif o.signal:
                            inst.then_inc(esem[e], 1)
                return body

            for e in ENGS:
                if self.ops[e]:
                    getattr(block, e)(mk(e))


def _rope_tab(dim):
    inv = (np.float32(10000.0) ** (-np.arange(0, dim, 2, dtype=np.float32) / np.float32(dim))).astype(np.float32)
    ang = (np.arange(SEQ, dtype=np.float32)[:, None] * inv[None, :]).astype(np.float32)
    return np.cos(ang).astype(np.float32), np.sin(ang).astype(np.float32)


def make_consts():
    c = {}
    c['ident'] = np.eye(128, dtype=np.float32)
    cos64, sin64 = _rope_tab(64)
    cos32, sin32 = _rope_tab(32)
    p = np.arange(128)
    c['c64'] = np.ascontiguousarray(cos64.T[p % 32, :])
    sg = np.where((p % 64) < 32, -1.0, 1.0).astype(np.float32)[:, None]
    c['s64'] = np.ascontiguousarray(sin64.T[p % 32, :] * sg)
    c['c32'] = np.ascontiguousarray(cos32.T[p % 16, :])
    sg = np.where((p % 32) < 16, -1.0, 1.0).astype(np.float32)[:, None]
    c['s32'] = np.ascontiguousarray(sin32.T[p % 16, :] * sg)
    d = np.arange(SEQ)[None, :] - np.arange(128)[:, None]
    c['tcaus'] = np.where(d >= 0, 0.0, NEG).astype(np.float32)
    mult = ((d >= 0) & (d <= 128)).astype(np.int32) + ((d >= 0) & (d <= 512) & (d % 4 == 0)).astype(np.int32) \
        + ((d >= 0) & (d % 16 == 0)).astype(np.int32)
    lut = np.array([NEG, 0.0, 8.0 * np.log(2.0), 8.0 * np.log(3.0)], dtype=np.float32)
    c['tdil'] = lut[mult].astype(np.float32)
    e = np.zeros((8, 8, 128), np.float32)
    for n in range(8):
        e[n, n, :] = 1.0
    c['eoh'] = e.reshape(8, 1024)
    gm = np.zeros((128, 16, 8), np.float32)
    for qt in range(16):
        gm[:, qt, (qt // 2):] = -1e30
    c['gmask'] = gm.reshape(128, 128)
    tp = np.arange(128)
    c['tris'] = (tp[:, None] < tp[None, :]).astype(np.float32)
    c['iotar'] = np.tile(np.arange(MOE_C, dtype=np.float32)[None, :], (128, 1))
    c['iotap'] = (tp[:, None] + 128.0 * np.arange(8)[None, :]).astype(np.float32)
    return c


def swap_halves(w, lo, nheads, hd):
    out = w
    for h in range(nheads):
        a = lo + h * hd
        first = w[..., a:a + hd // 2].copy()
        out[..., a:a + hd // 2] = w[..., a + hd // 2:a + hd]
        out[..., a + hd // 2:a + hd] = first
    return out


def build(layer_ids, stop=99):
    nc = bass.Bass("TRN2", target_bir_lowering=False)
    S = Sched()
    op = S.op
    dr = {}

    def din(name, shape):
        dr[name] = nc.dram_tensor(name, list(shape), F32, kind="ExternalInput").ap()
        return dr[name]

    x_in = din("x", [SEQ, DM])
    for n, shp in (('ident', [128, 128]), ('c64', [128, SEQ]), ('s64', [128, SEQ]), ('c32', [128, SEQ]),
                   ('s32', [128, SEQ]), ('tcaus', [128, SEQ]), ('tdil', [128, SEQ]), ('eoh', [8, 1024]),
                   ('gmask', [128, 128]), ('tris', [128, 128]), ('iotar', [128, MOE_C]), ('iotap', [128, 8])):
        din(n, shp)
    for L in layer_ids:
        if L % 2 == 0:
            din("win%d" % L, [DM, 2472]); din("wsw%d" % L, [DM, 2472]); din("wout%d" % L, [DM, DM])
            din("w1_%d" % L, [DM, DFF]); din("w3_%d" % L, [DM, DFF]); din("w2_%d" % L, [DFF, DM])
        else:
            din("win%d" % L, [DM, 3072]); din("wsw%d" % L, [DM, 3072]); din("wout%d" % L, [DM, DM])
            din("rt%d" % L, [DM, NEXP])
            din("w1_%d" % L, [NEXP, DM, DFFE]); din("w3_%d" % L, [NEXP, DM, DFFE]); din("w2_%d" % L, [NEXP, DFFE, DM])
        for n in ('g1', 'b1', 'g2', 'b2'):
            din("%s_%d" % (n, L), [1, DM])
    y_out = nc.dram_tensor("y", [SEQ, DM], F32, kind="ExternalOutput").ap()
    xsp = nc.dram_tensor("xsp", [SEQ, DM], F32, kind="Internal").ap()

    st = ExitStack()
    big = st.enter_context(nc.sbuf_tensor("big", [128, 204 * KB], U8))
    P = st.enter_context(nc.psum_tensor("P", [128, 8, 512], F32))

    def V(off, shape, dt):
        esz = 4 if dt == F32 else 2
        n = 1
        for s_ in shape[1:]:
            n *= s_
        v = big[0:shape[0], off:off + n * esz].bitcast(dt)
        if len(shape) == 3:
            v = v.rearrange("p (a b) -> p a b", a=shape[1])
        elif len(shape) == 4:
            v = v.rearrange("p (a b c) -> p a b c", a=shape[1], b=shape[2])
        return v

    def PS(i):
        return P[:, i, :]

    def PSB(i):
        return P[:, i, :].bitcast(BF16)

    xT = V(0, [128, 8, SEQ], BF16)
    lnG = V(32 * KB, [128, DM], F32)
    lnB = V(36 * KB, [128, DM], F32)
    identb = V(40 * KB, [128, 128], BF16)
    onesf = V(40 * KB + 256, [128, 128], F32)
    stats = V(41 * KB, [128, 2, 2, 6], F32)
    mv = V(41 * KB + 128, [128, 2, 2], F32)
    rstd = V(41 * KB + 192, [128, 2, 1], F32)
    m8 = V(41 * KB + 256, [128, 8], F32)
    Z0 = 42 * KB

    def fm(f, *a):
        return lambda e: f(e, *a)

    op('gpsimd', lambda e: e.dma_start(out=identb, in_=dr['ident']), writes=['identb'], dma_sem='c0')
    op('vector', lambda e: e.memset(onesf, 1.0), writes=['onesf'])

    def emit_xT(tt, xb, xb_res, bank):
        def tr(e):
            pv = PSB(bank)
            for c in range(8):
                i = e.transpose(out=pv[:, c * 128:(c + 1) * 128], in_=xb[:, c * 128:(c + 1) * 128], identity=identb)
            return i
        op('tensor', tr, reads=[xb_res, 'identb'], writes=['ps%d' % bank])
        op('vector', lambda e: e.tensor_copy(out=xT[:, :, tt * 128:(tt + 1) * 128],
                                             in_=PSB(bank).rearrange("p (a b) -> p a b", a=8)),
           reads=['ps%d' % bank], writes=['xT'])

    def emit_ln(z, z_res, xn, xn_res, xb, xb_res, r, z_res2=None):
        z_res2 = z_res2 or z_res
        st_ = stats[:, r]
        op('vector', lambda e: e.bn_stats(out=st_[:, 0, :], in_=z[:, 0:512]), reads=[z_res], writes=['stats%d' % r])
        op('vector', lambda e: e.bn_stats(out=st_[:, 1, :], in_=z[:, 512:1024]), reads=[z_res2], writes=['stats%db' % r])
        op('vector', lambda e: e.bn_aggr(out=mv[:, r, :], in_=st_.rearrange("p a b -> p (a b)")),
           reads=['stats%d' % r, 'stats%db' % r], writes=['mv%d' % r])
        op('vector', lambda e: e.tensor_scalar(out=rstd[:, r, :], in0=mv[:, r, 1:2], scalar1=EPS, scalar2=None,
                                               op0=ALU.add), reads=['mv%d' % r], writes=['rstd%d' % r])
        op('scalar', lambda e: e.activation(out=rstd[:, r, :], in_=rstd[:, r, :], func=AF.Sqrt),
           reads=['rstd%d' % r], writes=['rstd%d' % r])
        op('vector', lambda e: e.reciprocal(out=rstd[:, r, :], in_=rstd[:, r, :]), reads=['rstd%d' % r], writes=['rstd%d' % r])
        op('vector', lambda e: e.tensor_scalar(out=xn, in0=z, scalar1=mv[:, r, 0:1], scalar2=rstd[:, r, :],
                                               op0=ALU.subtract, op1=ALU.mult),
           reads=[z_res, z_res2, 'mv%d' % r, 'rstd%d' % r], writes=[xn_res])
        op('gpsimd', lambda e: e.tensor_tensor(out=xn, in0=xn, in1=lnG, op=ALU.mult), reads=[xn_res, 'lnG'], writes=[xn_res])
        op('gpsimd', lambda e: e.tensor_tensor(out=xn, in0=xn, in1=lnB, op=ALU.add), reads=[xn_res, 'lnB'], writes=[xn_res])
        if xb is not None:
            op('scalar', lambda e: e.copy(out=xb, in_=xn), reads=[xn_res], writes=[xb_res])

    def load_ln_params(L, which):
        g = dr["g%d_%d" % (which, L)]
        b = dr["b%d_%d" % (which, L)]
        op('sync', lambda e: e.dma_start(out=lnG, in_=g.to_broadcast([128, DM])), writes=['lnG'], dma_sem='lng')
        op('sync', lambda e: e.dma_start(out=lnB, in_=b.to_broadcast([128, DM])), writes=['lnB'], dma_sem='lnb')

    wcount = [0]

    def proj_fm(wsrc, wswsrc, col_specs, ncols, rope, dst_fn, dst_res, WB, Ct=None, St=None):
        s_ = wcount[0] % 2
        wcount[0] += 1
        wt = V(WB + s_ * 4 * KB, [128, 8, 128], BF16)
        ws = V(WB + s_ * 4 * KB + 2 * KB, [128, 8, 128], BF16)
        for (doff, slo, n) in col_specs:
            op('gpsimd', fm(lambda e, doff, slo, n: e.dma_start(
                out=wt[:, :, doff:doff + n], in_=wsrc[:, slo:slo + n].rearrange("(c p) n -> p c n", p=128)), doff, slo, n),
               writes=['wt%d' % s_], dma_sem='wt%d' % s_)
            if rope:
                op('gpsimd', fm(lambda e, doff, slo, n: e.dma_start(
                    out=ws[:, :, doff:doff + n], in_=wswsrc[:, slo:slo + n].rearrange("(c p) n -> p c n", p=128)), doff, slo, n),
                   writes=['ws%d' % s_], dma_sem='ws%d' % s_)
        for tc in range(4):
            pa = tc % 2
            tok = slice(tc * 512, (tc + 1) * 512)

            def mm(e, w_, bank, tok=tok):
                for k in range(8):
                    i = e.matmul(PS(bank)[0:ncols, :], lhsT=w_[:, k, 0:ncols], rhs=xT[:, k, tok],
                                 start=(k == 0), stop=(k == 7))
                return i
            op('tensor', fm(mm, wt, pa), reads=['wt%d' % s_, 'xT'], writes=['ps%d' % pa])
            dst = dst_fn(tc)
            if rope:
                op('tensor', fm(mm, ws, 2 + pa), reads=['ws%d' % s_, 'xT'], writes=['ps%d' % (2 + pa)])
                t1 = V(WB + 8 * KB + pa * 4 * KB, [128, 512], F32)
                t2 = V(WB + 8 * KB + pa * 4 * KB + 2 * KB, [128, 512], F32)
                op('vector', fm(lambda e, t1, pa, tok: e.tensor_tensor(out=t1[0:ncols, :], in0=PS(pa)[0:ncols, :],
                                                                       in1=Ct[0:ncols, tok], op=ALU.mult), t1, pa, tok),
                   reads=['ps%d' % pa, 'ctab', 'ctab32'], writes=['t1_%d' % pa])
                op('vector', fm(lambda e, t2, pa, tok: e.tensor_tensor(out=t2[0:ncols, :], in0=PS(2 + pa)[0:ncols, :],
                                                                       in1=St[0:ncols, tok], op=ALU.mult), t2, pa, tok),
                   reads=['ps%d' % (2 + pa), 'stab', 'stab32'], writes=['t2_%d' % pa])
                op('gpsimd', fm(lambda e, t1, t2, dst: e.tensor_tensor(out=dst, in0=t1[0:ncols, :], in1=t2[0:ncols, :],
                                                                       op=ALU.add), t1, t2, dst),
                   reads=['t1_%d' % pa, 't2_%d' % pa], writes=[dst_res])
            else:
                op('scalar', fm(lambda e, dst, pa: e.copy(out=dst, in_=PS(pa)[0:ncols, :]), dst, pa),
                   reads=['ps%d' % pa], writes=[dst_res])

    def proj_tm(wsrc, slo, ncols, evac, WB):
        s_ = wcount[0] % 2
        wcount[0] += 1
        wt = V(WB + s_ * 4 * KB, [128, 8, 128], BF16)
        op('gpsimd', lambda e: e.dma_start(out=wt[:, :, 0:ncols],
                                           in_=wsrc[:, slo:slo + ncols].rearrange("(c p) n -> p c n", p=128)),
           writes=['wt%d' % s_], dma_sem='wt%d' % s_)
        for tt in range(NT):
            bank = 4 + tt % 2

            def mm(e, tt=tt, bank=bank):
                for k in range(8):
                    i = e.matmul(PS(bank)[:, 0:ncols], lhsT=xT[:, k, tt * 128:(tt + 1) * 128], rhs=wt[:, k, 0:ncols],
                                 start=(k == 0), stop=(k == 7))
                return i
            op('tensor', mm, reads=['wt%d' % s_, 'xT'], writes=['ps%d' % bank])
            evac(tt, PS(bank)[:, 0:ncols], bank)

    ucount = [0]
    hcount = [0]
    deferred = []

    def defer(delay, fn):
        deferred.append([delay, fn])

    def tick():
        due = []
        for it in deferred:
            it[0] -= 1
        for it in list(deferred):
            if it[0] <= 0:
                due.append(it)
                deferred.remove(it)
        for it in due:
            it[1]()

    def flush_all():
        while deferred:
            it = deferred.pop(0)
            it[1]()

    def attn_head(chunks, kT_fn, qT_fn, bias_fn, V_fn, parity, OTdst_fn, reads, WKB):
        pT = [V(WKB + i * KB, [128, 512], BF16) for i in range(3)]
        rd = V(WKB + 3 * KB, [128, 512], F32)
        osb = [V(WKB + 5 * KB + i * 2 * KB, [128, 512], F32) for i in range(2)]
        M = 65 if parity == 0 else 128
        for (q0, N) in chunks:
            hc = hcount[0]
            hcount[0] += 1
            ob = 2 + hc % 2
            ktmax = (q0 + N - 1) // 128
            for kt in range(ktmax + 1):
                u = ucount[0]
                ucount[0] += 1
                sb = u % 2
                pb = u % 3
                qs = max(q0, kt * 128)
                n = q0 + N - qs
                bl, br = bias_fn(kt, qs, n, q0)

                def mms(e, kt=kt, qs=qs, n=n, sb=sb, bl=bl, br=br):
                    e.matmul(PS(sb)[:, 0:n], lhsT=kT_fn(kt), rhs=qT_fn(qs, n), start=True, stop=False)
                    return e.matmul(PS(sb)[:, 0:n], lhsT=bl, rhs=br, start=False, stop=True)
                op('tensor', mms, reads=list(reads), writes=['ps%d' % sb])
                op('scalar', fm(lambda e, pb, sb, n: e.activation(out=pT[pb][:, 0:n], in_=PS(sb)[:, 0:n], func=AF.Exp,
                                                                  scale=0.125), pb, sb, n),
                   reads=['ps%d' % sb], writes=['pT%d' % pb])

                def mmo(e, kt=kt, qs=qs, n=n, pb=pb, ob=ob, q0=q0, ktmax=ktmax):
                    return e.matmul(PS(ob)[0:M, qs - q0:qs - q0 + n], lhsT=V_fn(kt), rhs=pT[pb][:, 0:n],
                                    start=(kt == 0), stop=(kt == ktmax), skip_group_check=True)
                tick()
                defer(1, lambda mmo=mmo, pb=pb, ob=ob: op('tensor', mmo, reads=['pT%d' % pb] + list(reads),
                                                          writes=['ps%d' % ob]))
                if kt == ktmax:
                    dp = 64 if parity == 0 else 0
                    lo, hi = (0, 64) if parity == 0 else (64, 128)
                    mo = 64 if parity == 0 else 128
                    osl = osb[hc % 2]
                    dst = OTdst_fn(q0, N)

                    def norm1(ob=ob, N=N, dp=dp):
                        op('vector', lambda e: e.reciprocal(out=rd[dp:dp + 1, 0:N], in_=PS(ob)[dp:dp + 1, 0:N]),
                           reads=['ps%d' % ob], writes=['rd'])

                    def norm2(ob=ob, N=N, dp=dp, lo=lo, hi=hi, mo=mo, osl=osl, dst=dst, hc=hc):
                        op('tensor', lambda e: e.matmul(PS(4)[0:mo, 0:N], lhsT=onesf[dp:dp + 1, 0:mo], rhs=rd[dp:dp + 1, 0:N],
                                                        start=True, stop=True), reads=['rd', 'onesf'], writes=['ps4'])
                        op('scalar', lambda e: e.copy(out=osl[lo:hi, 0:N], in_=PS(ob)[lo:hi, 0:N]),
                           reads=['ps%d' % ob], writes=['osb%d' % (hc % 2)])
                        op('vector', lambda e: e.tensor_tensor(out=dst, in0=osl[lo:hi, 0:N], in1=PS(4)[lo:hi, 0:N], op=ALU.mult),
                           reads=['osb%d' % (hc % 2), 'ps4'], writes=['OT'])
                    defer(1, norm1)
                    defer(2, norm2)

    def init_vaug(VE, VO, nE, nO):
        op('gpsimd', lambda e: e.memset(VE, 1.0), writes=['VE'])
        op('gpsimd', lambda e: e.memset(VO, 0.0), writes=['VO'])
        op('gpsimd', lambda e: e.memset(VO[:, :, :, 0:1], 1.0), writes=['VO'])

    def initial_load():
        for tt in range(NT):
            r = tt % 2
            xs = V(Z0 + r * 4 * KB, [128, DM], F32)
            xb = V(Z0 + 8 * KB + r * 2 * KB, [128, DM], BF16)
            op('sync', fm(lambda e, xs, tt: e.dma_start(out=xs, in_=x_in[tt * 128:(tt + 1) * 128, :]), xs, tt),
               writes=['xs%d' % r], dma_sem='xs%d' % r)
            op('scalar', fm(lambda e, xs, xb: e.copy(out=xb, in_=xs), xs, xb), reads=['xs%d' % r], writes=['xb%d' % r])
            emit_xT(tt, xb, 'xb%d' % r, 7)

    OT = V(Z0, [128, 8, SEQ], BF16)
    c64 = V(Z0 + 32 * KB, [128, SEQ], F32)
    s64 = V(Z0 + 40 * KB, [128, SEQ], F32)
    tcaus = V(Z0 + 48 * KB, [128, SEQ], BF16)
    tdil = V(Z0 + 52 * KB, [128, SEQ], BF16)
    eoh = V(Z0 + 56 * KB, [8, 8, 128], BF16)
    gmask = V(Z0 + 58 * KB, [128, 16, 8], F32)
    M0 = Z0 + 59 * KB

    def load_mixer_consts():
        op('sync', lambda e: e.dma_start(out=c64, in_=dr['c64']), writes=['ctab'], dma_sem='c64')
        op('sync', lambda e: e.dma_start(out=s64, in_=dr['s64']), writes=['stab'], dma_sem='s64')
        op('gpsimd', lambda e: e.dma_start(out=tcaus, in_=dr['tcaus']), writes=['tcaus'], dma_sem='tcaus')
        op('gpsimd', lambda e: e.dma_start(out=tdil, in_=dr['tdil']), writes=['tdil'], dma_sem='tdil')
        op('gpsimd', lambda e: e.dma_start(out=eoh, in_=dr['eoh'].rearrange("p (a b) -> p a b", a=8)), writes=['eoh'], dma_sem='eoh')
        op('sync', lambda e: e.dma_start(out=gmask, in_=dr['gmask'].rearrange("p (a b) -> p a b", a=16)), writes=['gmask'], dma_sem='gmask')

    def dsa_phase(L):
        win = dr["win%d" % L]
        wsw = dr["wsw%d" % L]
        kaT = V(M0, [128, SEQ], BF16)
        ikT = V(M0 + 4 * KB, [128, SEQ], BF16)
        qaT = V(M0 + 8 * KB, [128, 4, SEQ], BF16)
        iqT = V(M0 + 24 * KB, [128, 3, SEQ], BF16)
        VaE = V(M0 + 36 * KB, [128, 16, 1, 65], BF16)
        VaO = V(M0 + 36 * KB + 2560, [128, 16, 1, 128], BF16)
        wtok = V(M0 + 36 * KB + 2560 + 4 * KB, [128, 16, 8], F32)
        W0 = M0 + 44 * KB
        c32 = V(W0, [128, SEQ], F32)
        s32 = V(W0 + 8 * KB, [128, SEQ], F32)
        WB = W0 + 16 * KB
        op('sync', lambda e: e.dma_start(out=c32, in_=dr['c32']), writes=['ctab32'], dma_sem='c32')
        op('sync', lambda e: e.dma_start(out=s32, in_=dr['s32']), writes=['stab32'], dma_sem='s32')
        proj_fm(win, wsw, [(0, 896, 32), (32, 896, 32), (64, 896, 32)], 96, True,
                lambda tc: ikT[0:96, tc * 512:(tc + 1) * 512], 'ikT', WB, c32, s32)
        for j in range(3):
            nc_ = 96 if j < 2 else 64
            proj_fm(win, wsw, [(0, 640 + 96 * j, nc_)], nc_, True,
                    fm(lambda tc, j, nc_: iqT[0:nc_, j, tc * 512:(tc + 1) * 512], j, nc_), 'iqT', WB, c32, s32)
        proj_tm(win, 928, 8, lambda tt, ps, bank: op(
            'scalar', lambda e: e.mul(out=wtok[:, tt, :], in_=ps, mul=1.0 / 16.0), reads=['ps%d' % bank], writes=['wtok']), WB)
        proj_fm(win, wsw, [(0, 512, 64), (64, 512, 64)], 128, True,
                lambda tc: kaT[:, tc * 512:(tc + 1) * 512], 'kaT', WB, c64, s64)
        for j in range(4):
            proj_fm(win, wsw, [(0, 128 * j, 128)], 128, True,
                    fm(lambda tc, j: qaT[:, j, tc * 512:(tc + 1) * 512], j), 'qaT', WB, c64, s64)
        if stop < 1:
            return
        init_vaug(VaE, VaO, 1, 1)

        def evac_va(tt, ps, bank):
            op('scalar', lambda e: e.copy(out=VaE[:, tt, 0, 0:64], in_=ps), reads=['ps%d' % bank], writes=['VE'])
            op('vector', lambda e: e.tensor_copy(out=VaO[:, tt, 0, 64:128], in_=ps), reads=['ps%d' % bank], writes=['VO'])
        proj_tm(win, 576, 64, evac_va, WB)
        S.barrier()
        if stop < 2:
            return
        sc = [V(W0 + i * 8 * KB, [128, SEQ], F32) for i in range(2)]
        rl = [V(W0 + 16 * KB + i * 2 * KB, [128, 512], F32) for i in range(2)]
        ng = V(W0 + 20 * KB, [128, SEQ], BF16)
        negT = V(W0 + 24 * KB, [128, 16, 512], BF16)
        WKB = W0 + 40 * KB
        rlc = [0]
        junk = [V(WKB + 9 * KB + i * 4 * KB, [128, SEQ], BF16) for i in range(2)]
        ntau = V(41 * KB + 320, [128, 2], F32)
        cnt = V(41 * KB + 336, [128, 2], F32)
        dlt = V(41 * KB + 352, [128, 2], F32)
        tsel = V(41 * KB + 368, [128, 2], F32)
        RNG = 4096.0
        NIT = 32

        def score_tile(qt):
            scb = sc[qt % 2]
            scr = 'sc%d' % (qt % 2)
            NK = 128 * (qt + 1)
            nkc = (NK + 511) // 512
            for kc in range(nkc):
                wdt = min(512, NK - kc * 512)
                for h in range(8):
                    bank = 5 + rlc[0] % 2
                    rb = rlc[0] % 2
                    rlc[0] += 1
                    pbase = 32 * (h % 3)

                    def mm(e, h=h, kc=kc, wdt=wdt, bank=bank, pbase=pbase, qt=qt):
                        return e.matmul(PS(bank)[:, 0:wdt], lhsT=iqT[pbase:pbase + 32, h // 3, qt * 128:(qt + 1) * 128],
                                        rhs=ikT[pbase:pbase + 32, kc * 512:kc * 512 + wdt], start=True, stop=True)
                    op('tensor', mm, reads=['iqT', 'ikT'], writes=['ps%d' % bank])
                    op('scalar', fm(lambda e, rb, bank, wdt: e.activation(out=rl[rb][:, 0:wdt], in_=PS(bank)[:, 0:wdt],
                                                                          func=AF.Relu), rb, bank, wdt),
                       reads=['ps%d' % bank], writes=['rl%d' % rb])
                    dstv = scb[:, kc * 512:kc * 512 + wdt]
                    if h == 0:
                        op('vector', fm(lambda e, dstv, rb, wdt, qt, h: e.tensor_scalar(
                            out=dstv, in0=rl[rb][:, 0:wdt], scalar1=wtok[:, qt, h:h + 1], scalar2=None, op0=ALU.mult),
                            dstv, rb, wdt, qt, h), reads=['rl%d' % rb, 'wtok'], writes=[scr])
                    else:
                        op('vector', fm(lambda e, dstv, rb, wdt, qt, h: e.scalar_tensor_tensor(
                            out=dstv, in0=rl[rb][:, 0:wdt], scalar=wtok[:, qt, h:h + 1], in1=dstv, op0=ALU.mult, op1=ALU.add),
                            dstv, rb, wdt, qt, h), reads=['rl%d' % rb, 'wtok', scr], writes=[scr])
            dg = scb[:, qt * 128:(qt + 1) * 128]
            op('gpsimd', fm(lambda e, dg: e.affine_select(out=dg, in_=dg, pattern=[[-1, 128]], compare_op=ALU.is_ge,
                                                          fill=-3e30, base=0, channel_multiplier=1), dg),
               reads=[scr], writes=[scr])

        def bisect_tiles(qts):
            qts = [q for q in qts if q >= 2]
            for qt in qts:
                j = qt % 2
                op('vector', fm(lambda e, j: e.memset(ntau[:, j:j + 1], 0.0), j), writes=['ntau%d' % j])
            for it in range(NIT):
                step = RNG / float(2 ** (it + 1))
                for qt in qts:
                    j = qt % 2
                    NK = 128 * (qt + 1)
                    scb = sc[j]
                    op('scalar', fm(lambda e, j, NK, scb: e.activation(out=junk[j][:, 0:NK], in_=scb[:, 0:NK], func=AF.Sign,
                                                                       bias=ntau[:, j:j + 1], scale=1.0,
                                                                       accum_out=cnt[:, j:j + 1]), j, NK, scb),
                       reads=['sc%d' % j, 'ntau%d' % j], writes=['junk%d' % j, 'cnt%d' % j])
                    op('vector', fm(lambda e, j, NK, step: e.tensor_scalar(out=dlt[:, j:j + 1], in0=cnt[:, j:j + 1],
                                                                           scalar1=float(512 - NK), scalar2=-2.0 * step,
                                                                           op0=ALU.is_ge, op1=ALU.mult), j, NK, step),
                       reads=['cnt%d' % j], writes=['dlt%d' % j])
                    op('vector', fm(lambda e, j, step: e.scalar_tensor_tensor(out=ntau[:, j:j + 1], in0=dlt[:, j:j + 1],
                                                                              scalar=step, in1=ntau[:, j:j + 1],
                                                                              op0=ALU.add, op1=ALU.add), j, step),
                       reads=['dlt%d' % j, 'ntau%d' % j], writes=['ntau%d' % j])
            last = RNG / float(2 ** NIT)
            for qt in qts:
                j = qt % 2
                op('vector', fm(lambda e, j: e.tensor_scalar(out=tsel[:, j:j + 1], in0=ntau[:, j:j + 1], scalar1=-1.0,
                                                             scalar2=-last, op0=ALU.mult, op1=ALU.add), j),
                   reads=['ntau%d' % j], writes=['tsel%d' % j])

        def mask_tile(qt):
            j = qt % 2
            scb = sc[j]
            scr = 'sc%d' % j
            NK = 128 * (qt + 1)
            if qt >= 2:
                op('vector', fm(lambda e, scb, NK, j: e.tensor_scalar(out=ng[:, 0:NK], in0=scb[:, 0:NK], scalar1=tsel[:, j:j + 1],
                                                                      scalar2=NEG, op0=ALU.is_lt, op1=ALU.mult), scb, NK, j),
                   reads=[scr, 'tsel%d' % j], writes=['ng'])
            else:
                op('vector', fm(lambda e, scb, NK: e.tensor_scalar(out=ng[:, 0:NK], in0=scb[:, 0:NK], scalar1=-1e29,
                                                                   scalar2=NEG, op0=ALU.is_lt, op1=ALU.mult), scb, NK),
                   reads=[scr], writes=['ng'])
            qcol = (qt % 4) * 128
            for k0 in range(0, qt + 1, 4):
                k1 = min(qt + 1, k0 + 4)

                def tr(e, k0=k0, k1=k1):
                    pv = PSB(7)
                    for kt in range(k0, k1):
                        i = e.transpose(out=pv[:, (kt - k0) * 128:(kt - k0 + 1) * 128], in_=ng[:, kt * 128:(kt + 1) * 128],
                                        identity=identb)
                    return i
                op('tensor', tr, reads=['ng', 'identb'], writes=['ps7'])
                op('vector', fm(lambda e, k0, k1, qcol: e.tensor_copy(
                    out=negT[:, k0:k1, qcol:qcol + 128],
                    in_=PSB(7)[:, 0:(k1 - k0) * 128].rearrange("p (a b) -> p a b", a=k1 - k0)), k0, k1, qcol),
                   reads=['ps7'], writes=['negT'])

        for qp in range(0, NT, 2):
            score_tile(qp)
            score_tile(qp + 1)
            bisect_tiles([qp, qp + 1])
            mask_tile(qp)
            mask_tile(qp + 1)
            if qp % 4 == 2:
                q0 = (qp // 4) * 512
                for h in range(8):
                    par = h % 2
                    pb_ = 64 * par
                    attn_head(
                        [(q0, 512)],
                        lambda kt, pb_=pb_: kaT[pb_:pb_ + 64, kt * 128:(kt + 1) * 128],
                        lambda qs, n, pb_=pb_, h=h: qaT[pb_:pb_ + 64, h // 2, qs:qs + n],
                        lambda kt, qs, n, q0_: (identb, negT[:, kt, qs - q0_:qs - q0_ + n]),
                        (lambda kt: VaE[:, kt, 0, :]) if par == 0 else (lambda kt: VaO[:, kt, 0, :]),
                        par,
                        lambda q0_, N, pb_=pb_, h=h: OT[pb_:pb_ + 64, h // 2, q0_:q0_ + N],
                        ['kaT', 'qaT', 'negT', 'VE', 'VO', 'identb'], WKB)
                flush_all()
        S.barrier()

    def pair_phase(L, kind, npairs, qoff, koff, voff, otc0):
        win = dr["win%d" % L]
        wsw = dr["wsw%d" % L]
        qT = V(M0, [128, SEQ], BF16)
        kT = V(M0 + 4 * KB, [128, SEQ], BF16)
        VE = V(M0 + 8 * KB, [128, 16, 1, 65], BF16)
        VO = V(M0 + 8 * KB + 2560, [128, 16, 1, 128], BF16)
        G0 = M0 + 15 * KB
        kmf = V(G0, [128, 8], F32)
        kmb = V(G0 + 64, [128, 8], BF16)
        gm = V(G0 + 256, [128, 16, 2, 8], F32)
        nsel = V(G0 + 256 + KB, [128, 16, 2, 8], BF16)
        nselT = V(G0 + 2 * KB, [8, 2, SEQ], BF16)
        m8p = V(G0 + 10 * KB, [128, 8], F32)
        WB = G0 + 11 * KB
        WKB = WB + 16 * KB
        init_vaug(VE, VO, 1, 1)
        for j in range(npairs):
            if stop < 3.015:
                continue
            proj_fm(win, wsw, [(0, qoff + 128 * j, 128)], 128, True, lambda tc: qT[:, tc * 512:(tc + 1) * 512], 'qT', WB, c64, s64)
            if stop < 3.025:
                continue
            proj_fm(win, wsw, [(0, koff + 128 * j, 128)], 128, True, lambda tc: kT[:, tc * 512:(tc + 1) * 512], 'kT', WB, c64, s64)
            if stop < 3.035:
                continue

            def evac_v(tt, ps, bank):
                op('scalar', lambda e: e.copy(out=VE[:, tt, 0, 0:64], in_=ps[:, 0:64]), reads=['ps%d' % bank], writes=['VE'])
                op('scalar', lambda e: e.copy(out=VO[:, tt, 0, 64:128], in_=ps[:, 64:128]), reads=['ps%d' % bank], writes=['VO'])
            proj_tm(win, voff + 128 * j, 128, evac_v, WB)
            if stop < 3.1:
                continue
            if kind == 'moba':
                op('vector', lambda e: e.tensor_reduce(out=kmf, in_=kT.rearrange("p (n k) -> p n k", n=8), axis=AX.X, op=ALU.add),
                   reads=['kT'], writes=['kmf'])
                op('vector', lambda e: e.tensor_scalar(out=kmb, in0=kmf, scalar1=1.0 / 256.0, scalar2=None, op0=ALU.mult),
                   reads=['kmf'], writes=['kmb'])

                def gmm(e):
                    for qt in range(NT):
                        for hh in range(2):
                            i = e.matmul(PS(5)[:, (qt * 2 + hh) * 8:(qt * 2 + hh) * 8 + 8],
                                         lhsT=qT[64 * hh:64 * hh + 64, qt * 128:(qt + 1) * 128],
                                         rhs=kmb[64 * hh:64 * hh + 64, :], start=True, stop=True)
                    return i
                op('tensor', gmm, reads=['qT', 'kmb'], writes=['ps5'])
                for hh in range(2):
                    op('vector', fm(lambda e, hh: e.tensor_tensor(
                        out=gm[:, :, hh, :], in0=PS(5)[:, 0:256].rearrange("p (a b c) -> p a b c", a=16, b=2)[:, :, hh, :],
                        in1=gmask, op=ALU.add), hh), reads=['ps5', 'gmask'], writes=['gm'])
                for qt in range(NT if stop >= 3.2 else 0):
                    for hh in range(2):
                        op('vector', fm(lambda e, qt, hh: e.max(out=m8p, in_=gm[:, qt, hh, :]), qt, hh), reads=['gm'], writes=['m8p'])
                        op('vector', fm(lambda e, qt, hh: e.tensor_scalar(out=nsel[:, qt, hh, :], in0=gm[:, qt, hh, :],
                                                                          scalar1=m8p[:, 2:3], scalar2=NEG, op0=ALU.is_lt,
                                                                          op1=ALU.mult), qt, hh),
                           reads=['gm', 'm8p'], writes=['nsel'])
                for q4 in range(4 if stop >= 3.3 else 0):
                    def trs(e, q4=q4):
                        pv = PSB(7)
                        for hh in range(2):
                            for t4 in range(4):
                                qt = q4 * 4 + t4
                                i = e.transpose(out=pv[0:8, hh * 512 + t4 * 128:hh * 512 + (t4 + 1) * 128],
                                                in_=nsel[:, qt, hh, :], identity=identb)
                        return i
                    op('tensor', trs, reads=['nsel', 'identb'], writes=['ps7'])
                    op('vector', fm(lambda e, q4: e.tensor_copy(
                        out=nselT[:, :, q4 * 512:(q4 + 1) * 512],
                        in_=PSB(7)[0:8, :].rearrange("p (a b) -> p a b", a=2)), q4), reads=['ps7'], writes=['nselT'])
            for hh in range(2 if stop >= 3.4 else 0):
                pb_ = 64 * hh
                if kind == 'moba':
                    chunks = [(256 * b, 256) for b in range(8)]

                    def bias_fn(kt, qs, n, q0_, hh=hh):
                        b = q0_ // 256
                        if kt // 2 < b:
                            return eoh[:, kt // 2, :], nselT[:, hh, qs:qs + n]
                        return identb, tcaus[:, qs - 128 * kt:qs - 128 * kt + n]
                    rds = ['kT', 'qT', 'nselT', 'VE', 'VO', 'identb', 'tcaus', 'eoh']
                else:
                    chunks = [(512 * c, 512) for c in range(4)]

                    def bias_fn(kt, qs, n, q0_):
                        return identb, tdil[:, qs - 128 * kt:qs - 128 * kt + n]
                    rds = ['kT', 'qT', 'VE', 'VO', 'identb', 'tdil']
                attn_head(chunks,
                          lambda kt, pb_=pb_: kT[pb_:pb_ + 64, kt * 128:(kt + 1) * 128],
                          lambda qs, n, pb_=pb_: qT[pb_:pb_ + 64, qs:qs + n],
                          bias_fn,
                          (lambda kt: VE[:, kt, 0, :]) if hh == 0 else (lambda kt: VO[:, kt, 0, :]),
                          hh,
                          lambda q0_, N, pb_=pb_, j=j: OT[pb_:pb_ + 64, otc0 + j, q0_:q0_ + N],
                          rds, WKB)
            flush_all()
        S.barrier()

    ACC = V(140 * KB, [128, NT, DM], F32)

    XTOK = V(76 * KB, [128, NT, DM], BF16)

    def outproj_phase(L, resid_src):
        sparse = SPARSE_MOE and (L % 2 == 1)
        wo = V(108 * KB, [128, 8, DM], BF16) if sparse else V(Z0 + 32 * KB, [128, 8, DM], BF16)
        wout = dr["wout%d" % L]
        for hf in range(2):
            op('gpsimd', fm(lambda e, hf: e.dma_start(out=wo[:, :, hf * 512:(hf + 1) * 512],
                                                      in_=wout[:, hf * 512:(hf + 1) * 512].rearrange("(c p) n -> p c n", p=128)), hf),
               writes=['wo'], dma_sem='wo')
        load_ln_params(L, 1)
        for tt in range(NT):
            r = tt % 2
            xr = V(Z0 + 48 * KB + r * 4 * KB, [128, DM], F32)
            z = V(Z0 + 56 * KB + r * 4 * KB, [128, DM], F32)
            xb = V(Z0 + 64 * KB + r * 2 * KB, [128, DM], BF16)
            if sparse:
                xr = V(124 * KB + r * 4 * KB, [128, DM], F32)
                z = V(132 * KB + r * 4 * KB, [128, DM], F32)
                xb = XTOK[:, tt, :]
            op('sync', fm(lambda e, xr, tt: e.dma_start(out=xr, in_=resid_src[tt * 128:(tt + 1) * 128, :]), xr, tt),
               writes=['xr%d' % r], dma_sem='xr%d' % r)
            for hf in range(2):
                bank = 2 * r + hf

                def mm(e, tt=tt, hf=hf, bank=bank):
                    for c in range(8):
                        i = e.matmul(PS(bank), lhsT=OT[:, c, tt * 128:(tt + 1) * 128], rhs=wo[:, c, hf * 512:(hf + 1) * 512],
                                     start=(c == 0), stop=(c == 7))
                    return i
                op('tensor', mm, reads=['OT', 'wo'], writes=['ps%d' % bank])
                op('vector', fm(lambda e, z, xr, hf, bank: e.scalar_tensor_tensor(
                    out=z[:, hf * 512:(hf + 1) * 512], in0=xr[:, hf * 512:(hf + 1) * 512], scalar=ALPHA, in1=PS(bank),
                    op0=ALU.mult, op1=ALU.add), z, xr, hf, bank), reads=['xr%d' % r, 'ps%d' % bank], writes=['z%d' % r])
            emit_ln(z, 'z%d' % r, z, 'z%d' % r, xb, 'xb%d' % r, r)
            op('scalar', fm(lambda e, tt, z: e.mul(out=ACC[:, tt, :], in_=z, mul=ALPHA), tt, z), reads=['z%d' % r], writes=['acc%d_0' % tt, 'acc%d_1' % tt])
            emit_xT(tt, xb, 'xb%d' % r, 7)
        S.barrier()

    def ffn_phase(L, is_last):
        moe = (L % 2 == 1)
        nexp = NEXP if moe else 1
        nchunks = (DFFE if moe else DFF) // 128
        groups = []
        c0 = 0
        while c0 < nchunks:
            g = min(4, nchunks - c0)
            groups.append((c0, g))
            c0 += g
        hT = [V(Z0 + 48 * KB + i * 4 * KB, [128, 4, 512], BF16) for i in range(3)]
        sil = [V(Z0 + 60 * KB + i * KB, [128, 512], BF16) for i in range(2)]
        G0 = Z0 + 62 * KB
        rw = V(G0, [128, 8, 8], BF16)
        lg = V(G0 + 256, [128, 16, 8], F32)
        m8a = V(G0 + 256 + 512, [128, 16, 8], F32)
        gates = V(G0 + 256 + 1024, [128, 16, 8], F32)
        e2 = V(G0 + 256 + 1536, [128, 16, 1], F32)
        p1 = V(G0 + 256 + 1536 + 64, [128, 16, 1], F32)
        p2 = V(G0 + 256 + 1536 + 128, [128, 16, 1], F32)
        tmpg = V(G0 + 256 + 1536 + 192, [128, 16, 8], F32)
        if moe:
            op('gpsimd', lambda e: e.dma_start(out=rw, in_=dr["rt%d" % L].rearrange("(c p) n -> p c n", p=128)),
               writes=['rw'], dma_sem='rw')

            def lmm(e):
                for tt in range(NT):
                    for c in range(8):
                        i = e.matmul(PS(6)[:, tt * 8:(tt + 1) * 8], lhsT=xT[:, c, tt * 128:(tt + 1) * 128], rhs=rw[:, c, :],
                                     start=(c == 0), stop=(c == 7))
                return i
            op('tensor', lmm, reads=['xT', 'rw'], writes=['ps6'])
            op('vector', lambda e: e.tensor_copy(out=lg, in_=PS(6)[:, 0:128].rearrange("p (a b) -> p a b", a=16)),
               reads=['ps6'], writes=['lg'])
            for tt in range(NT):
                op('vector', fm(lambda e, tt: e.max(out=m8a[:, tt, :], in_=lg[:, tt, :]), tt), reads=['lg'], writes=['m8a'])
            op('vector', lambda e: e.tensor_tensor(out=e2, in0=m8a[:, :, 1:2], in1=m8a[:, :, 0:1], op=ALU.subtract),
               reads=['m8a'], writes=['e2'])
            op('scalar', lambda e: e.activation(out=e2, in_=e2, func=AF.Exp), reads=['e2'], writes=['e2'])
            op('vector', lambda e: e.tensor_scalar(out=p1, in0=e2, scalar1=1.0, scalar2=None, op0=ALU.add), reads=['e2'], writes=['p1'])
            op('vector', lambda e: e.reciprocal(out=p1, in_=p1), reads=['p1'], writes=['p1'])
            op('vector', lambda e: e.tensor_tensor(out=p2, in0=e2, in1=p1, op=ALU.mult), reads=['e2', 'p1'], writes=['p2'])
            op('vector', lambda e: e.tensor_tensor(out=gates, in0=lg, in1=m8a[:, :, 0:1].to_broadcast([128, 16, 8]), op=ALU.is_equal),
               reads=['lg', 'm8a'], writes=['gates'])
            op('vector', lambda e: e.tensor_tensor(out=gates, in0=gates, in1=p1.to_broadcast([128, 16, 8]), op=ALU.mult),
               reads=['gates', 'p1'], writes=['gates'])
            op('vector', lambda e: e.tensor_tensor(out=tmpg, in0=lg, in1=m8a[:, :, 1:2].to_broadcast([128, 16, 8]), op=ALU.is_equal),
               reads=['lg', 'm8a'], writes=['tmpg'])
            op('vector', lambda e: e.tensor_tensor(out=tmpg, in0=tmpg, in1=p2.to_broadcast([128, 16, 8]), op=ALU.mult),
               reads=['tmpg', 'p2'], writes=['tmpg'])
            op('vector', lambda e: e.tensor_tensor(out=gates, in0=gates, in1=tmpg, op=ALU.add), reads=['gates', 'tmpg'], writes=['gates'])

        units = []
        for ex in range(nexp):
            for (c0, g) in groups:
                for tc in range(4):
                    units.append((ex, c0, g, tc))
        wsl = [0]
        state = {}

        def load_w(ex, c0, g):
            s_ = wsl[0] % 2
            wsl[0] += 1
            base = Z0 + s_ * 24 * KB
            w1s = V(base, [128, 8, 512], BF16)
            w3s = V(base + 8 * KB, [128, 8, 512], BF16)
            w2s = V(base + 16 * KB, [128, 4, DM], BF16)
            if moe:
                a1 = dr["w1_%d" % L][ex]; a3 = dr["w3_%d" % L][ex]; a2 = dr["w2_%d" % L][ex]
            else:
                a1 = dr["w1_%d" % L]; a3 = dr["w3_%d" % L]; a2 = dr["w2_%d" % L]
            cs = slice(c0 * 128, (c0 + g) * 128)
            op('gpsimd', lambda e: e.dma_start(out=w1s[:, :, 0:g * 128], in_=a1[:, cs].rearrange("(c p) n -> p c n", p=128)),
               writes=['fw%d' % s_], dma_sem='fw%d' % s_)
            op('gpsimd', lambda e: e.dma_start(out=w3s[:, :, 0:g * 128], in_=a3[:, cs].rearrange("(c p) n -> p c n", p=128)),
               writes=['fw%d' % s_], dma_sem='fw%d' % s_)
            op('gpsimd', lambda e: e.dma_start(out=w2s[:, 0:g, :], in_=a2[cs, :].rearrange("(j p) n -> p j n", p=128)),
               writes=['fw%d' % s_], dma_sem='fw%d' % s_)
            return (s_, w1s, w3s, w2s)

        def partA(ui):
            ex, c0, g, tc = units[ui]
            if (ex, c0) not in state:
                state[(ex, c0)] = load_w(ex, c0, g)
            s_, w1s, w3s, w2s = state[(ex, c0)]
            hb = ui % 3
            tok = slice(tc * 512, (tc + 1) * 512)
            for j in range(g):
                pa = j % 2

                def mm(e, w_, bank, j=j):
                    for k in range(8):
                        i = e.matmul(PS(bank), lhsT=w_[:, k, j * 128:(j + 1) * 128], rhs=xT[:, k, tok], start=(k == 0), stop=(k == 7))
                    return i
                op('tensor', fm(mm, w1s, pa), reads=['fw%d' % s_, 'xT'], writes=['ps%d' % pa])
                op('tensor', fm(mm, w3s, 2 + pa), reads=['fw%d' % s_, 'xT'], writes=['ps%d' % (2 + pa)])
                op('scalar', fm(lambda e, pa: e.activation(out=sil[pa], in_=PS(pa), func=AF.Silu), pa),
                   reads=['ps%d' % pa], writes=['sil%d' % pa])
                op('vector', fm(lambda e, pa, j, hb: e.tensor_tensor(out=hT[hb][:, j, :], in0=sil[pa], in1=PS(2 + pa), op=ALU.mult),
                                pa, j, hb), reads=['sil%d' % pa, 'ps%d' % (2 + pa)], writes=['hT%d' % hb])

        ycount = [0]

        def partB(ui):
            ex, c0, g, tc = units[ui]
            s_, w1s, w3s, w2s = state[(ex, c0)]
            hb = ui % 3
            for t4 in range(4):
                tt = tc * 4 + t4
                for hf in range(2):
                    bank = 4 + ycount[0] % 2
                    ycount[0] += 1

                    def mm(e, t4=t4, hf=hf, bank=bank):
                        for j in range(g):
                            i = e.matmul(PS(bank), lhsT=hT[hb][:, j, t4 * 128:(t4 + 1) * 128], rhs=w2s[:, j, hf * 512:(hf + 1) * 512],
                                         start=(j == 0), stop=(j == g - 1))
                        return i
                    op('tensor', mm, reads=['hT%d' % hb, 'fw%d' % s_], writes=['ps%d' % bank])
                    dst = ACC[:, tt, hf * 512:(hf + 1) * 512]
                    sc_ = gates[:, tt, ex:ex + 1] if moe else 1.0
                    op('vector', fm(lambda e, dst, bank, sc_: e.scalar_tensor_tensor(out=dst, in0=PS(bank), scalar=sc_, in1=dst,
                                                                                     op0=ALU.mult, op1=ALU.add), dst, bank, sc_),
                       reads=['ps%d' % bank, 'acc%d_%d' % (tt, hf), 'gates'], writes=['acc%d_%d' % (tt, hf)])

        for ui in range(len(units)):
            partA(ui)
            if ui >= 1:
                partB(ui - 1)
        partB(len(units) - 1)
        load_ln_params(L, 2)
        for tt in range(NT):
            r = tt % 2
            xn = V(Z0 + 66 * KB + r * 4 * KB, [128, DM], F32)
            xb = V(Z0 + 74 * KB + r * 2 * KB, [128, DM], BF16)
            z = ACC[:, tt, :]
            emit_ln(z, 'acc%d_0' % tt, xn, 'xn%d' % r, None if is_last else xb, 'xb%d' % r, r, z_res2='acc%d_1' % tt)
            dstd = y_out if is_last else xsp
            op('sync', fm(lambda e, xn, tt, dstd: e.dma_start(out=dstd[tt * 128:(tt + 1) * 128, :], in_=xn), xn, tt, dstd),
               reads=['xn%d' % r], writes=['xout'], dma_sem='xo%d' % r)
            if not is_last:
                emit_xT(tt, xb, 'xb%d' % r, 7)
        S.barrier()

    def moe_sparse_phase(L, is_last):
        C = MOE_C
        NST = C // 128
        CCH = [(0, 512), (512, C - 512)]
        SEL = V(0, [128, NT, C], BF16)
        SELT = V(0, [128, NST, SEQ], BF16)
        YBF = V(20 * KB, [128, NST, DM], BF16)
        XG = V(108 * KB, [128, 8, C], BF16)
        YACC = V(118 * KB, [128, NST, DM], F32)
        sil = [V(30 * KB, [128, C], BF16), V(138 * KB, [128, C], BF16)]
        hT = [V(66 * KB + i * 3 * KB, [128, 2, C], BF16) for i in range(3)]
        G0 = 32 * KB
        rw = V(G0, [128, 8, 8], BF16)
        lg = V(G0 + 256, [128, 16, 8], F32)
        m8a = V(G0 + 768, [128, 16, 8], F32)
        gates = V(G0 + 1280, [128, 16, 8], F32)
        tmpg = V(G0 + 1792, [128, 16, 8], F32)
        e2 = V(G0 + 2304, [128, 16, 1], F32)
        p1 = V(G0 + 2368, [128, 16, 1], F32)
        p2 = V(G0 + 2432, [128, 16, 1], F32)
        ind = V(G0 + 2560, [128, 16, 8], BF16)
        posm = V(G0 + 2816, [128, 16, 8], F32)
        iotap = V(G0 + 3328, [128, 8], F32)
        tris = V(G0 + 3392, [128, 128], BF16)
        onesb = V(G0 + 3648, [128, 128], BF16)
        iotar = V(G0 + 4096, [128, C], F32)
        op('gpsimd', lambda e: e.dma_start(out=rw, in_=dr["rt%d" % L].rearrange("(c p) n -> p c n", p=128)),
           writes=['rw'], dma_sem='rw')
        op('gpsimd', lambda e: e.dma_start(out=tris, in_=dr['tris']), writes=['tris'], dma_sem='tris')
        op('sync', lambda e: e.dma_start(out=iotap, in_=dr['iotap']), writes=['iotap'], dma_sem='iotap')
        op('sync', lambda e: e.dma_start(out=iotar, in_=dr['iotar']), writes=['iotar'], dma_sem='iotar')
        op('vector', lambda e: e.memset(onesb, 1.0), writes=['onesb'])

        def lmm(e):
            for tt in range(NT):
                for c in range(8):
                    i = e.matmul(PS(6)[:, tt * 8:(tt + 1) * 8], lhsT=xT[:, c, tt * 128:(tt + 1) * 128], rhs=rw[:, c, :],
                                 start=(c == 0), stop=(c == 7))
            return i
        op('tensor', lmm, reads=['xT', 'rw'], writes=['ps6'])
        op('vector', lambda e: e.tensor_copy(out=lg, in_=PS(6)[:, 0:128].rearrange("p (a b) -> p a b", a=16)),
           reads=['ps6'], writes=['lg'])
        for tt in range(NT):
            op('vector', fm(lambda e, tt: e.max(out=m8a[:, tt, :], in_=lg[:, tt, :]), tt), reads=['lg'], writes=['m8a'])
        op('vector', lambda e: e.tensor_tensor(out=e2, in0=m8a[:, :, 1:2], in1=m8a[:, :, 0:1], op=ALU.subtract),
           reads=['m8a'], writes=['e2'])
        op('scalar', lambda e: e.activation(out=e2, in_=e2, func=AF.Exp), reads=['e2'], writes=['e2'])
        op('vector', lambda e: e.tensor_scalar(out=p1, in0=e2, scalar1=1.0, scalar2=None, op0=ALU.add), reads=['e2'], writes=['p1'])
        op('vector', lambda e: e.reciprocal(out=p1, in_=p1), reads=['p1'], writes=['p1'])
        op('vector', lambda e: e.tensor_tensor(out=p2, in0=e2, in1=p1, op=ALU.mult), reads=['e2', 'p1'], writes=['p2'])
        op('vector', lambda e: e.tensor_tensor(out=gates, in0=lg, in1=m8a[:, :, 0:1].to_broadcast([128, 16, 8]), op=ALU.is_equal),
           reads=['lg', 'm8a'], writes=['gates'])
        op('vector', lambda e: e.tensor_tensor(out=gates, in0=gates, in1=p1.to_broadcast([128, 16, 8]), op=ALU.mult),
           reads=['gates', 'p1'], writes=['gates'])
        op('vector', lambda e: e.tensor_tensor(out=tmpg, in0=lg, in1=m8a[:, :, 1:2].to_broadcast([128, 16, 8]), op=ALU.is_equal),
           reads=['lg', 'm8a'], writes=['tmpg'])
        op('vector', lambda e: e.tensor_tensor(out=tmpg, in0=tmpg, in1=p2.to_broadcast([128, 16, 8]), op=ALU.mult),
           reads=['tmpg', 'p2'], writes=['tmpg'])
        op('vector', lambda e: e.tensor_tensor(out=gates, in0=gates, in1=tmpg, op=ALU.add), reads=['gates', 'tmpg'], writes=['gates'])
        op('vector', lambda e: e.tensor_scalar(out=ind, in0=gates, scalar1=0.0, scalar2=None, op0=ALU.is_gt),
           reads=['gates'], writes=['ind'])

        def pmm(e):
            for tt in range(NT):
                for tp in range(tt + 1):
                    i = e.matmul(PS(6)[:, tt * 8:(tt + 1) * 8], lhsT=(onesb if tp < tt else tris), rhs=ind[:, tp, :],
                                 start=(tp == 0), stop=(tp == tt))
            return i
        op('tensor', pmm, reads=['ind', 'onesb', 'tris'], writes=['ps6'])
        op('vector', lambda e: e.scalar_tensor_tensor(out=posm, in0=PS(6)[:, 0:128].rearrange("p (a b) -> p a b", a=16), scalar=1.0,
                                                      in1=ind, op0=ALU.add, op1=ALU.mult), reads=['ps6', 'ind'], writes=['posm'])
        op('vector', lambda e: e.tensor_scalar(out=posm, in0=posm, scalar1=-1.0, scalar2=None, op0=ALU.add),
           reads=['posm'], writes=['posm'])
        S.barrier()

        nchunks = DFFE // 128
        groups = [(c0, 2) for c0 in range(0, nchunks, 2)]
        a1_ = dr["w1_%d" % L]
        a3_ = dr["w3_%d" % L]
        a2_ = dr["w2_%d" % L]
        wsl = [0]
        bk = [0]
        ycount = [0]

        def load_w(ex, c0, g):
            s_ = wsl[0] % 3
            wsl[0] += 1
            base = (Z0 + s_ * 12 * KB) if s_ < 2 else 0
            w1s = V(base, [128, 8, 256], BF16)
            w3s = V(base + 4 * KB, [128, 8, 256], BF16)
            w2s = V(base + 8 * KB, [128, 2, DM], BF16)
            cs = slice(c0 * 128, (c0 + g) * 128)
            wr = ['fw%d' % s_] + (['R0'] if s_ == 2 else [])
            op('gpsimd', lambda e: e.dma_start(out=w1s, in_=a1_[ex][:, cs].rearrange("(c p) n -> p c n", p=128)),
               writes=wr, dma_sem='fw%d' % s_)
            op('gpsimd', lambda e: e.dma_start(out=w3s, in_=a3_[ex][:, cs].rearrange("(c p) n -> p c n", p=128)),
               writes=wr, dma_sem='fw%d' % s_)
            op('gpsimd', lambda e: e.dma_start(out=w2s, in_=a2_[ex][cs, :].rearrange("(j p) n -> p j n", p=128)),
               writes=wr, dma_sem='fw%d' % s_)
            return (s_, w1s, w3s, w2s)

        for ex in range(NEXP):
            for tt in range(NT):
                eng = 'vector'
                op(eng, fm(lambda e, tt, ex: e.tensor_scalar(out=SEL[:, tt, :], in0=iotar, scalar1=posm[:, tt, ex:ex + 1],
                                                            scalar2=None, op0=ALU.is_equal), tt, ex),
                   reads=['iotar', 'posm', 'R0'], writes=['sel%d' % tt])
            for fc in range(8):
                ba = bk[0] % 2
                bk[0] += 1

                def gmm(e, fc=fc, ba=ba):
                    for tt in range(NT):
                        e.matmul(PS(ba), lhsT=XTOK[:, tt, fc * 128:(fc + 1) * 128], rhs=SEL[:, tt, 0:512],
                                 start=(tt == 0), stop=(tt == NT - 1))
                    for tt in range(NT):
                        i = e.matmul(PS(2 + ba)[:, 0:C - 512], lhsT=XTOK[:, tt, fc * 128:(fc + 1) * 128], rhs=SEL[:, tt, 512:C],
                                     start=(tt == 0), stop=(tt == NT - 1))
                    return i
                op('tensor', gmm, reads=['xtok', 'R0'] + ['sel%d' % t for t in range(NT)], writes=['ps%d' % ba, 'ps%d' % (2 + ba)])
                op('scalar', fm(lambda e, fc, ba: e.copy(out=XG[:, fc, 0:512], in_=PS(ba)), fc, ba),
                   reads=['ps%d' % ba], writes=['xg'])
                op('scalar', fm(lambda e, fc, ba: e.copy(out=XG[:, fc, 512:C], in_=PS(2 + ba)[:, 0:C - 512]), fc, ba),
                   reads=['ps%d' % (2 + ba)], writes=['xg'])
            wstate = {}

            def partA(gi):
                c0, g = groups[gi]
                wstate[gi] = load_w(ex, c0, g)
                s_, w1s, w3s, w2s = wstate[gi]
                hb = gi % 3
                for (cs0, cn) in CCH:
                    for j in range(g):
                        pa = bk[0] % 2
                        bk[0] += 1

                        def mm(e, w_, bank, j=j, cs0=cs0, cn=cn):
                            for k in range(8):
                                i = e.matmul(PS(bank)[:, 0:cn], lhsT=w_[:, k, j * 128:(j + 1) * 128], rhs=XG[:, k, cs0:cs0 + cn],
                                             start=(k == 0), stop=(k == 7))
                            return i
                        r0 = ['R0'] if s_ == 2 else []
                        op('tensor', fm(mm, w1s, pa), reads=['fw%d' % s_, 'xg'] + r0, writes=['ps%d' % pa])
                        op('tensor', fm(mm, w3s, 2 + pa), reads=['fw%d' % s_, 'xg'] + r0, writes=['ps%d' % (2 + pa)])
                        op('scalar', fm(lambda e, pa, cn: e.activation(out=sil[pa][:, 0:cn], in_=PS(pa)[:, 0:cn], func=AF.Silu), pa, cn),
                           reads=['ps%d' % pa], writes=['sil%d' % pa])
                        op('vector', fm(lambda e, pa, j, hb, cs0, cn: e.tensor_tensor(
                            out=hT[hb][:, j, cs0:cs0 + cn], in0=sil[pa][:, 0:cn], in1=PS(2 + pa)[:, 0:cn], op=ALU.mult),
                            pa, j, hb, cs0, cn), reads=['sil%d' % pa, 'ps%d' % (2 + pa)], writes=['hT%d' % hb])

            def partB(gi):
                c0, g = groups[gi]
                s_, w1s, w3s, w2s = wstate[gi]
                hb = gi % 3
                for st_ in range(NST):
                    for hf in range(2):
                        bank = 4 + ycount[0] % 2
                        ycount[0] += 1

                        def mm(e, st_=st_, hf=hf, bank=bank):
                            for j in range(g):
                                i = e.matmul(PS(bank), lhsT=hT[hb][:, j, st_ * 128:(st_ + 1) * 128],
                                             rhs=w2s[:, j, hf * 512:(hf + 1) * 512], start=(j == 0), stop=(j == g - 1))
                            return i
                        op('tensor', mm, reads=['hT%d' % hb, 'fw%d' % s_] + (['R0'] if s_ == 2 else []), writes=['ps%d' % bank])
                        dst = YACC[:, st_, hf * 512:(hf + 1) * 512]
                        rn = 'yacc%d_%d' % (st_, hf)
                        if gi == 0:
                            op('vector', fm(lambda e, dst, bank: e.tensor_copy(out=dst, in_=PS(bank)), dst, bank),
                               reads=['ps%d' % bank], writes=[rn])
                        else:
                            op('vector', fm(lambda e, dst, bank: e.tensor_tensor(out=dst, in0=dst, in1=PS(bank), op=ALU.add), dst, bank),
                               reads=['ps%d' % bank, rn], writes=[rn])

            for gi in range(len(groups)):
                partA(gi)
                if gi >= 1:
                    partB(gi - 1)
            partB(len(groups) - 1)
            for st_ in range(NST):
                eng = 'scalar' if st_ % 2 == 0 else 'gpsimd'
                if eng == 'scalar':
                    op(eng, fm(lambda e, st_: e.copy(out=YBF[:, st_, :], in_=YACC[:, st_, :]), st_),
                       reads=['yacc%d_0' % st_, 'yacc%d_1' % st_], writes=['ybf'])
                else:
                    op(eng, fm(lambda e, st_: e.tensor_copy(out=YBF[:, st_, :], in_=YACC[:, st_, :]), st_),
                       reads=['yacc%d_0' % st_, 'yacc%d_1' % st_], writes=['ybf'])
            for tc in range(4):
                qb = 6 + tc % 2

                def bmm(e, tc=tc, qb=qb, ex=ex):
                    for t4 in range(4):
                        tt = tc * 4 + t4
                        for tp in range(tt + 1):
                            i = e.matmul(PS(qb)[:, t4 * 128:(t4 + 1) * 128],
                                         lhsT=ind[:, tp, ex:ex + 1].to_broadcast([128, 128]),
                                         rhs=(onesb if tp < tt else tris), start=(tp == 0), stop=(tp == tt))
                    return i
                op('tensor', bmm, reads=['ind', 'onesb', 'tris'] + ['sel%d' % t for t in range(NT)], writes=['ps%d' % qb])
                for st_ in range(NST):
                    op('vector', fm(lambda e, tc, qb, st_: e.tensor_scalar(out=SELT[:, st_, tc * 512:(tc + 1) * 512], in0=PS(qb),
                                                                           scalar1=iotap[:, st_:st_ + 1], scalar2=None,
                                                                           op0=ALU.is_equal), tc, qb, st_),
                       reads=['ps%d' % qb, 'iotap'], writes=['selt', 'R0'])
            for tt in range(NT):
                for hf in range(2):
                    bank = 4 + ycount[0] % 2
                    ycount[0] += 1

                    def smm(e, tt=tt, hf=hf, bank=bank):
                        for st_ in range(NST):
                            i = e.matmul(PS(bank), lhsT=SELT[:, st_, tt * 128:(tt + 1) * 128], rhs=YBF[:, st_, hf * 512:(hf + 1) * 512],
                                         start=(st_ == 0), stop=(st_ == NST - 1))
                        return i
                    op('tensor', smm, reads=['selt', 'ybf', 'R0'] + ['sel%d' % t for t in range(NT)], writes=['ps%d' % bank])
                    dst = ACC[:, tt, hf * 512:(hf + 1) * 512]
                    op('vector', fm(lambda e, dst, bank, tt, ex: e.scalar_tensor_tensor(
                        out=dst, in0=PS(bank), scalar=gates[:, tt, ex:ex + 1], in1=dst, op0=ALU.mult, op1=ALU.add), dst, bank, tt, ex),
                       reads=['ps%d' % bank, 'acc%d_%d' % (tt, hf), 'gates'], writes=['acc%d_%d' % (tt, hf)])
        S.barrier()
        load_ln_params(L, 2)
        for tt in range(NT):
            r = tt % 2
            xn = V(Z0 + 66 * KB + r * 4 * KB, [128, DM], F32)
            xb = V(Z0 + 74 * KB + r * 2 * KB, [128, DM], BF16)
            z = ACC[:, tt, :]
            emit_ln(z, 'acc%d_0' % tt, xn, 'xn%d' % r, None if is_last else xb, 'xb%d' % r, r, z_res2='acc%d_1' % tt)
            dstd = y_out if is_last else xsp
            op('sync', fm(lambda e, xn, tt, dstd: e.dma_start(out=dstd[tt * 128:(tt + 1) * 128, :], in_=xn), xn, tt, dstd),
               reads=['xn%d' % r], writes=['xout'], dma_sem='xo%d' % r)
            if not is_last:
                emit_xT(tt, xb, 'xb%d' % r, 7)
        S.barrier()

    initial_load()
    S.barrier()
    resid = x_in
    for li, L in enumerate(layer_ids):
        load_mixer_consts()
        if L % 2 == 0:
            if not (3.0 < stop <= 3.05):
                dsa_phase(L)
            if stop < 3:
                break
            pair_phase(L, 'moba', 4, 936, 1448, 1960, 4)
        else:
            pair_phase(L, 'dil', 8, 0, 1024, 2048, 0)
        if stop < 4:
            break
        outproj_phase(L, resid)
        if stop < 5:
            break
        if SPARSE_MOE and L % 2 == 1:
            moe_sparse_phase(L, li == len(layer_ids) - 1)
        else:
            ffn_phase(L, li == len(layer_ids) - 1)
        resid = xsp
    S.barrier()
    S.emit(nc)
    st.close()
    return nc


_CACHE = {}


def _layer_inputs(L, inp):
    i = L // 2
    d = {}
    if L % 2 == 0:
        w = np.ascontiguousarray(inp['even_w_in'][i])
        d["win%d" % L] = w
        sw = w.copy()
        sw = swap_halves(sw, 0, 8, 64)
        sw = swap_halves(sw, 512, 1, 64)
        sw = swap_halves(sw, 640, 8, 32)
        sw = swap_halves(sw, 896, 1, 32)
        sw = swap_halves(sw, 936, 8, 64)
        sw = swap_halves(sw, 1448, 8, 64)
        d["wsw%d" % L] = sw
        d["wout%d" % L] = np.ascontiguousarray(inp['even_w_out'][i])
        d["w1_%d" % L] = np.ascontiguousarray(inp['even_w1'][i])
        d["w3_%d" % L] = np.ascontiguousarray(inp['even_w3'][i])
        d["w2_%d" % L] = np.ascontiguousarray(inp['even_w2'][i])
        pre = 'even'
    else:
        w = np.ascontiguousarray(inp['odd_w_in'][i])
        d["win%d" % L] = w
        sw = w.copy()
        sw = swap_halves(sw, 0, 16, 64)
        sw = swap_halves(sw, 1024, 16, 64)
        d["wsw%d" % L] = sw
        d["wout%d" % L] = np.ascontiguousarray(inp['odd_w_out'][i])
        d["rt%d" % L] = np.ascontiguousarray(inp['odd_router'][i])
        d["w1_%d" % L] = np.ascontiguousarray(inp['odd_w1'][i])
        d["w3_%d" % L] = np.ascontiguousarray(inp['odd_w3'][i])
        d["w2_%d" % L] = np.ascontiguousarray(inp['odd_w2'][i])
        pre = 'odd'
    d["g1_%d" % L] = np.ascontiguousarray(inp[pre + '_ln1_g'][i]).reshape(1, DM)
    d["b1_%d" % L] = np.ascontiguousarray(inp[pre + '_ln1_b'][i]).reshape(1, DM)
    d["g2_%d" % L] = np.ascontiguousarray(inp[pre + '_ln2_g'][i]).reshape(1, DM)
    d["b2_%d" % L] = np.ascontiguousarray(inp[pre + '_ln2_b'][i]).reshape(1, DM)
    return d


def swap_fix(w):
    return w


def run_layers(x, layer_ids, inp):
    key = tuple(layer_ids)
    if key not in _CACHE:
        _CACHE[key] = build(list(layer_ids))
    nc = _CACHE[key]
    shared = dict(make_consts())
    for L in layer_ids:
        shared.update(_layer_inputs(L, inp))
    in_maps = []
    for b in range(8):
        m = dict(shared)
        m["x"] = np.ascontiguousarray(x[b])
        in_maps.append(m)
    res = run_bass_kernel_spmd(nc, in_maps, core_ids=list(range(8)))
    return np.stack([np.asarray(r["y"]) for r in res.results], axis=0).astype(np.float32)


LAUNCH_GROUPS = [[0, 1, 2, 3]]


def kernel(**inputs):
    inp = {k: np.asarray(v) for k, v in inputs.items()}
    x = np.asarray(inp['x'], dtype=np.float32)
    for grp in LAUNCH_GROUPS:
        x = run_layers(x, grp, inp)
    return x
```

```python
import numpy as np
import concourse.bass as bass
import concourse.mybir as mybir
from concourse.bass_utils import run_bass_kernel_spmd
from contextlib import ExitStack

F32 = mybir.dt.float32
BF16 = mybir.dt.bfloat16
U8 = mybir.dt.uint8
ALU = mybir.AluOpType
AF = mybir.ActivationFunctionType
AX = mybir.AxisListType

SEQ = 2048
DM = 1024
NT = 16
DEPTH = 4
ALPHA = float((2 * DEPTH) ** 0.25)
EPS = 1e-5
NEG = -30000.0
DFF = 2816
DFFE = 3584
NEXP = 8
KB = 1024
MOE_C = 640
SPARSE_MOE = True

ENGS = ['tensor', 'vector', 'scalar', 'gpsimd', 'sync']


class _Op(object):
    __slots__ = ('eng', 'fn', 'waits', 'dma_sem', 'idx', 'signal', 'count', 'dma_count')


class Sched(object):
    def __init__(self):
        self.ops = dict((e, []) for e in ENGS)
        self.last_write = {}
        self.readers = {}
        self.waited = dict((e, {}) for e in ENGS)
        self.dma_counts = {}
        self.dma_last = {}
        self.last_compute = {}

    def _dep(self, o, d, kind):
        if d is None or d is o:
            return
        if d.dma_sem is None:
            if d.eng == o.eng and o.dma_sem is None:
                if o.eng == 'tensor':
                    return
            key = d.eng
            val = d.idx
        else:
            key = ('dma', d.dma_sem)
            val = d.dma_count
        w = self.waited[o.eng]
        if w.get(key, -1) >= val:
            return
        w[key] = val
        d.signal = True
        o.waits.append(d)

    def op(self, eng, fn, reads=(), writes=(), dma_sem=None):
        o = _Op()
        o.eng = eng
        o.fn = fn
        o.waits = []
        o.dma_sem = dma_sem
        o.signal = False
        o.idx = len(self.ops[eng])
        if dma_sem is not None:
            c = self.dma_counts.get(dma_sem, 0) + 1
            self.dma_counts[dma_sem] = c
            o.dma_count = c
            self.dma_last[dma_sem] = o
        elif fn is not None:
            self.last_compute[eng] = o
        writes = list(writes) + [r for r in reads if r.startswith('ps') and r not in writes]
        reads = [r for r in reads if not r.startswith('ps')]
        for r in reads:
            self._dep(o, self.last_write.get(r), 'raw')
        for w_ in writes:
            self._dep(o, self.last_write.get(w_), 'waw')
            for rd in self.readers.get(w_, ()):
                self._dep(o, rd, 'war')
        for r in reads:
            self.readers.setdefault(r, []).append(o)
        for w_ in writes:
            self.last_write[w_] = o
            self.readers[w_] = []
        self.ops[eng].append(o)
        return o

    def barrier(self):
        lasts = list(self.last_compute.values()) + list(self.dma_last.values())
        for e in ENGS:
            o = self.op(e, None)
            for d in lasts:
                if d.dma_sem is None and d.eng == e and e in ('tensor', 'sync'):
                    continue
                self._dep(o, d, 'raw')
        self.last_write = {}
        self.readers = {}

    def emit(self, nc):
        dma_names = sorted(self.dma_counts.keys(), key=str)
        with ExitStack() as st:
            esem = {}
            for e in ENGS:
                esem[e] = st.enter_context(nc.semaphore("s_" + e))
            dsem = {}
            for i, n in enumerate(dma_names):
                dsem[n] = st.enter_context(nc.semaphore("d%d" % i))
            for e in ENGS:
                c = 0
                for o in self.ops[e]:
                    if o.dma_sem is None and o.signal:
                        assert o.fn is not None
                        c += 1
                        o.count = c
            block = st.enter_context(nc.Block())

            def mk(e):
                def body(eng):
                    for o in self.ops[e]:
                        for d in o.waits:
                            if d.dma_sem is None:
                                eng.wait_ge(esem[d.eng], d.count)
                            else:
                                eng.wait_ge(dsem[d.dma_sem], 16 * d.dma_count)
                        if o.fn is None:
                            continue
                        inst = o.fn(eng)
                        if o.dma_sem is not None:
                            inst.then_inc(dsem[o.dma_sem], 16)
                        elif o.signal:
                            inst.then_inc(esem[e], 1)
                return body

            for e in ENGS:
                if self.ops[e]:
                    getattr(block, e)(mk(e))


def _rope_tab(dim):
    inv = (np.float32(10000.0) ** (-np.arange(0, dim, 2, dtype=np.float32) / np.float32(dim))).astype(np.float32)
    ang = (np.arange(SEQ, dtype=np.float32)[:, None] * inv[None, :]).astype(np.float32)
    return np.cos(ang).astype(np.float32), np.sin(ang).astype(np.float32)


def make_consts():
    c = {}
    c['ident'] = np.eye(128, dtype=np.float32)
    cos64, sin64 = _rope_tab(64)
    cos32, sin32 = _rope_tab(32)
    p = np.arange(128)
    c['c64'] = np.ascontiguousarray(cos64.T[p % 32, :])
    sg = np.where((p % 64) < 32, -1.0, 1.0).astype(np.float32)[:, None]
    c['s64'] = np.ascontiguousarray(sin64.T[p % 32, :] * sg)
    c['c32'] = np.ascontiguousarray(cos32.T[p % 16, :])
    sg = np.where((p % 32) < 16, -1.0, 1.0).astype(np.float32)[:, None]
    c['s32'] = np.ascontiguousarray(sin32.T[p % 16, :] * sg)
    d = np.arange(SEQ)[None, :] - np.arange(128)[:, None]
    c['tcaus'] = np.where(d >= 0, 0.0, NEG).astype(np.float32)
    mult = ((d >= 0) & (d <= 128)).astype(np.int32) + ((d >= 0) & (d <= 512) & (d % 4 == 0)).astype(np.int32) \
        + ((d >= 0) & (d % 16 == 0)).astype(np.int32)
    lut = np.array([NEG, 0.0, 8.0 * np.log(2.0), 8.0 * np.log(3.0)], dtype=np.float32)
    c['tdil'] = lut[mult].astype(np.float32)
    e = np.zeros((8, 8, 128), np.float32)
    for n in range(8):
        e[n, n, :] = 1.0
    c['eoh'] = e.reshape(8, 1024)
    gm = np.zeros((128, 16, 8), np.float32)
    for qt in range(16):
        gm[:, qt, (qt // 2):] = -1e30
    c['gmask'] = gm.reshape(128, 128)
    tp = np.arange(128)
    c['tris'] = (tp[:, None] < tp[None, :]).astype(np.float32)
    c['iotar'] = np.tile(np.arange(MOE_C, dtype=np.float32)[None, :], (128, 1))
    c['iotap'] = (tp[:, None] + 128.0 * np.arange(8)[None, :]).astype(np.float32)
    return c


def swap_halves(w, lo, nheads, hd):
    out = w
    for h in range(nheads):
        a = lo + h * hd
        first = w[..., a:a + hd // 2].copy()
        out[..., a:a + hd // 2] = w[..., a + hd // 2:a + hd]
        out[..., a + hd // 2:a + hd] = first
    return out


def build(layer_ids, stop=99):
    nc = bass.Bass("TRN2", target_bir_lowering=False)
    S = Sched()
    op = S.op
    dr = {}

    def din(name, shape):
        dr[name] = nc.dram_tensor(name, list(shape), F32, kind="ExternalInput").ap()
        return dr[name]

    x_in = din("x", [SEQ, DM])
    for n, shp in (('ident', [128, 128]), ('c64', [128, SEQ]), ('s64', [128, SEQ]), ('c32', [128, SEQ]),
                   ('s32', [128, SEQ]), ('tcaus', [128, SEQ]), ('tdil', [128, SEQ]), ('eoh', [8, 1024]),
                   ('gmask', [128, 128]), ('tris', [128, 128]), ('iotar', [128, MOE_C]), ('iotap', [128, 8])):
        din(n, shp)
    for L in layer_ids:
        if L % 2 == 0:
            din("win%d" % L, [DM, 2472]); din("wsw%d" % L, [DM, 2472]); din("wout%d" % L, [DM, DM])
            din("w1_%d" % L, [DM, DFF]); din("w3_%d" % L, [DM, DFF]); din("w2_%d" % L, [DFF, DM])
        else:
            din("win%d" % L, [DM, 3072]); din("wsw%d" % L, [DM, 3072]); din("wout%d" % L, [DM, DM])
            din("rt%d" % L, [DM, NEXP])
            din("w1_%d" % L, [NEXP, DM, DFFE]); din("w3_%d" % L, [NEXP, DM, DFFE]); din("w2_%d" % L, [NEXP, DFFE, DM])
        for n in ('g1', 'b1', 'g2', 'b2'):
            din("%s_%d" % (n, L), [1, DM])
    y_out = nc.dram_tensor("y", [SEQ, DM], F32, kind="ExternalOutput").ap()
    xsp = nc.dram_tensor("xsp", [SEQ, DM], F32, kind="Internal").ap()

    st = ExitStack()
    big = st.enter_context(nc.sbuf_tensor("big", [128, 204 * KB], U8))
    P = st.enter_context(nc.psum_tensor("P", [128, 8, 512], F32))

    def V(off, shape, dt):
        esz = 4 if dt == F32 else 2
        n = 1
        for s_ in shape[1:]:
            n *= s_
        v = big[0:shape[0], off:off + n * esz].bitcast(dt)
        if len(shape) == 3:
            v = v.rearrange("p (a b) -> p a b", a=shape[1])
        elif len(shape) == 4:
            v = v.rearrange("p (a b c) -> p a b c", a=shape[1], b=shape[2])
        return v

    def PS(i):
        return P[:, i, :]

    def PSB(i):
        return P[:, i, :].bitcast(BF16)

    xT = V(0, [128, 8, SEQ], BF16)
    lnG = V(32 * KB, [128, DM], F32)
    lnB = V(36 * KB, [128, DM], F32)
    identb = V(40 * KB, [128, 128], BF16)
    onesf = V(40 * KB + 256, [128, 128], F32)
    stats = V(41 * KB, [128, 2, 2, 6], F32)
    mv = V(41 * KB + 128, [128, 2, 2], F32)
    rstd = V(41 * KB + 192, [128, 2, 1], F32)
    m8 = V(41 * KB + 256, [128, 8], F32)
    Z0 = 42 * KB

    def fm(f, *a):
        return lambda e: f(e, *a)

    op('gpsimd', lambda e: e.dma_start(out=identb, in_=dr['ident']), writes=['identb'], dma_sem='c0')
    op('vector', lambda e: e.memset(onesf, 1.0), writes=['onesf'])

    def emit_xT(tt, xb, xb_res, bank):
        def tr(e):
            pv = PSB(bank)
            for c in range(8):
                i = e.transpose(out=pv[:, c * 128:(c + 1) * 128], in_=xb[:, c * 128:(c + 1) * 128], identity=identb)
            return i
        op('tensor', tr, reads=[xb_res, 'identb'], writes=['ps%d' % bank])
        op('vector', lambda e: e.tensor_copy(out=xT[:, :, tt * 128:(tt + 1) * 128],
                                             in_=PSB(bank).rearrange("p (a b) -> p a b", a=8)),
           reads=['ps%d' % bank], writes=['xT'])

    def emit_ln(z, z_res, xn, xn_res, xb, xb_res, r, z_res2=None):
        z_res2 = z_res2 or z_res
        st_ = stats[:, r]
        op('vector', lambda e: e.bn_stats(out=st_[:, 0, :], in_=z[:, 0:512]), reads=[z_res], writes=['stats%d' % r])
        op('vector', lambda e: e.bn_stats(out=st_[:, 1, :], in_=z[:, 512:1024]), reads=[z_res2], writes=['stats%db' % r])
        op('vector', lambda e: e.bn_aggr(out=mv[:, r, :], in_=st_.rearrange("p a b -> p (a b)")),
           reads=['stats%d' % r, 'stats%db' % r], writes=['mv%d' % r])
        op('vector', lambda e: e.tensor_scalar(out=rstd[:, r, :], in0=mv[:, r, 1:2], scalar1=EPS, scalar2=None,
                                               op0=ALU.add), reads=['mv%d' % r], writes=['rstd%d' % r])
        op('scalar', lambda e: e.activation(out=rstd[:, r, :], in_=rstd[:, r, :], func=AF.Sqrt),
           reads=['rstd%d' % r], writes=['rstd%d' % r])
        op('vector', lambda e: e.reciprocal(out=rstd[:, r, :], in_=rstd[:, r, :]), reads=['rstd%d' % r], writes=['rstd%d' % r])
        op('vector', lambda e: e.tensor_scalar(out=xn, in0=z, scalar1=mv[:, r, 0:1], scalar2=rstd[:, r, :],
                                               op0=ALU.subtract, op1=ALU.mult),
           reads=[z_res, z_res2, 'mv%d' % r, 'rstd%d' % r], writes=[xn_res])
        op('gpsimd', lambda e: e.tensor_tensor(out=xn, in0=xn, in1=lnG, op=ALU.mult), reads=[xn_res, 'lnG'], writes=[xn_res])
        op('gpsimd', lambda e: e.tensor_tensor(out=xn, in0=xn, in1=lnB, op=ALU.add), reads=[xn_res, 'lnB'], writes=[xn_res])
        if xb is not None:
            op('scalar', lambda e: e.copy(out=xb, in_=xn), reads=[xn_res], writes=[xb_res])

    def load_ln_params(L, which):
        g = dr["g%d_%d" % (which, L)]
        b = dr["b%d_%d" % (which, L)]
        op('sync', lambda e: e.dma_start(out=lnG, in_=g.to_broadcast([128, DM])), writes=['lnG'], dma_sem='lng')
        op('sync', lambda e: e.dma_start(out=lnB, in_=b.to_broadcast([128, DM])), writes=['lnB'], dma_sem='lnb')

    wcount = [0]

    def proj_fm(wsrc, wswsrc, col_specs, ncols, rope, dst_fn, dst_res, WB, Ct=None, St=None):
        s_ = wcount[0] % 2
        wcount[0] += 1
        wt = V(WB + s_ * 4 * KB, [128, 8, 128], BF16)
        ws = V(WB + s_ * 4 * KB + 2 * KB, [128, 8, 128], BF16)
        for (doff, slo, n) in col_specs:
            op('gpsimd', fm(lambda e, doff, slo, n: e.dma_start(
                out=wt[:, :, doff:doff + n], in_=wsrc[:, slo:slo + n].rearrange("(c p) n -> p c n", p=128)), doff, slo, n),
               writes=['wt%d' % s_], dma_sem='wt%d' % s_)
            if rope:
                op('gpsimd', fm(lambda e, doff, slo, n: e.dma_start(
                    out=ws[:, :, doff:doff + n], in_=wswsrc[:, slo:slo + n].rearrange("(c p) n -> p c n", p=128)), doff, slo, n),
                   writes=['ws%d' % s_], dma_sem='ws%d' % s_)
        for tc in range(4):
            pa = tc % 2
            tok = slice(tc * 512, (tc + 1) * 512)

            def mm(e, w_, bank, tok=tok):
                for k in range(8):
                    i = e.matmul(PS(bank)[0:ncols, :], lhsT=w_[:, k, 0:ncols], rhs=xT[:, k, tok],
                                 start=(k == 0), stop=(k == 7))
                return i
            op('tensor', fm(mm, wt, pa), reads=['wt%d' % s_, 'xT'], writes=['ps%d' % pa])
            dst = dst_fn(tc)
            if rope:
                op('tensor', fm(mm, ws, 2 + pa), reads=['ws%d' % s_, 'xT'], writes=['ps%d' % (2 + pa)])
                t1 = V(WB + 8 * KB + pa * 4 * KB, [128, 512], F32)
                t2 = V(WB + 8 * KB + pa * 4 * KB + 2 * KB, [128, 512], F32)
                op('vector', fm(lambda e, t1, pa, tok: e.tensor_tensor(out=t1[0:ncols, :], in0=PS(pa)[0:ncols, :],
                                                                       in1=Ct[0:ncols, tok], op=ALU.mult), t1, pa, tok),
                   reads=['ps%d' % pa, 'ctab', 'ctab32'], writes=['t1_%d' % pa])
                op('vector', fm(lambda e, t2, pa, tok: e.tensor_tensor(out=t2[0:ncols, :], in0=PS(2 + pa)[0:ncols, :],
                                                                       in1=St[0:ncols, tok], op=ALU.mult), t2, pa, tok),
                   reads=['ps%d' % (2 + pa), 'stab', 'stab32'], writes=['t2_%d' % pa])
                op('gpsimd', fm(lambda e, t1, t2, dst: e.tensor_tensor(out=dst, in0=t1[0:ncols, :], in1=t2[0:ncols, :],
                                                                       op=ALU.add), t1, t2, dst),
                   reads=['t1_%d' % pa, 't2_%d' % pa], writes=[dst_res])
            else:
                op('scalar', fm(lambda e, dst, pa: e.copy(out=dst, in_=PS(pa)[0:ncols, :]), dst, pa),
                   reads=['ps%d' % pa], writes=[dst_res])

    def proj_tm(wsrc, slo, ncols, evac, WB):
        s_ = wcount[0] % 2
        wcount[0] += 1
        wt = V(WB + s_ * 4 * KB, [128, 8, 128], BF16)
        op('gpsimd', lambda e: e.dma_start(out=wt[:, :, 0:ncols],
                                           in_=wsrc[:, slo:slo + ncols].rearrange("(c p) n -> p c n", p=128)),
           writes=['wt%d' % s_], dma_sem='wt%d' % s_)
        for tt in range(NT):
            bank = 4 + tt % 2

            def mm(e, tt=tt, bank=bank):
                for k in range(8):
                    i = e.matmul(PS(bank)[:, 0:ncols], lhsT=xT[:, k, tt * 128:(tt + 1) * 128], rhs=wt[:, k, 0:ncols],
                                 start=(k == 0), stop=(k == 7))
                return i
            op('tensor', mm, reads=['wt%d' % s_, 'xT'], writes=['ps%d' % bank])
            evac(tt, PS(bank)[:, 0:ncols], bank)

    ucount = [0]
    hcount = [0]
    deferred = []

    def defer(delay, fn):
        deferred.append([delay, fn])

    def tick():
        due = []
        for it in deferred:
            it[0] -= 1
        for it in list(deferred):
            if it[0] <= 0:
                due.append(it)
                deferred.remove(it)
        for it in due:
            it[1]()

    def flush_all():
        while deferred:
            it = deferred.pop(0)
            it[1]()

    def attn_head(chunks, kT_fn, qT_fn, bias_fn, V_fn, parity, OTdst_fn, reads, WKB):
        pT = [V(WKB + i * KB, [128, 512], BF16) for i in range(3)]
        rd = V(WKB + 3 * KB, [128, 512], F32)
        osb = [V(WKB + 5 * KB + i * 2 * KB, [128, 512], F32) for i in range(2)]
        M = 65 if parity == 0 else 128
        for (q0, N) in chunks:
            hc = hcount[0]
            hcount[0] += 1
            ob = 2 + hc % 2
            ktmax = (q0 + N - 1) // 128
            for kt in range(ktmax + 1):
                u = ucount[0]
                ucount[0] += 1
                sb = u % 2
                pb = u % 3
                qs = max(q0, kt * 128)
                n = q0 + N - qs
                bl, br = bias_fn(kt, qs, n, q0)

                def mms(e, kt=kt, qs=qs, n=n, sb=sb, bl=bl, br=br):
                    e.matmul(PS(sb)[:, 0:n], lhsT=kT_fn(kt), rhs=qT_fn(qs, n), start=True, stop=False)
                    return e.matmul(PS(sb)[:, 0:n], lhsT=bl, rhs=br, start=False, stop=True)
                op('tensor', mms, reads=list(reads), writes=['ps%d' % sb])
                op('scalar', fm(lambda e, pb, sb, n: e.activation(out=pT[pb][:, 0:n], in_=PS(sb)[:, 0:n], func=AF.Exp,
                                                                  scale=0.125), pb, sb, n),
                   reads=['ps%d' % sb], writes=['pT%d' % pb])

                def mmo(e, kt=kt, qs=qs, n=n, pb=pb, ob=ob, q0=q0, ktmax=ktmax):
                    return e.matmul(PS(ob)[0:M, qs - q0:qs - q0 + n], lhsT=V_fn(kt), rhs=pT[pb][:, 0:n],
                                    start=(kt == 0), stop=(kt == ktmax), skip_group_check=True)
                tick()
                defer(1, lambda mmo=mmo, pb=pb, ob=ob: op('tensor', mmo, reads=['pT%d' % pb] + list(reads),
                                                          writes=['ps%d' % ob]))
                if kt == ktmax:
                    dp = 64 if parity == 0 else 0
                    lo, hi = (0, 64) if parity == 0 else (64, 128)
                    mo = 64 if parity == 0 else 128
                    osl = osb[hc % 2]
                    dst = OTdst_fn(q0, N)

                    def norm1(ob=ob, N=N, dp=dp):
                        op('vector', lambda e: e.reciprocal(out=rd[dp:dp + 1, 0:N], in_=PS(ob)[dp:dp + 1, 0:N]),
                           reads=['ps%d' % ob], writes=['rd'])

                    def norm2(ob=ob, N=N, dp=dp, lo=lo, hi=hi, mo=mo, osl=osl, dst=dst, hc=hc):
                        op('tensor', lambda e: e.matmul(PS(4)[0:mo, 0:N], lhsT=onesf[dp:dp + 1, 0:mo], rhs=rd[dp:dp + 1, 0:N],
                                                        start=True, stop=True), reads=['rd', 'onesf'], writes=['ps4'])
                        op('scalar', lambda e: e.copy(out=osl[lo:hi, 0:N], in_=PS(ob)[lo:hi, 0:N]),
                           reads=['ps%d' % ob], writes=['osb%d' % (hc % 2)])
                        op('vector', lambda e: e.tensor_tensor(out=dst, in0=osl[lo:hi, 0:N], in1=PS(4)[lo:hi, 0:N], op=ALU.mult),
                           reads=['osb%d' % (hc % 2), 'ps4'], writes=['OT'])
                    defer(1, norm1)
                    defer(2, norm2)

    def init_vaug(VE, VO, nE, nO):
        op('gpsimd', lambda e: e.memset(VE, 1.0), writes=['VE'])
        op('gpsimd', lambda e: e.memset(VO, 0.0), writes=['VO'])
        op('gpsimd', lambda e: e.memset(VO[:, :, :, 0:1], 1.0), writes=['VO'])

    def initial_load():
        for tt in range(NT):
            r = tt % 2
            xs = V(Z0 + r * 4 * KB, [128, DM], F32)
            xb = V(Z0 + 8 * KB + r * 2 * KB, [128, DM], BF16)
            op('sync', fm(lambda e, xs, tt: e.dma_start(out=xs, in_=x_in[tt * 128:(tt + 1) * 128, :]), xs, tt),
               writes=['xs%d' % r], dma_sem='xs%d' % r)
            op('scalar', fm(lambda e, xs, xb: e.copy(out=xb, in_=xs), xs, xb), reads=['xs%d' % r], writes=['xb%d' % r])
            emit_xT(tt, xb, 'xb%d' % r, 7)

    OT = V(Z0, [128, 8, SEQ], BF16)
    c64 = V(Z0 + 32 * KB, [128, SEQ], F32)
    s64 = V(Z0 + 40 * KB, [128, SEQ], F32)
    tcaus = V(Z0 + 48 * KB, [128, SEQ], BF16)
    tdil = V(Z0 + 52 * KB, [128, SEQ], BF16)
    eoh = V(Z0 + 56 * KB, [8, 8, 128], BF16)
    gmask = V(Z0 + 58 * KB, [128, 16, 8], F32)
    M0 = Z0 + 59 * KB

    def load_mixer_consts():
        op('sync', lambda e: e.dma_start(out=c64, in_=dr['c64']), writes=['ctab'], dma_sem='c64')
        op('sync', lambda e: e.dma_start(out=s64, in_=dr['s64']), writes=['stab'], dma_sem='s64')
        op('gpsimd', lambda e: e.dma_start(out=tcaus, in_=dr['tcaus']), writes=['tcaus'], dma_sem='tcaus')
        op('gpsimd', lambda e: e.dma_start(out=tdil, in_=dr['tdil']), writes=['tdil'], dma_sem='tdil')
        op('gpsimd', lambda e: e.dma_start(out=eoh, in_=dr['eoh'].rearrange("p (a b) -> p a b", a=8)), writes=['eoh'], dma_sem='eoh')
        op('sync', lambda e: e.dma_start(out=gmask, in_=dr['gmask'].rearrange("p (a b) -> p a b", a=16)), writes=['gmask'], dma_sem='gmask')

    def dsa_phase(L):
        win = dr["win%d" % L]
        wsw = dr["wsw%d" % L]
        kaT = V(M0, [128, SEQ], BF16)
        ikT = V(M0 + 4 * KB, [128, SEQ], BF16)
        qaT = V(M0 + 8 * KB, [128, 4, SEQ], BF16)
        iqT = V(M0 + 24 * KB, [128, 3, SEQ], BF16)
        VaE = V(M0 + 36 * KB, [128, 16, 1, 65], BF16)
        VaO = V(M0 + 36 * KB + 2560, [128, 16, 1, 128], BF16)
        wtok = V(M0 + 36 * KB + 2560 + 4 * KB, [128, 16, 8], F32)
        W0 = M0 + 44 * KB
        c32 = V(W0, [128, SEQ], F32)
        s32 = V(W0 + 8 * KB, [128, SEQ], F32)
        WB = W0 + 16 * KB
        op('sync', lambda e: e.dma_start(out=c32, in_=dr['c32']), writes=['ctab32'], dma_sem='c32')
        op('sync', lambda e: e.dma_start(out=s32, in_=dr['s32']), writes=['stab32'], dma_sem='s32')
        proj_fm(win, wsw, [(0, 896, 32), (32, 896, 32), (64, 896, 32)], 96, True,
                lambda tc: ikT[0:96, tc * 512:(tc + 1) * 512], 'ikT', WB, c32, s32)
        for j in range(3):
            nc_ = 96 if j < 2 else 64
            proj_fm(win, wsw, [(0, 640 + 96 * j, nc_)], nc_, True,
                    fm(lambda tc, j, nc_: iqT[0:nc_, j, tc * 512:(tc + 1) * 512], j, nc_), 'iqT', WB, c32, s32)
        proj_tm(win, 928, 8, lambda tt, ps, bank: op(
            'scalar', lambda e: e.mul(out=wtok[:, tt, :], in_=ps, mul=1.0 / 16.0), reads=['ps%d' % bank], writes=['wtok']), WB)
        proj_fm(win, wsw, [(0, 512, 64), (64, 512, 64)], 128, True,
                lambda tc: kaT[:, tc * 512:(tc + 1) * 512], 'kaT', WB, c64, s64)
        for j in range(4):
            proj_fm(win, wsw, [(0, 128 * j, 128)], 128, True,
                    fm(lambda tc, j: qaT[:, j, tc * 512:(tc + 1) * 512], j), 'qaT', WB, c64, s64)
        if stop < 1:
            return
        init_vaug(VaE, VaO, 1, 1)

        def evac_va(tt, ps, bank):
            op('scalar', lambda e: e.copy(out=VaE[:, tt, 0, 0:64], in_=ps), reads=['ps%d' % bank], writes=['VE'])
            op('vector', lambda e: e.tensor_copy(out=VaO[:, tt, 0, 64:128], in_=ps), reads=['ps%d' % bank], writes=['VO'])
        proj_tm(win, 576, 64, evac_va, WB)
        S.barrier()
        if stop < 2:
            return
        sc = [V(W0 + i * 8 * KB, [128, SEQ], F32) for i in range(2)]
        rl = [V(W0 + 16 * KB + i * 2 * KB, [128, 512], F32) for i in range(2)]
        ng = V(W0 + 20 * KB, [128, SEQ], BF16)
        negT = V(W0 + 24 * KB, [128, 16, 512], BF16)
        WKB = W0 + 40 * KB
        rlc = [0]
        junk = [V(WKB + 9 * KB + i * 4 * KB, [128, SEQ], BF16) for i in range(2)]
        ntau = V(41 * KB + 320, [128, 2], F32)
        cnt = V(41 * KB + 336, [128, 2], F32)
        dlt = V(41 * KB + 352, [128, 2], F32)
        tsel = V(41 * KB + 368, [128, 2], F32)
        RNG = 64.0
        NIT = 26

        def score_tile(qt):
            scb = sc[qt % 2]
            scr = 'sc%d' % (qt % 2)
            NK = 128 * (qt + 1)
            nkc = (NK + 511) // 512
            for kc in range(nkc):
                wdt = min(512, NK - kc * 512)
                for h in range(8):
                    bank = 5 + rlc[0] % 2
                    rb = rlc[0] % 2
                    rlc[0] += 1
                    pbase = 32 * (h % 3)

                    def mm(e, h=h, kc=kc, wdt=wdt, bank=bank, pbase=pbase, qt=qt):
                        return e.matmul(PS(bank)[:, 0:wdt], lhsT=iqT[pbase:pbase + 32, h // 3, qt * 128:(qt + 1) * 128],
                                        rhs=ikT[pbase:pbase + 32, kc * 512:kc * 512 + wdt], start=True, stop=True)
                    op('tensor', mm, reads=['iqT', 'ikT'], writes=['ps%d' % bank])
                    op('scalar', fm(lambda e, rb, bank, wdt: e.activation(out=rl[rb][:, 0:wdt], in_=PS(bank)[:, 0:wdt],
                                                                          func=AF.Relu), rb, bank, wdt),
                       reads=['ps%d' % bank], writes=['rl%d' % rb])
                    dstv = scb[:, kc * 512:kc * 512 + wdt]
                    if h == 0:
                        op('vector', fm(lambda e, dstv, rb, wdt, qt, h: e.tensor_scalar(
                            out=dstv, in0=rl[rb][:, 0:wdt], scalar1=wtok[:, qt, h:h + 1], scalar2=None, op0=ALU.mult),
                            dstv, rb, wdt, qt, h), reads=['rl%d' % rb, 'wtok'], writes=[scr])
                    else:
                        op('vector', fm(lambda e, dstv, rb, wdt, qt, h: e.scalar_tensor_tensor(
                            out=dstv, in0=rl[rb][:, 0:wdt], scalar=wtok[:, qt, h:h + 1], in1=dstv, op0=ALU.mult, op1=ALU.add),
                            dstv, rb, wdt, qt, h), reads=['rl%d' % rb, 'wtok', scr], writes=[scr])
            dg = scb[:, qt * 128:(qt + 1) * 128]
            op('gpsimd', fm(lambda e, dg: e.affine_select(out=dg, in_=dg, pattern=[[-1, 128]], compare_op=ALU.is_ge,
                                                          fill=-3e30, base=0, channel_multiplier=1), dg),
               reads=[scr], writes=[scr])

        def bisect_tiles(qts):
            qts = [q for q in qts if q >= 2]
            for qt in qts:
                j = qt % 2
                op('vector', fm(lambda e, j: e.memset(ntau[:, j:j + 1], 0.0), j), writes=['ntau%d' % j])
            for it in range(NIT):
                step = RNG / float(2 ** (it + 1))
                for qt in qts:
                    j = qt % 2
                    NK = 128 * (qt + 1)
                    scb = sc[j]
                    op('scalar', fm(lambda e, j, NK, scb: e.activation(out=junk[j][:, 0:NK], in_=scb[:, 0:NK], func=AF.Sign,
                                                                       bias=ntau[:, j:j + 1], scale=1.0,
                                                                       accum_out=cnt[:, j:j + 1]), j, NK, scb),
                       reads=['sc%d' % j, 'ntau%d' % j], writes=['junk%d' % j, 'cnt%d' % j])
                    op('vector', fm(lambda e, j, NK, step: e.tensor_scalar(out=dlt[:, j:j + 1], in0=cnt[:, j:j + 1],
                                                                           scalar1=float(512 - NK), scalar2=-2.0 * step,
                                                                           op0=ALU.is_ge, op1=ALU.mult), j, NK, step),
                       reads=['cnt%d' % j], writes=['dlt%d' % j])
                    op('vector', fm(lambda e, j, step: e.scalar_tensor_tensor(out=ntau[:, j:j + 1], in0=dlt[:, j:j + 1],
                                                                              scalar=step, in1=ntau[:, j:j + 1],
                                                                              op0=ALU.add, op1=ALU.add), j, step),
                       reads=['dlt%d' % j, 'ntau%d' % j], writes=['ntau%d' % j])
            last = RNG / float(2 ** NIT)
            for qt in qts:
                j = qt % 2
                op('vector', fm(lambda e, j: e.tensor_scalar(out=tsel[:, j:j + 1], in0=ntau[:, j:j + 1], scalar1=-1.0,
                                                             scalar2=-last, op0=ALU.mult, op1=ALU.add), j),
                   reads=['ntau%d' % j], writes=['tsel%d' % j])

        def mask_tile(qt):
            j = qt % 2
            scb = sc[j]
            scr = 'sc%d' % j
            NK = 128 * (qt + 1)
            if qt >= 2:
                op('vector', fm(lambda e, scb, NK, j: e.tensor_scalar(out=ng[:, 0:NK], in0=scb[:, 0:NK], scalar1=tsel[:, j:j + 1],
                                                                      scalar2=NEG, op0=ALU.is_lt, op1=ALU.mult), scb, NK, j),
                   reads=[scr, 'tsel%d' % j], writes=['ng'])
            else:
                op('vector', fm(lambda e, scb, NK: e.tensor_scalar(out=ng[:, 0:NK], in0=scb[:, 0:NK], scalar1=-1e29,
                                                                   scalar2=NEG, op0=ALU.is_lt, op1=ALU.mult), scb, NK),
                   reads=[scr], writes=['ng'])
            qcol = (qt % 4) * 128
            for k0 in range(0, qt + 1, 4):
                k1 = min(qt + 1, k0 + 4)

                def tr(e, k0=k0, k1=k1):
                    pv = PSB(7)
                    for kt in range(k0, k1):
                        i = e.transpose(out=pv[:, (kt - k0) * 128:(kt - k0 + 1) * 128], in_=ng[:, kt * 128:(kt + 1) * 128],
                                        identity=identb)
                    return i
                op('tensor', tr, reads=['ng', 'identb'], writes=['ps7'])
                op('vector', fm(lambda e, k0, k1, qcol: e.tensor_copy(
                    out=negT[:, k0:k1, qcol:qcol + 128],
                    in_=PSB(7)[:, 0:(k1 - k0) * 128].rearrange("p (a b) -> p a b", a=k1 - k0)), k0, k1, qcol),
                   reads=['ps7'], writes=['negT'])

        for qp in range(0, NT, 2):
            score_tile(qp)
            score_tile(qp + 1)
            bisect_tiles([qp, qp + 1])
            mask_tile(qp)
            mask_tile(qp + 1)
            if qp % 4 == 2:
                q0 = (qp // 4) * 512
                for h in range(8):
                    par = h % 2
                    pb_ = 64 * par
                    attn_head(
                        [(q0, 512)],
                        lambda kt, pb_=pb_: kaT[pb_:pb_ + 64, kt * 128:(kt + 1) * 128],
                        lambda qs, n, pb_=pb_, h=h: qaT[pb_:pb_ + 64, h // 2, qs:qs + n],
                        lambda kt, qs, n, q0_: (identb, negT[:, kt, qs - q0_:qs - q0_ + n]),
                        (lambda kt: VaE[:, kt, 0, :]) if par == 0 else (lambda kt: VaO[:, kt, 0, :]),
                        par,
                        lambda q0_, N, pb_=pb_, h=h: OT[pb_:pb_ + 64, h // 2, q0_:q0_ + N],
                        ['kaT', 'qaT', 'negT', 'VE', 'VO', 'identb'], WKB)
                flush_all()
        S.barrier()

    def pair_phase(L, kind, npairs, qoff, koff, voff, otc0):
        win = dr["win%d" % L]
        wsw = dr["wsw%d" % L]
        qT = V(M0, [128, SEQ], BF16)
        kT = V(M0 + 4 * KB, [128, SEQ], BF16)
        VE = V(M0 + 8 * KB, [128, 16, 1, 65], BF16)
        VO = V(M0 + 8 * KB + 2560, [128, 16, 1, 128], BF16)
        G0 = M0 + 15 * KB
        kmf = V(G0, [128, 8], F32)
        kmb = V(G0 + 64, [128, 8], BF16)
        gm = V(G0 + 256, [128, 16, 2, 8], F32)
        nsel = V(G0 + 256 + KB, [128, 16, 2, 8], BF16)
        nselT = V(G0 + 2 * KB, [8, 2, SEQ], BF16)
        m8p = V(G0 + 10 * KB, [128, 8], F32)
        WB = G0 + 11 * KB
        WKB = WB + 16 * KB
        init_vaug(VE, VO, 1, 1)
        for j in range(npairs):
            if stop < 3.015:
                continue
            proj_fm(win, wsw, [(0, qoff + 128 * j, 128)], 128, True, lambda tc: qT[:, tc * 512:(tc + 1) * 512], 'qT', WB, c64, s64)
            if stop < 3.025:
                continue
            proj_fm(win, wsw, [(0, koff + 128 * j, 128)], 128, True, lambda tc: kT[:, tc * 512:(tc + 1) * 512], 'kT', WB, c64, s64)
            if stop < 3.035:
                continue

            def evac_v(tt, ps, bank):
                op('scalar', lambda e: e.copy(out=VE[:, tt, 0, 0:64], in_=ps[:, 0:64]), reads=['ps%d' % bank], writes=['VE'])
                op('scalar', lambda e: e.copy(out=VO[:, tt, 0, 64:128], in_=ps[:, 64:128]), reads=['ps%d' % bank], writes=['VO'])
            proj_tm(win, voff + 128 * j, 128, evac_v, WB)
            if stop < 3.1:
                continue
            if kind == 'moba':
                op('vector', lambda e: e.tensor_reduce(out=kmf, in_=kT.rearrange("p (n k) -> p n k", n=8), axis=AX.X, op=ALU.add),
                   reads=['kT'], writes=['kmf'])
                op('vector', lambda e: e.tensor_scalar(out=kmb, in0=kmf, scalar1=1.0 / 256.0, scalar2=None, op0=ALU.mult),
                   reads=['kmf'], writes=['kmb'])

                def gmm(e):
                    for qt in range(NT):
                        for hh in range(2):
                            i = e.matmul(PS(5)[:, (qt * 2 + hh) * 8:(qt * 2 + hh) * 8 + 8],
                                         lhsT=qT[64 * hh:64 * hh + 64, qt * 128:(qt + 1) * 128],
                                         rhs=kmb[64 * hh:64 * hh + 64, :], start=True, stop=True)
                    return i
                op('tensor', gmm, reads=['qT', 'kmb'], writes=['ps5'])
                for hh in range(2):
                    op('vector', fm(lambda e, hh: e.tensor_tensor(
                        out=gm[:, :, hh, :], in0=PS(5)[:, 0:256].rearrange("p (a b c) -> p a b c", a=16, b=2)[:, :, hh, :],
                        in1=gmask, op=ALU.add), hh), reads=['ps5', 'gmask'], writes=['gm'])
                for qt in range(NT if stop >= 3.2 else 0):
                    for hh in range(2):
                        op('vector', fm(lambda e, qt, hh: e.max(out=m8p, in_=gm[:, qt, hh, :]), qt, hh), reads=['gm'], writes=['m8p'])
                        op('vector', fm(lambda e, qt, hh: e.tensor_scalar(out=nsel[:, qt, hh, :], in0=gm[:, qt, hh, :],
                                                                          scalar1=m8p[:, 2:3], scalar2=NEG, op0=ALU.is_lt,
                                                                          op1=ALU.mult), qt, hh),
                           reads=['gm', 'm8p'], writes=['nsel'])
                for q4 in range(4 if stop >= 3.3 else 0):
                    def trs(e, q4=q4):
                        pv = PSB(7)
                        for hh in range(2):
                            for t4 in range(4):
                                qt = q4 * 4 + t4
                                i = e.transpose(out=pv[0:8, hh * 512 + t4 * 128:hh * 512 + (t4 + 1) * 128],
                                                in_=nsel[:, qt, hh, :], identity=identb)
                        return i
                    op('tensor', trs, reads=['nsel', 'identb'], writes=['ps7'])
                    op('vector', fm(lambda e, q4: e.tensor_copy(
                        out=nselT[:, :, q4 * 512:(q4 + 1) * 512],
                        in_=PSB(7)[0:8, :].rearrange("p (a b) -> p a b", a=2)), q4), reads=['ps7'], writes=['nselT'])
            for hh in range(2 if stop >= 3.4 else 0):
                pb_ = 64 * hh
                if kind == 'moba':
                    chunks = [(256 * b, 256) for b in range(8)]

                    def bias_fn(kt, qs, n, q0_, hh=hh):
                        b = q0_ // 256
                        if kt // 2 < b:
                            return eoh[:, kt // 2, :], nselT[:, hh, qs:qs + n]
                        return identb, tcaus[:, qs - 128 * kt:qs - 128 * kt + n]
                    rds = ['kT', 'qT', 'nselT', 'VE', 'VO', 'identb', 'tcaus', 'eoh']
                else:
                    chunks = [(512 * c, 512) for c in range(4)]

                    def bias_fn(kt, qs, n, q0_):
                        return identb, tdil[:, qs - 128 * kt:qs - 128 * kt + n]
                    rds = ['kT', 'qT', 'VE', 'VO', 'identb', 'tdil']
                attn_head(chunks,
                          lambda kt, pb_=pb_: kT[pb_:pb_ + 64, kt * 128:(kt + 1) * 128],
                          lambda qs, n, pb_=pb_: qT[pb_:pb_ + 64, qs:qs + n],
                          bias_fn,
                          (lambda kt: VE[:, kt, 0, :]) if hh == 0 else (lambda kt: VO[:, kt, 0, :]),
                          hh,
                          lambda q0_, N, pb_=pb_, j=j: OT[pb_:pb_ + 64, otc0 + j, q0_:q0_ + N],
                          rds, WKB)
            flush_all()
        S.barrier()

    ACC = V(140 * KB, [128, NT, DM], F32)

    XTOK = V(76 * KB, [128, NT, DM], BF16)

    def outproj_phase(L, resid_src):
        sparse = SPARSE_MOE and (L % 2 == 1)
        wo = V(108 * KB, [128, 8, DM], BF16) if sparse else V(Z0 + 32 * KB, [128, 8, DM], BF16)
        wout = dr["wout%d" % L]
        for hf in range(2):
            op('gpsimd', fm(lambda e, hf: e.dma_start(out=wo[:, :, hf * 512:(hf + 1) * 512],
                                                      in_=wout[:, hf * 512:(hf + 1) * 512].rearrange("(c p) n -> p c n", p=128)), hf),
               writes=['wo'], dma_sem='wo')
        load_ln_params(L, 1)
        for tt in range(NT):
            r = tt % 2
            xr = V(Z0 + 48 * KB + r * 4 * KB, [128, DM], F32)
            z = V(Z0 + 56 * KB + r * 4 * KB, [128, DM], F32)
            xb = V(Z0 + 64 * KB + r * 2 * KB, [128, DM], BF16)
            if sparse:
                xr = V(124 * KB + r * 4 * KB, [128, DM], F32)
                z = V(132 * KB + r * 4 * KB, [128, DM], F32)
                xb = XTOK[:, tt, :]
            op('sync', fm(lambda e, xr, tt: e.dma_start(out=xr, in_=resid_src[tt * 128:(tt + 1) * 128, :]), xr, tt),
               writes=['xr%d' % r], dma_sem='xr%d' % r)
            for hf in range(2):
                bank = 2 * r + hf

                def mm(e, tt=tt, hf=hf, bank=bank):
                    for c in range(8):
                        i = e.matmul(PS(bank), lhsT=OT[:, c, tt * 128:(tt + 1) * 128], rhs=wo[:, c, hf * 512:(hf + 1) * 512],
                                     start=(c == 0), stop=(c == 7))
                    return i
                op('tensor', mm, reads=['OT', 'wo'], writes=['ps%d' % bank])
                op('vector', fm(lambda e, z, xr, hf, bank: e.scalar_tensor_tensor(
                    out=z[:, hf * 512:(hf + 1) * 512], in0=xr[:, hf * 512:(hf + 1) * 512], scalar=ALPHA, in1=PS(bank),
                    op0=ALU.mult, op1=ALU.add), z, xr, hf, bank), reads=['xr%d' % r, 'ps%d' % bank], writes=['z%d' % r])
            emit_ln(z, 'z%d' % r, z, 'z%d' % r, xb, 'xb%d' % r, r)
            op('scalar', fm(lambda e, tt, z: e.mul(out=ACC[:, tt, :], in_=z, mul=ALPHA), tt, z), reads=['z%d' % r], writes=['acc%d_0' % tt, 'acc%d_1' % tt])
            emit_xT(tt, xb, 'xb%d' % r, 7)
        S.barrier()

    def ffn_phase(L, is_last):
        moe = (L % 2 == 1)
        nexp = NEXP if moe else 1
        nchunks = (DFFE if moe else DFF) // 128
        groups = []
        c0 = 0
        while c0 < nchunks:
            g = min(4, nchunks - c0)
            groups.append((c0, g))
            c0 += g
        hT = [V(Z0 + 48 * KB + i * 4 * KB, [128, 4, 512], BF16) for i in range(3)]
        sil = [V(Z0 + 60 * KB + i * KB, [128, 512], BF16) for i in range(2)]
        G0 = Z0 + 62 * KB
        rw = V(G0, [128, 8, 8], BF16)
        lg = V(G0 + 256, [128, 16, 8], F32)
        m8a = V(G0 + 256 + 512, [128, 16, 8], F32)
        gates = V(G0 + 256 + 1024, [128, 16, 8], F32)
        e2 = V(G0 + 256 + 1536, [128, 16, 1], F32)
        p1 = V(G0 + 256 + 1536 + 64, [128, 16, 1], F32)
        p2 = V(G0 + 256 + 1536 + 128, [128, 16, 1], F32)
        tmpg = V(G0 + 256 + 1536 + 192, [128, 16, 8], F32)
        if moe:
            op('gpsimd', lambda e: e.dma_start(out=rw, in_=dr["rt%d" % L].rearrange("(c p) n -> p c n", p=128)),
               writes=['rw'], dma_sem='rw')

            def lmm(e):
                for tt in range(NT):
                    for c in range(8):
                        i = e.matmul(PS(6)[:, tt * 8:(tt + 1) * 8], lhsT=xT[:, c, tt * 128:(tt + 1) * 128], rhs=rw[:, c, :],
                                     start=(c == 0), stop=(c == 7))
                return i
            op('tensor', lmm, reads=['xT', 'rw'], writes=['ps6'])
            op('vector', lambda e: e.tensor_copy(out=lg, in_=PS(6)[:, 0:128].rearrange("p (a b) -> p a b", a=16)),
               reads=['ps6'], writes=['lg'])
            for tt in range(NT):
                op('vector', fm(lambda e, tt: e.max(out=m8a[:, tt, :], in_=lg[:, tt, :]), tt), reads=['lg'], writes=['m8a'])
            op('vector', lambda e: e.tensor_tensor(out=e2, in0=m8a[:, :, 1:2], in1=m8a[:, :, 0:1], op=ALU.subtract),
               reads=['m8a'], writes=['e2'])
            op('scalar', lambda e: e.activation(out=e2, in_=e2, func=AF.Exp), reads=['e2'], writes=['e2'])
            op('vector', lambda e: e.tensor_scalar(out=p1, in0=e2, scalar1=1.0, scalar2=None, op0=ALU.add), reads=['e2'], writes=['p1'])
            op('vector', lambda e: e.reciprocal(out=p1, in_=p1), reads=['p1'], writes=['p1'])
            op('vector', lambda e: e.tensor_tensor(out=p2, in0=e2, in1=p1, op=ALU.mult), reads=['e2', 'p1'], writes=['p2'])
            op('vector', lambda e: e.tensor_tensor(out=gates, in0=lg, in1=m8a[:, :, 0:1].to_broadcast([128, 16, 8]), op=ALU.is_equal),
               reads=['lg', 'm8a'], writes=['gates'])
            op('vector', lambda e: e.tensor_tensor(out=gates, in0=gates, in1=p1.to_broadcast([128, 16, 8]), op=ALU.mult),
               reads=['gates', 'p1'], writes=['gates'])
            op('vector', lambda e: e.tensor_tensor(out=tmpg, in0=lg, in1=m8a[:, :, 1:2].to_broadcast([128, 16, 8]), op=ALU.is_equal),
               reads=['lg', 'm8a'], writes=['tmpg'])
            op('vector', lambda e: e.tensor_tensor(out=tmpg, in0=tmpg, in1=p2.to_broadcast([128, 16, 8]), op=ALU.mult),
               reads=['tmpg', 'p2'], writes=['tmpg'])
            op('vector', lambda e: e.tensor_tensor(out=gates, in0=gates, in1=tmpg, op=ALU.add), reads=['gates', 'tmpg'], writes=['gates'])

        units = []
        for ex in range(nexp):
            for (c0, g) in groups:
                for tc in range(4):
                    units.append((ex, c0, g, tc))
        wsl = [0]
        state = {}

        def load_w(ex, c0, g):
            s_ = wsl[0] % 2
            wsl[0] += 1
            base = Z0 + s_ * 24 * KB
            w1s = V(base, [128, 8, 512], BF16)
            w3s = V(base + 8 * KB, [128, 8, 512], BF16)
            w2s = V(base + 16 * KB, [128, 4, DM], BF16)
            if moe:
                a1 = dr["w1_%d" % L][ex]; a3 = dr["w3_%d" % L][ex]; a2 = dr["w2_%d" % L][ex]
            else:
                a1 = dr["w1_%d" % L]; a3 = dr["w3_%d" % L]; a2 = dr["w2_%d" % L]
            cs = slice(c0 * 128, (c0 + g) * 128)
            op('gpsimd', lambda e: e.dma_start(out=w1s[:, :, 0:g * 128], in_=a1[:, cs].rearrange("(c p) n -> p c n", p=128)),
               writes=['fw%d' % s_], dma_sem='fw%d' % s_)
            op('gpsimd', lambda e: e.dma_start(out=w3s[:, :, 0:g * 128], in_=a3[:, cs].rearrange("(c p) n -> p c n", p=128)),
               writes=['fw%d' % s_], dma_sem='fw%d' % s_)
            op('gpsimd', lambda e: e.dma_start(out=w2s[:, 0:g, :], in_=a2[cs, :].rearrange("(j p) n -> p j n", p=128)),
               writes=['fw%d' % s_], dma_sem='fw%d' % s_)
            return (s_, w1s, w3s, w2s)

        def partA(ui):
            ex, c0, g, tc = units[ui]
            if (ex, c0) not in state:
                state[(ex, c0)] = load_w(ex, c0, g)
            s_, w1s, w3s, w2s = state[(ex, c0)]
            hb = ui % 3
            tok = slice(tc * 512, (tc + 1) * 512)
            for j in range(g):
                pa = j % 2

                def mm(e, w_, bank, j=j):
                    for k in range(8):
                        i = e.matmul(PS(bank), lhsT=w_[:, k, j * 128:(j + 1) * 128], rhs=xT[:, k, tok], start=(k == 0), stop=(k == 7))
                    return i
                op('tensor', fm(mm, w1s, pa), reads=['fw%d' % s_, 'xT'], writes=['ps%d' % pa])
                op('tensor', fm(mm, w3s, 2 + pa), reads=['fw%d' % s_, 'xT'], writes=['ps%d' % (2 + pa)])
                op('scalar', fm(lambda e, pa: e.activation(out=sil[pa], in_=PS(pa), func=AF.Silu), pa),
                   reads=['ps%d' % pa], writes=['sil%d' % pa])
                op('vector', fm(lambda e, pa, j, hb: e.tensor_tensor(out=hT[hb][:, j, :], in0=sil[pa], in1=PS(2 + pa), op=ALU.mult),
                                pa, j, hb), reads=['sil%d' % pa, 'ps%d' % (2 + pa)], writes=['hT%d' % hb])

        ycount = [0]

        def partB(ui):
            ex, c0, g, tc = units[ui]
            s_, w1s, w3s, w2s = state[(ex, c0)]
            hb = ui % 3
            for t4 in range(4):
                tt = tc * 4 + t4
                for hf in range(2):
                    bank = 4 + ycount[0] % 2
                    ycount[0] += 1

                    def mm(e, t4=t4, hf=hf, bank=bank):
                        for j in range(g):
                            i = e.matmul(PS(bank), lhsT=hT[hb][:, j, t4 * 128:(t4 + 1) * 128], rhs=w2s[:, j, hf * 512:(hf + 1) * 512],
                                         start=(j == 0), stop=(j == g - 1))
                        return i
                    op('tensor', mm, reads=['hT%d' % hb, 'fw%d' % s_], writes=['ps%d' % bank])
                    dst = ACC[:, tt, hf * 512:(hf + 1) * 512]
                    sc_ = gates[:, tt, ex:ex + 1] if moe else 1.0
                    op('vector', fm(lambda e, dst, bank, sc_: e.scalar_tensor_tensor(out=dst, in0=PS(bank), scalar=sc_, in1=dst,
                                                                                     op0=ALU.mult, op1=ALU.add), dst, bank, sc_),
                       reads=['ps%d' % bank, 'acc%d_%d' % (tt, hf), 'gates'], writes=['acc%d_%d' % (tt, hf)])

        for ui in range(len(units)):
            partA(ui)
            if ui >= 1:
                partB(ui - 1)
        partB(len(units) - 1)
        load_ln_params(L, 2)
        for tt in range(NT):
            r = tt % 2
            xn = V(Z0 + 66 * KB + r * 4 * KB, [128, DM], F32)
            xb = V(Z0 + 74 * KB + r * 2 * KB, [128, DM], BF16)
            z = ACC[:, tt, :]
            emit_ln(z, 'acc%d_0' % tt, xn, 'xn%d' % r, None if is_last else xb, 'xb%d' % r, r, z_res2='acc%d_1' % tt)
            dstd = y_out if is_last else xsp
            op('sync', fm(lambda e, xn, tt, dstd: e.dma_start(out=dstd[tt * 128:(tt + 1) * 128, :], in_=xn), xn, tt, dstd),
               reads=['xn%d' % r], writes=['xout'], dma_sem='xo%d' % r)
            if not is_last:
                emit_xT(tt, xb, 'xb%d' % r, 7)
        S.barrier()

    def moe_sparse_phase(L, is_last):
        C = MOE_C
        NST = C // 128
        CCH = [(0, 512), (512, C - 512)]
        SEL = V(0, [128, NT, C], BF16)
        YBF = V(20 * KB, [128, NST, DM], BF16)
        XG = V(108 * KB, [128, 8, C], BF16)
        YACC = V(118 * KB, [128, NST, DM], F32)
        sil = [V(30 * KB, [128, C], BF16), V(138 * KB, [128, C], BF16)]
        hT = [V(66 * KB + i * 3 * KB, [128, 2, C], BF16) for i in range(3)]
        G0 = 32 * KB
        rw = V(G0, [128, 8, 8], BF16)
        lg = V(G0 + 256, [128, 16, 8], F32)
        m8a = V(G0 + 768, [128, 16, 8], F32)
        gates = V(G0 + 1280, [128, 16, 8], F32)
        tmpg = V(G0 + 1792, [128, 16, 8], F32)
        e2 = V(G0 + 2304, [128, 16, 1], F32)
        p1 = V(G0 + 2368, [128, 16, 1], F32)
        p2 = V(G0 + 2432, [128, 16, 1], F32)
        ind = V(G0 + 2560, [128, 16, 8], BF16)
        posm = V(G0 + 2816, [128, 16, 8], F32)
        iotap = V(G0 + 3328, [128, 8], F32)
        tris = V(G0 + 3392, [128, 128], BF16)
        onesb = V(G0 + 3648, [128, 128], BF16)
        iotar = V(G0 + 4096, [128, C], F32)
        op('gpsimd', lambda e: e.dma_start(out=rw, in_=dr["rt%d" % L].rearrange("(c p) n -> p c n", p=128)),
           writes=['rw'], dma_sem='rw')
        op('gpsimd', lambda e: e.dma_start(out=tris, in_=dr['tris']), writes=['tris'], dma_sem='tris')
        op('sync', lambda e: e.dma_start(out=iotap, in_=dr['iotap']), writes=['iotap'], dma_sem='iotap')
        op('sync', lambda e: e.dma_start(out=iotar, in_=dr['iotar']), writes=['iotar'], dma_sem='iotar')
        op('vector', lambda e: e.memset(onesb, 1.0), writes=['onesb'])

        def lmm(e):
            for tt in range(NT):
                for c in range(8):
                    i = e.matmul(PS(6)[:, tt * 8:(tt + 1) * 8], lhsT=xT[:, c, tt * 128:(tt + 1) * 128], rhs=rw[:, c, :],
                                 start=(c == 0), stop=(c == 7))
            return i
        op('tensor', lmm, reads=['xT', 'rw'], writes=['ps6'])
        op('vector', lambda e: e.tensor_copy(out=lg, in_=PS(6)[:, 0:128].rearrange("p (a b) -> p a b", a=16)),
           reads=['ps6'], writes=['lg'])
        for tt in range(NT):
            op('vector', fm(lambda e, tt: e.max(out=m8a[:, tt, :], in_=lg[:, tt, :]), tt), reads=['lg'], writes=['m8a'])
        op('vector', lambda e: e.tensor_tensor(out=e2, in0=m8a[:, :, 1:2], in1=m8a[:, :, 0:1], op=ALU.subtract),
           reads=['m8a'], writes=['e2'])
        op('scalar', lambda e: e.activation(out=e2, in_=e2, func=AF.Exp), reads=['e2'], writes=['e2'])
        op('vector', lambda e: e.tensor_scalar(out=p1, in0=e2, scalar1=1.0, scalar2=None, op0=ALU.add), reads=['e2'], writes=['p1'])
        op('vector', lambda e: e.reciprocal(out=p1, in_=p1), reads=['p1'], writes=['p1'])
        op('vector', lambda e: e.tensor_tensor(out=p2, in0=e2, in1=p1, op=ALU.mult), reads=['e2', 'p1'], writes=['p2'])
        op('vector', lambda e: e.tensor_tensor(out=gates, in0=lg, in1=m8a[:, :, 0:1].to_broadcast([128, 16, 8]), op=ALU.is_equal),
           reads=['lg', 'm8a'], writes=['gates'])
        op('vector', lambda e: e.tensor_tensor(out=gates, in0=gates, in1=p1.to_broadcast([128, 16, 8]), op=ALU.mult),
           reads=['gates', 'p1'], writes=['gates'])
        op('vector', lambda e: e.tensor_tensor(out=tmpg, in0=lg, in1=m8a[:, :, 1:2].to_broadcast([128, 16, 8]), op=ALU.is_equal),
           reads=['lg', 'm8a'], writes=['tmpg'])
        op('vector', lambda e: e.tensor_tensor(out=tmpg, in0=tmpg, in1=p2.to_broadcast([128, 16, 8]), op=ALU.mult),
           reads=['tmpg', 'p2'], writes=['tmpg'])
        op('vector', lambda e: e.tensor_tensor(out=gates, in0=gates, in1=tmpg, op=ALU.add), reads=['gates', 'tmpg'], writes=['gates'])
        op('vector', lambda e: e.tensor_scalar(out=ind, in0=gates, scalar1=0.0, scalar2=None, op0=ALU.is_gt),
           reads=['gates'], writes=['ind'])

        def pmm(e):
            for tt in range(NT):
                for tp in range(tt + 1):
                    i = e.matmul(PS(6)[:, tt * 8:(tt + 1) * 8], lhsT=(onesb if tp < tt else tris), rhs=ind[:, tp, :],
                                 start=(tp == 0), stop=(tp == tt))
            return i
        op('tensor', pmm, reads=['ind', 'onesb', 'tris'], writes=['ps6'])
        op('vector', lambda e: e.scalar_tensor_tensor(out=posm, in0=PS(6)[:, 0:128].rearrange("p (a b) -> p a b", a=16), scalar=1.0,
                                                      in1=ind, op0=ALU.add, op1=ALU.mult), reads=['ps6', 'ind'], writes=['posm'])
        op('vector', lambda e: e.tensor_scalar(out=posm, in0=posm, scalar1=-1.0, scalar2=None, op0=ALU.add),
           reads=['posm'], writes=['posm'])
        S.barrier()

        nchunks = DFFE // 128
        groups = [(c0, 2) for c0 in range(0, nchunks, 2)]
        a1_ = dr["w1_%d" % L]
        a3_ = dr["w3_%d" % L]
        a2_ = dr["w2_%d" % L]
        wsl = [0]
        bk = [0]
        ycount = [0]

        def load_w(ex, c0, g):
            s_ = wsl[0] % 3
            wsl[0] += 1
            base = (Z0 + s_ * 12 * KB) if s_ < 2 else 0
            w1s = V(base, [128, 8, 256], BF16)
            w3s = V(base + 4 * KB, [128, 8, 256], BF16)
            w2s = V(base + 8 * KB, [128, 2, DM], BF16)
            cs = slice(c0 * 128, (c0 + g) * 128)
            wr = ['fw%d' % s_] + (['R0'] if s_ == 2 else [])
            op('gpsimd', lambda e: e.dma_start(out=w1s, in_=a1_[ex][:, cs].rearrange("(c p) n -> p c n", p=128)),
               writes=wr, dma_sem='fw%d' % s_)
            op('gpsimd', lambda e: e.dma_start(out=w3s, in_=a3_[ex][:, cs].rearrange("(c p) n -> p c n", p=128)),
               writes=wr, dma_sem='fw%d' % s_)
            op('gpsimd', lambda e: e.dma_start(out=w2s, in_=a2_[ex][cs, :].rearrange("(j p) n -> p j n", p=128)),
               writes=wr, dma_sem='fw%d' % s_)
            return (s_, w1s, w3s, w2s)

        YN = ['yacc%d_%d' % (a_, b_) for a_ in range(NST) for b_ in range(2)]

        def sel_build(ex):
            for tt in range(NT):
                op('vector', fm(lambda e, tt, ex: e.tensor_scalar(out=SEL[:, tt, :], in0=iotar, scalar1=posm[:, tt, ex:ex + 1],
                                                                 scalar2=None, op0=ALU.is_equal), tt, ex),
                   reads=['iotar', 'posm'], writes=['sel%d' % tt, 'R0'])

        def gather(ex):
            for fc in range(8):
                ba = bk[0] % 2
                bk[0] += 1

                def gmm(e, fc=fc, ba=ba):
                    for tt in range(NT):
                        e.matmul(PS(ba), lhsT=XTOK[:, tt, fc * 128:(fc + 1) * 128], rhs=SEL[:, tt, 0:512],
                                 start=(tt == 0), stop=(tt == NT - 1))
                    for tt in range(NT):
                        i = e.matmul(PS(2 + ba)[:, 0:C - 512], lhsT=XTOK[:, tt, fc * 128:(fc + 1) * 128], rhs=SEL[:, tt, 512:C],
                                     start=(tt == 0), stop=(tt == NT - 1))
                    return i
                op('tensor', gmm, reads=['xtok', 'R0'] + ['sel%d' % t for t in range(NT)], writes=['ps%d' % ba, 'ps%d' % (2 + ba)])
                op('scalar', fm(lambda e, fc, ba: e.copy(out=XG[:, fc, 0:512], in_=PS(ba)), fc, ba),
                   reads=['ps%d' % ba], writes=['xg'])
                op('scalar', fm(lambda e, fc, ba: e.copy(out=XG[:, fc, 512:C], in_=PS(2 + ba)[:, 0:C - 512]), fc, ba),
                   reads=['ps%d' % (2 + ba)], writes=['xg'])

        SELT = V(118 * KB, [128, NST, SEQ], BF16)

        def selt_mm(ex):
            for tc in range(4):
                qb = 6 + tc % 2

                def bmm(e, tc=tc, qb=qb, ex=ex):
                    for t4 in range(4):
                        tt = tc * 4 + t4
                        for tp in range(tt + 1):
                            i = e.matmul(PS(qb)[:, t4 * 128:(t4 + 1) * 128],
                                         lhsT=ind[:, tp, ex:ex + 1].to_broadcast([128, 128]),
                                         rhs=(onesb if tp < tt else tris), start=(tp == 0), stop=(tp == tt))
                    return i
                op('tensor', bmm, reads=['ind', 'onesb', 'tris'], writes=['ps%d' % qb])
                for st_ in range(NST):
                    op('vector', fm(lambda e, tc, qb, st_: e.tensor_scalar(out=SELT[:, st_, tc * 512:(tc + 1) * 512], in0=PS(qb),
                                                                           scalar1=iotap[:, st_:st_ + 1], scalar2=None,
                                                                           op0=ALU.is_equal), tc, qb, st_),
                       reads=['ps%d' % qb, 'iotap'], writes=['selt'] + YN)

        def scatter(ex):
            for tt in range(NT):
                for hf in range(2):
                    bank = 4 + ycount[0] % 2
                    ycount[0] += 1

                    def smm(e, tt=tt, hf=hf, bank=bank):
                        for st_ in range(NST):
                            i = e.matmul(PS(bank), lhsT=SELT[:, st_, tt * 128:(tt + 1) * 128], rhs=YBF[:, st_, hf * 512:(hf + 1) * 512],
                                         start=(st_ == 0), stop=(st_ == NST - 1))
                        return i
                    op('tensor', smm, reads=['selt', 'ybf'] + YN, writes=['ps%d' % bank])
                    dst = ACC[:, tt, hf * 512:(hf + 1) * 512]
                    op('vector', fm(lambda e, dst, bank, tt, ex: e.scalar_tensor_tensor(
                        out=dst, in0=PS(bank), scalar=gates[:, tt, ex:ex + 1], in1=dst, op0=ALU.mult, op1=ALU.add), dst, bank, tt, ex),
                       reads=['ps%d' % bank, 'acc%d_%d' % (tt, hf), 'gates'], writes=['acc%d_%d' % (tt, hf)])

        sel_build(0)
        gather(0)
        for ex in range(NEXP):
            wstate = {}

            def partA(gi):
                c0, g = groups[gi]
                wstate[gi] = load_w(ex, c0, g)
                s_, w1s, w3s, w2s = wstate[gi]
                hb = gi % 3
                for (cs0, cn) in CCH:
                    for j in range(g):
                        pa = bk[0] % 2
                        bk[0] += 1

                        def mm(e, w_, bank, j=j, cs0=cs0, cn=cn):
                            for k in range(8):
                                i = e.matmul(PS(bank)[:, 0:cn], lhsT=w_[:, k, j * 128:(j + 1) * 128], rhs=XG[:, k, cs0:cs0 + cn],
                                             start=(k == 0), stop=(k == 7))
                            return i
                        r0 = ['R0'] if s_ == 2 else []
                        op('tensor', fm(mm, w1s, pa), reads=['fw%d' % s_, 'xg'] + r0, writes=['ps%d' % pa])
                        op('tensor', fm(mm, w3s, 2 + pa), reads=['fw%d' % s_, 'xg'] + r0, writes=['ps%d' % (2 + pa)])
                        op('scalar', fm(lambda e, pa, cn: e.activation(out=sil[pa][:, 0:cn], in_=PS(pa)[:, 0:cn], func=AF.Silu), pa, cn),
                           reads=['ps%d' % pa], writes=['sil%d' % pa])
                        op('vector', fm(lambda e, pa, j, hb, cs0, cn: e.tensor_tensor(
                            out=hT[hb][:, j, cs0:cs0 + cn], in0=sil[pa][:, 0:cn], in1=PS(2 + pa)[:, 0:cn], op=ALU.mult),
                            pa, j, hb, cs0, cn), reads=['sil%d' % pa, 'ps%d' % (2 + pa)], writes=['hT%d' % hb])

            def partB(gi):
                c0, g = groups[gi]
                s_, w1s, w3s, w2s = wstate[gi]
                hb = gi % 3
                for st_ in range(NST):
                    for hf in range(2):
                        bank = 4 + ycount[0] % 2
                        ycount[0] += 1

                        def mm(e, st_=st_, hf=hf, bank=bank):
                            for j in range(g):
                                i = e.matmul(PS(bank), lhsT=hT[hb][:, j, st_ * 128:(st_ + 1) * 128],
                                             rhs=w2s[:, j, hf * 512:(hf + 1) * 512], start=(j == 0), stop=(j == g - 1))
                            return i
                        op('tensor', mm, reads=['hT%d' % hb, 'fw%d' % s_] + (['R0'] if s_ == 2 else []), writes=['ps%d' % bank])
                        dst = YACC[:, st_, hf * 512:(hf + 1) * 512]
                        rn = 'yacc%d_%d' % (st_, hf)
                        if gi == 0:
                            op('vector', fm(lambda e, dst, bank: e.tensor_copy(out=dst, in_=PS(bank)), dst, bank),
                               reads=['ps%d' % bank], writes=[rn])
                        else:
                            op('vector', fm(lambda e, dst, bank: e.tensor_tensor(out=dst, in0=dst, in1=PS(bank), op=ALU.add), dst, bank),
                               reads=['ps%d' % bank, rn], writes=[rn])

            for gi in range(len(groups)):
                partA(gi)
                if gi >= 1:
                    partB(gi - 1)
            partB(len(groups) - 1)
            for st_ in range(NST):
                eng = 'scalar' if st_ % 2 == 0 else 'gpsimd'
                if eng == 'scalar':
                    op(eng, fm(lambda e, st_: e.copy(out=YBF[:, st_, :], in_=YACC[:, st_, :]), st_),
                       reads=['yacc%d_0' % st_, 'yacc%d_1' % st_], writes=['ybf'])
                else:
                    op(eng, fm(lambda e, st_: e.tensor_copy(out=YBF[:, st_, :], in_=YACC[:, st_, :]), st_),
                       reads=['yacc%d_0' % st_, 'yacc%d_1' % st_], writes=['ybf'])
            if ex + 1 < NEXP:
                sel_build(ex + 1)
            selt_mm(ex)
            if ex + 1 < NEXP:
                gather(ex + 1)
            scatter(ex)
        S.barrier()
        load_ln_params(L, 2)
        for tt in range(NT):
            r = tt % 2
            xn = V(Z0 + 66 * KB + r * 4 * KB, [128, DM], F32)
            xb = V(Z0 + 74 * KB + r * 2 * KB, [128, DM], BF16)
            z = ACC[:, tt, :]
            emit_ln(z, 'acc%d_0' % tt, xn, 'xn%d' % r, None if is_last else xb, 'xb%d' % r, r, z_res2='acc%d_1' % tt)
            dstd = y_out if is_last else xsp
            op('sync', fm(lambda e, xn, tt, dstd: e.dma_start(out=dstd[tt * 128:(tt + 1) * 128, :], in_=xn), xn, tt, dstd),
               reads=['xn%d' % r], writes=['xout'], dma_sem='xo%d' % r)
            if not is_last:
                emit_xT(tt, xb, 'xb%d' % r, 7)
        S.barrier()

    initial_load()
    S.barrier()
    resid = x_in
    for li, L in enumerate(layer_ids):
        load_mixer_consts()
        if L % 2 == 0:
            if not (3.0 < stop <= 3.05):
                dsa_phase(L)
            if stop < 3:
                break
            pair_phase(L, 'moba', 4, 936, 1448, 1960, 4)
        else:
            pair_phase(L, 'dil', 8, 0, 1024, 2048, 0)
        if stop < 4:
            break
        outproj_phase(L, resid)
        if stop < 5:
            break
        if SPARSE_MOE and L % 2 == 1:
            moe_sparse_phase(L, li == len(layer_ids) - 1)
        else:
            ffn_phase(L, li == len(layer_ids) - 1)
        resid = xsp
    S.barrier()
    S.emit(nc)
    st.close()
    return nc


_CACHE = {}


def _layer_inputs(L, inp):
    i = L // 2
    d = {}
    if L % 2 == 0:
        w = np.ascontiguousarray(inp['even_w_in'][i])
        d["win%d" % L] = w
        sw = w.copy()
        sw = swap_halves(sw, 0, 8, 64)
        sw = swap_halves(sw, 512, 1, 64)
        sw = swap_halves(sw, 640, 8, 32)
        sw = swap_halves(sw, 896, 1, 32)
        sw = swap_halves(sw, 936, 8, 64)
        sw = swap_halves(sw, 1448, 8, 64)
        d["wsw%d" % L] = sw
        d["wout%d" % L] = np.ascontiguousarray(inp['even_w_out'][i])
        d["w1_%d" % L] = np.ascontiguousarray(inp['even_w1'][i])
        d["w3_%d" % L] = np.ascontiguousarray(inp['even_w3'][i])
        d["w2_%d" % L] = np.ascontiguousarray(inp['even_w2'][i])
        pre = 'even'
    else:
        w = np.ascontiguousarray(inp['odd_w_in'][i])
        d["win%d" % L] = w
        sw = w.copy()
        sw = swap_halves(sw, 0, 16, 64)
        sw = swap_halves(sw, 1024, 16, 64)
        d["wsw%d" % L] = sw
        d["wout%d" % L] = np.ascontiguousarray(inp['odd_w_out'][i])
        d["rt%d" % L] = np.ascontiguousarray(inp['odd_router'][i])
        d["w1_%d" % L] = np.ascontiguousarray(inp['odd_w1'][i])
        d["w3_%d" % L] = np.ascontiguousarray(inp['odd_w3'][i])
        d["w2_%d" % L] = np.ascontiguousarray(inp['odd_w2'][i])
        pre = 'odd'
    d["g1_%d" % L] = np.ascontiguousarray(inp[pre + '_ln1_g'][i]).reshape(1, DM)
    d["b1_%d" % L] = np.ascontiguousarray(inp[pre + '_ln1_b'][i]).reshape(1, DM)
    d["g2_%d" % L] = np.ascontiguousarray(inp[pre + '_ln2_g'][i]).reshape(1, DM)
    d["b2_%d" % L] = np.ascontiguousarray(inp[pre + '_ln2_b'][i]).reshape(1, DM)
    return d


def swap_fix(w):
    return w


def run_layers(x, layer_ids, inp):
    key = tuple(layer_ids)
    if key not in _CACHE:
        _CACHE[key] = build(list(layer_ids))
    nc = _CACHE[key]
    shared = dict(make_consts())
    for L in layer_ids:
        shared.update(_layer_inputs(L, inp))
    in_maps = []
    for b in range(8):
        m = dict(shared)
        m["x"] = np.ascontiguousarray(x[b])
        in_maps.append(m)
    res = run_bass_kernel_spmd(nc, in_maps, core_ids=list(range(8)))
    return np.stack([np.asarray(r["y"]) for r in res.results], axis=0).astype(np.float32)


LAUNCH_GROUPS = [[0, 1, 2, 3]]


def kernel(**inputs):
    inp = {k: np.asarray(v) for k, v in inputs.items()}
    x = np.asarray(inp['x'], dtype=np.float32)
    for grp in LAUNCH_GROUPS:
        x = run_layers(x, grp, inp)
    return x
```
